# Optimizing a Trainium2 kernel written in Bass

```python
import math
import jax, jax.numpy as jnp
from jax import lax
import numpy as np

D_MODEL = 1024
BATCH = 32
SEQ = 2048
DEPTH = 4

HEAD_DIM = 64
N_BRANCHES = 4
BRANCH_WIDTH = D_MODEL // 4

A_HEADS = BRANCH_WIDTH // HEAD_DIM
A_PATTERNS = ((128, 1), (512, 4), (2048, 16))
A_BLOCK = 128
B_HEADS = BRANCH_WIDTH // HEAD_DIM
B_LATENT = D_MODEL // 16
IDX_HEADS = 8
IDX_DIM = 32
TOPK_MAX = 256
TOPK_DIV = 4
Q_BLOCK = 128
C_HEADS = 4
C_KEY_DIM = 32
C_VAL_DIM = BRANCH_WIDTH // C_HEADS
C_GATE_RANK = 16
C_GATE_TAU = 16.0
C_CHUNK = 64
D_HEADS = 4
D_HEAD_SIZE = BRANCH_WIDTH // D_HEADS
D_DECAY_RANK = 16
D_ICLR_RANK = 16
D_GATE_RANK = 32
D_GN_EPS = 64e-5
D_IN_WIDTH = 3 * BRANCH_WIDTH + D_DECAY_RANK + D_ICLR_RANK + D_GATE_RANK
RPB_BUCKETS = 32
RPB_MAX_DIST = 2048
RPB_HEADS = A_HEADS + B_HEADS
MOE_GROUPS = 4
MOE_PER_GROUP = 8
MOE_EXPERTS = MOE_GROUPS * MOE_PER_GROUP
MOE_TOPK = 2
MOE_HIDDEN = 256
DN_ALPHA = (2 * DEPTH) ** 0.25
DN_BETA = (8 * DEPTH) ** -0.25
LN_EPS = 1e-5

IN_SPLITS = (
    ("a_q", A_HEADS * HEAD_DIM), ("a_k", A_HEADS * HEAD_DIM), ("a_v", A_HEADS * HEAD_DIM),
    ("b_q", B_HEADS * B_LATENT), ("b_ckv", B_LATENT), ("b_iq", IDX_HEADS * IDX_DIM),
    ("b_ik", IDX_DIM), ("b_iw", IDX_HEADS),
    ("c_q", C_HEADS * C_KEY_DIM), ("c_k", C_HEADS * C_KEY_DIM), ("c_v", C_HEADS * C_VAL_DIM),
    ("c_a", C_GATE_RANK), ("c_g", BRANCH_WIDTH),
    ("d", D_IN_WIDTH),
    ("gate", N_BRANCHES * D_MODEL),
)
IN_WIDTH = sum(w for _, w in IN_SPLITS)

kernel_name = "hybrid_gated_four_mixer_hmoe_deepnorm"


def _split_in(h):
    out = {}
    off = 0
    for name, w in IN_SPLITS:
        out[name] = h[..., off:off + w]
        off += w
    return out


def _layer_norm(x, g, b):
    xf = x.astype(jnp.float32)
    mu = jnp.mean(xf, -1, keepdims=True)
    var = jnp.mean(jnp.square(xf - mu), -1, keepdims=True)
    return ((xf - mu) * lax.rsqrt(var + LN_EPS) * g + b).astype(x.dtype)


def _rms_norm(x, g, eps=1e-6):
    xf = x.astype(jnp.float32)
    return (xf * lax.rsqrt(jnp.mean(xf * xf, -1, keepdims=True) + eps) * g).astype(x.dtype)


def _t5_bucket(dist):
    exact = RPB_BUCKETS // 2
    d = jnp.maximum(dist, 0)
    df = jnp.maximum(d, 1).astype(jnp.float32)
    large = exact + (jnp.log(df / exact) / math.log(RPB_MAX_DIST / exact)
                     * (RPB_BUCKETS - exact)).astype(jnp.int32)
    large = jnp.minimum(large, RPB_BUCKETS - 1)
    return jnp.where(d < exact, d, large)


def _dilated_pattern(q, k, v, bias_tab, window, dilation):
    bsz, seq, nh, dh = q.shape
    lr = seq // dilation
    span = window // dilation
    nb = -(-lr // A_BLOCK)
    lp = nb * A_BLOCK
    bd = bsz * dilation

    def regroup(z):
        z = z.reshape(bsz, lr, dilation, nh, dh).transpose(0, 2, 1, 3, 4)
        return z.reshape(bd, lr, nh, dh)

    qr, kr, vr = regroup(q), regroup(k), regroup(v)
    qb = jnp.pad(qr, ((0, 0), (0, lp - lr), (0, 0), (0, 0))).reshape(bd, nb, A_BLOCK, nh, dh)

    def band(z):
        zp = jnp.pad(z, ((0, 0), (A_BLOCK, lp - lr), (0, 0), (0, 0)))
        prev = zp[:, :lp].reshape(bd, nb, A_BLOCK, nh, dh)
        cur = zp[:, A_BLOCK:].reshape(bd, nb, A_BLOCK, nh, dh)
        return jnp.concatenate([prev, cur], axis=2)

    kb, vb = band(kr), band(vr)
    qi = jnp.arange(A_BLOCK)[:, None]
    kj = jnp.arange(2 * A_BLOCK)[None, :]
    rdist = qi + A_BLOCK - kj
    in_band = (rdist >= 0) & (rdist <= span)
    bias = jnp.transpose(bias_tab[_t5_bucket(rdist * dilation)], (2, 0, 1))
    kpos = jnp.arange(nb)[:, None] * A_BLOCK - A_BLOCK + kj
    mask = in_band[None] & (kpos >= 0)[:, None, :]

    s = jnp.einsum('bnqhd,bnkhd->bnhqk', qb, kb).astype(jnp.float32) * dh ** -0.5 + bias
    s = jnp.where(mask[None, :, None], s, -jnp.inf)
    m = jnp.max(s, -1, keepdims=True)
    p = jnp.exp(s - m)
    den = jnp.sum(p, -1)
    o = jnp.einsum('bnhqk,bnkhd->bnqhd', p, vb.astype(jnp.float32))
    o = o / jnp.swapaxes(den, 2, 3)[..., None]
    lse = jnp.swapaxes(m[..., 0] + jnp.log(den), 2, 3)
    o = o.reshape(bd, lp, nh, dh)
    lse = lse.reshape(bd, lp, nh)

    def ungroup(z):
        z = z[:, :lr].reshape((bsz, dilation, lr) + z.shape[2:])
        z = jnp.moveaxis(z, 1, 2)
        return z.reshape((bsz, seq) + z.shape[3:])

    return ungroup(o), ungroup(lse)


def _mixer_a(q, k, v, bias_tab):
    bsz, seq, _ = q.shape
    shp = (bsz, seq, A_HEADS, HEAD_DIM)
    q, k, v = q.reshape(shp), k.reshape(shp), v.reshape(shp)
    outs, lses = [], []
    for window, dilation in A_PATTERNS:
        o, lse = _dilated_pattern(q, k, v, bias_tab, window, dilation)
        outs.append(o)
        lses.append(lse)
    wts = jax.nn.softmax(jnp.stack(lses), axis=0)
    o = jnp.sum(jnp.stack(outs) * wts[..., None], axis=0)
    return o.reshape(bsz, seq, A_HEADS * HEAD_DIM).astype(q.dtype)


def _mixer_b(q, ckv, iq, ik, iw, kv_gain, w_uv, bias_tab):
    bsz, seq, _ = q.shape
    keep = min(TOPK_MAX, seq // TOPK_DIV)
    nblk = seq // Q_BLOCK
    q = q.reshape(bsz, seq, B_HEADS, B_LATENT)
    ckv = _rms_norm(ckv, kv_gain)
    iq = iq.reshape(bsz, seq, IDX_HEADS, IDX_DIM)

    def to_blocks(z):
        return jnp.moveaxis(z.reshape((bsz, nblk, Q_BLOCK) + z.shape[2:]), 1, 0)

    def one_block(args):
        bi, qb, iqb, iwb = args
        tpos = bi * Q_BLOCK + jnp.arange(Q_BLOCK)
        rel = jax.nn.relu(jnp.einsum('bqhd,bsd->bqhs', iqb, ik).astype(jnp.float32) * IDX_DIM ** -0.5)
        score = jnp.einsum('bqhs,bqh->bqs', rel, iwb.astype(jnp.float32)) * IDX_HEADS ** -0.5
        admissible = jnp.arange(seq)[None, :] <= tpos[:, None]
        score = jnp.where(admissible[None], score, -jnp.inf)
        _, idx = lax.top_k(score, keep)
        sel = jax.vmap(lambda c, i: c[i])(ckv, idx)
        logits = jnp.einsum('bqhc,bqkc->bqhk', qb, sel).astype(jnp.float32) * B_LATENT ** -0.5
        dist = tpos[None, :, None] - idx
        bias = jnp.moveaxis(bias_tab[_t5_bucket(dist)], -1, 2)
        logits = jnp.where((dist >= 0)[:, :, None, :], logits + bias, -jnp.inf)
        p = jax.nn.softmax(logits, axis=-1)
        return jnp.einsum('bqhk,bqkc->bqhc', p.astype(sel.dtype), sel)

    ob = lax.map(one_block, (jnp.arange(nblk), to_blocks(q), to_blocks(iq), to_blocks(iw)))
    o = jnp.moveaxis(ob, 0, 1).reshape(bsz, seq, B_HEADS, B_LATENT)
    o = jnp.einsum('bshc,hcd->bshd', o, w_uv)
    return o.reshape(bsz, seq, B_HEADS * HEAD_DIM).astype(q.dtype)


def _mixer_c(q, k, v, a_low, og, a_up, a_bias, norm_gain):
    f32 = jnp.float32
    bsz, seq, _ = q.shape
    nc = seq // C_CHUNK
    log_a = jax.nn.log_sigmoid((a_low @ a_up + a_bias).astype(f32)) / C_GATE_TAU

    def chunks(z, dim):
        return z.astype(f32).reshape(bsz, nc, C_CHUNK, C_HEADS, dim)

    qc = chunks(q, C_KEY_DIM) * C_KEY_DIM ** -0.5
    kc = chunks(k, C_KEY_DIM)
    vc = chunks(v, C_VAL_DIM)
    cum = jnp.cumsum(chunks(log_a, C_KEY_DIM), axis=2)
    last = cum[:, :, -1]
    q_dec = qc * jnp.exp(cum)
    k_inv = kc * jnp.exp(-cum)
    causal = jnp.tril(jnp.ones((C_CHUNK, C_CHUNK), bool))
    att = jnp.where(causal, jnp.einsum('bnihd,bnjhd->bnhij', q_dec, k_inv), 0.0)
    o_intra = jnp.einsum('bnhij,bnjhv->bnihv', att, vc)
    upd = jnp.einsum('bnjhd,bnjhv->bnhdv', kc * jnp.exp(last[:, :, None] - cum), vc)
    dec = jnp.exp(last)

    def step(state, inp):
        d, u = inp
        return d[..., None] * state + u, state

    init = jnp.zeros((bsz, C_HEADS, C_KEY_DIM, C_VAL_DIM), f32)
    _, before = lax.scan(step, init, (jnp.moveaxis(dec, 1, 0), jnp.moveaxis(upd, 1, 0)))
    before = jnp.moveaxis(before, 0, 1)
    o = o_intra + jnp.einsum('bnihd,bnhdv->bnihv', q_dec, before)
    o = o.reshape(bsz, seq, C_HEADS, C_VAL_DIM)
    o = o * lax.rsqrt(jnp.mean(o * o, -1, keepdims=True) + 1e-6)
    o = o.reshape(bsz, seq, BRANCH_WIDTH) * norm_gain
    return (jax.nn.silu(og.astype(f32)) * o).astype(q.dtype)


def _token_shift(z, mu):
    prev = jnp.pad(z, ((0, 0), (1, 0), (0, 0)))[:, :-1]
    return z + (prev - z) * mu


def _mixer_d(seg, mu, w0, w2, a0, a2, g2, k_k, k_a, r_k, gn_w, gn_b):
    f32 = jnp.float32
    out_dtype = seg.dtype
    bsz, seq, _ = seg.shape
    bw = BRANCH_WIDTH
    seg = _token_shift(seg, mu).astype(f32)
    r, k, v = seg[..., :bw], seg[..., bw:2 * bw], seg[..., 2 * bw:3 * bw]
    o1 = 3 * bw
    o2 = o1 + D_DECAY_RANK
    o3 = o2 + D_ICLR_RANK
    wl, al, gl = seg[..., o1:o2], seg[..., o2:o3], seg[..., o3:o3 + D_GATE_RANK]
    w_raw = -jax.nn.softplus(-(w0 + jnp.tanh(wl) @ w2)) - 0.5
    w = jnp.exp(-jnp.exp(w_raw))
    a = jax.nn.sigmoid(a0 + al @ a2)
    g = jax.nn.sigmoid(gl) @ g2

    def heads(z):
        return z.astype(f32).reshape(bsz, seq, D_HEADS, D_HEAD_SIZE)

    kk = heads(k * k_k)
    kk = kk / jnp.maximum(jnp.sqrt(jnp.sum(kk * kk, -1, keepdims=True)), 1e-12)
    k = k * (1.0 + (a - 1.0) * k_a)
    rh, kh, vh, wh, ah = heads(r), heads(k), heads(v), heads(w), heads(a)
    a_vec = -kk
    b_vec = kk * ah

    def step(state, inp):
        rt, wt, kt, vt, at, bt = inp
        sa = jnp.einsum('bhvk,bhk->bhv', state, at)
        state = (state * wt[:, :, None, :] + sa[..., None] * bt[:, :, None, :]
                 + vt[..., None] * kt[:, :, None, :])
        return state, jnp.einsum('bhvk,bhk->bhv', state, rt)

    init = jnp.zeros((bsz, D_HEADS, D_HEAD_SIZE, D_HEAD_SIZE), f32)
    xs = tuple(jnp.moveaxis(z, 1, 0) for z in (rh, wh, kh, vh, a_vec, b_vec))
    _, y = lax.scan(step, init, xs)
    y = jnp.moveaxis(y, 0, 1)
    mu_y = jnp.mean(y, -1, keepdims=True)
    var_y = jnp.mean(jnp.square(y - mu_y), -1, keepdims=True)
    y = ((y - mu_y) * lax.rsqrt(var_y + D_GN_EPS)).reshape(bsz, seq, bw) * gn_w + gn_b
    bonus = jnp.sum(rh * kh * r_k.reshape(D_HEADS, D_HEAD_SIZE), -1, keepdims=True) * vh
    y = (y + bonus.reshape(bsz, seq, bw)) * g
    return y.astype(out_dtype)


def _moe(x, wr_g, br_g, wr_e, br_e, w_gate, w_up, w_down):
    f32 = jnp.float32

    def one_seq(xs):
        lg = (xs @ wr_g).astype(f32) + br_g
        pg = jax.nn.softmax(lg, axis=-1)
        ptop, gsel = lax.top_k(pg, 1)
        le = ((xs @ wr_e).astype(f32) + br_e).reshape(-1, MOE_GROUPS, MOE_PER_GROUP)
        le = jnp.take_along_axis(le, gsel[:, :, None], axis=1)[:, 0]
        lv, esel = lax.top_k(le, MOE_TOPK)
        we = jax.nn.softmax(lv, axis=-1) * ptop
        local = jnp.einsum('sk,ske->se', we, jax.nn.one_hot(esel, MOE_PER_GROUP, dtype=f32))
        gate = (jax.nn.one_hot(gsel[:, 0], MOE_GROUPS, dtype=f32)[:, :, None]
                * local[:, None, :]).reshape(-1, MOE_EXPERTS)
        hg = jnp.einsum('sd,edh->seh', xs, w_gate)
        hu = jnp.einsum('sd,edh->seh', xs, w_up)
        h = jax.nn.silu(hg) * hu * gate[..., None].astype(xs.dtype)
        return jnp.einsum('seh,ehd->sd', h, w_down)

    return lax.map(one_seq, x).astype(x.dtype)


def setup_inputs(seed: int = 0) -> dict:
    key = jax.random.key(seed)
    ks = jax.random.split(key, 32)
    f32 = jnp.float32

    def nrm(k, shape, scale):
        return jax.random.normal(k, shape, f32) * scale

    L = DEPTH
    bw = BRANCH_WIDTH
    return {
        "x": nrm(ks[0], (BATCH, SEQ, D_MODEL), 1.0),
        "rpb_table": nrm(ks[1], (RPB_BUCKETS, RPB_HEADS), 0.5),
        "w_in": nrm(ks[2], (L, D_MODEL, IN_WIDTH), D_MODEL ** -0.5),
        "b_kv_gain": 1.0 + nrm(ks[3], (L, B_LATENT), 0.05),
        "b_w_uv": nrm(ks[4], (L, B_HEADS, B_LATENT, HEAD_DIM), B_LATENT ** -0.5),
        "c_a_up": nrm(ks[5], (L, C_GATE_RANK, C_HEADS * C_KEY_DIM), C_GATE_RANK ** -0.5),
        "c_a_bias": nrm(ks[6], (L, C_HEADS * C_KEY_DIM), 0.5),
        "c_norm_gain": 1.0 + nrm(ks[7], (L, bw), 0.05),
        "d_mu": jax.random.uniform(ks[8], (L, D_IN_WIDTH), f32),
        "d_w0": nrm(ks[9], (L, bw), 0.5),
        "d_w2": nrm(ks[10], (L, D_DECAY_RANK, bw), 0.5 * D_DECAY_RANK ** -0.5),
        "d_a0": nrm(ks[11], (L, bw), 0.1),
        "d_a2": nrm(ks[12], (L, D_ICLR_RANK, bw), 0.5 * D_ICLR_RANK ** -0.5),
        "d_g2": nrm(ks[13], (L, D_GATE_RANK, bw), D_GATE_RANK ** -0.5),
        "d_k_k": 0.85 + nrm(ks[14], (L, bw), 0.05),
        "d_k_a": 1.0 + nrm(ks[15], (L, bw), 0.05),
        "d_r_k": nrm(ks[16], (L, bw), 0.1),
        "d_gn_w": 1.0 + nrm(ks[17], (L, bw), 0.05),
        "d_gn_b": nrm(ks[18], (L, bw), 0.01),
        "w_branch": nrm(ks[19], (L, N_BRANCHES, bw, D_MODEL), bw ** -0.5),
        "w_out": nrm(ks[20], (L, D_MODEL, D_MODEL), D_MODEL ** -0.5 * DN_BETA),
        "ln_g": 1.0 + nrm(ks[21], (L, 2, D_MODEL), 0.05),
        "ln_b": nrm(ks[22], (L, 2, D_MODEL), 0.01),
        "router_g": nrm(ks[23], (L, D_MODEL, MOE_GROUPS), D_MODEL ** -0.5),
        "router_g_bias": nrm(ks[24], (L, MOE_GROUPS), 0.01),
        "router_e": nrm(ks[25], (L, D_MODEL, MOE_EXPERTS), D_MODEL ** -0.5),
        "router_e_bias": nrm(ks[26], (L, MOE_EXPERTS), 0.01),
        "moe_w_gate": nrm(ks[27], (L, MOE_EXPERTS, D_MODEL, MOE_HIDDEN), D_MODEL ** -0.5),
        "moe_w_up": nrm(ks[28], (L, MOE_EXPERTS, D_MODEL, MOE_HIDDEN), D_MODEL ** -0.5),
        "moe_w_down": nrm(ks[29], (L, MOE_EXPERTS, MOE_HIDDEN, D_MODEL), MOE_HIDDEN ** -0.5 * DN_BETA),
    }


def reference(x, rpb_table, w_in, b_kv_gain, b_w_uv, c_a_up, c_a_bias, c_norm_gain,
              d_mu, d_w0, d_w2, d_a0, d_a2, d_g2, d_k_k, d_k_a, d_r_k, d_gn_w, d_gn_b,
              w_branch, w_out, ln_g, ln_b, router_g, router_g_bias, router_e, router_e_bias,
              moe_w_gate, moe_w_up, moe_w_down):
    bsz, seq, _ = x.shape
    for l in range(DEPTH):
        p = _split_in(x @ w_in[l])
        ya = _mixer_a(p["a_q"], p["a_k"], p["a_v"], rpb_table[:, :A_HEADS])
        yb = _mixer_b(p["b_q"], p["b_ckv"], p["b_iq"], p["b_ik"], p["b_iw"],
                      b_kv_gain[l], b_w_uv[l], rpb_table[:, A_HEADS:])
        yc = _mixer_c(p["c_q"], p["c_k"], p["c_v"], p["c_a"], p["c_g"],
                      c_a_up[l], c_a_bias[l], c_norm_gain[l])
        yd = _mixer_d(p["d"], d_mu[l], d_w0[l], d_w2[l], d_a0[l], d_a2[l], d_g2[l],
                      d_k_k[l], d_k_a[l], d_r_k[l], d_gn_w[l], d_gn_b[l])
        ys = jnp.stack([ya.astype(x.dtype), yb.astype(x.dtype), yc.astype(x.dtype), yd.astype(x.dtype)], axis=2)
        gates = jax.nn.sigmoid(p["gate"].reshape(bsz, seq, N_BRANCHES, D_MODEL))
        merged = jnp.sum(gates * jnp.einsum('bsnc,ncd->bsnd', ys, w_branch[l]), axis=2)
        x = _layer_norm(DN_ALPHA * x + merged @ w_out[l], ln_g[l, 0], ln_b[l, 0])
        y = _moe(x, router_g[l], router_g_bias[l], router_e[l], router_e_bias[l],
                 moe_w_gate[l], moe_w_up[l], moe_w_down[l])
        x = _layer_norm(DN_ALPHA * x + y, ln_g[l, 1], ln_b[l, 1])
    return x
```

```python
import math
from contextlib import ExitStack
import numpy as np
import concourse.bass as bass
import concourse.mybir as mybir
from concourse.bass_utils import run_bass_kernel_spmd

F32 = mybir.dt.float32
BF16 = mybir.dt.bfloat16
AF = mybir.ActivationFunctionType
ALU = mybir.AluOpType
AX = mybir.AxisListType

NCORES = 8
D = 1024
SEQ = 2048
DEPTH = 4
NT = SEQ // 128
INW = 7096
BW = 256
DN_ALPHA = (2 * DEPTH) ** 0.25
LN_EPS = 1e-5
O_AQ, O_AK, O_AV, O_BQ, O_CKV, O_IQ, O_IK, O_IW = 0, 256, 512, 768, 1024, 1088, 1344, 1376
O_CQ, O_CK, O_CV, O_CA, O_CG, O_D, O_GATE = 1384, 1512, 1640, 1896, 1912, 2168, 3000
import os
CCUT = int(os.environ.get('CCUT', '0'))
NDS = 24
NE = 2048 + 128
WSHAPES = {
    "rpb_table": [32, 8], "b_kv_gain": [4, 64], "b_w_uv": [4, 4, 64, 64], "c_a_up": [4, 16, 128], "c_a_bias": [4, 128],
    "c_norm_gain": [4, 256], "d_mu": [4, 832], "d_w0": [4, 256], "d_w2": [4, 16, 256], "d_a0": [4, 256], "d_a2": [4, 16, 256],
    "d_g2": [4, 32, 256], "d_k_k": [4, 256], "d_k_a": [4, 256], "d_r_k": [4, 256], "d_gn_w": [4, 256], "d_gn_b": [4, 256],
    "w_branch": [4, 4, 256, 1024], "w_out": [4, 1024, 1024], "ln_g": [4, 2, 1024], "ln_b": [4, 2, 1024],
    "router_g": [4, 1024, 4], "router_g_bias": [4, 4], "router_e": [4, 1024, 32], "router_e_bias": [4, 32],
    "moe_w_gate": [4, 32, 1024, 256], "moe_w_up": [4, 32, 1024, 256], "moe_w_down": [4, 32, 256, 1024],
}


class T:
    __slots__ = ("lw", "rd")

    def __init__(self):
        self.lw = None
        self.rd = {}


class K:
    ENG = ("pe", "act", "dve", "pool", "sp")

    def __init__(self, nc, es):
        self.nc = nc
        self.prog = {e: [] for e in self.ENG}
        self.sem = {}
        self.cnt = {}
        for e in self.ENG:
            self.sem[e] = es.enter_context(nc.semaphore("s_" + e))
            self.cnt[e] = 0
        self.known = {e: {} for e in self.ENG}
        self.dq = {}
        self.dqi = {}
        for q in ("sp", "pool", "act"):
            self.dq[q] = []
            self.dqi[q] = 0
            for i in range(NDS):
                key = "d_%s%d" % (q, i)
                self.sem[key] = es.enter_context(nc.semaphore(key))
                self.cnt[key] = 0
                self.dq[q].append(key)

    def _deps(self, reads, writes):
        d = {}
        for r in reads:
            if r.lw is not None:
                k, v = r.lw
                if d.get(k, 0) < v:
                    d[k] = v
        for w in writes:
            if w.lw is not None:
                k, v = w.lw
                if d.get(k, 0) < v:
                    d[k] = v
            for k, v in w.rd.items():
                if d.get(k, 0) < v:
                    d[k] = v
        return d

    def _wait(self, e, d):
        kn = self.known[e]
        for k, v in d.items():
            if k == e and e == "pe":
                continue
            if kn.get(k, 0) < v:
                self.prog[e].append(("w", k, v))
                kn[k] = v

    def op(self, e, fn, reads=(), writes=()):
        self._wait(e, self._deps(reads, writes))
        self.cnt[e] += 1
        c = self.cnt[e]
        self.prog[e].append(("o", fn, e, 1))
        for w in writes:
            w.lw = (e, c)
            w.rd = {}
        for r in reads:
            if r not in writes:
                r.rd[e] = c

    def dma(self, q, out_ap, in_ap, reads=(), writes=(), **kw):
        i = self.dqi[q]
        self.dqi[q] = (i + 1) % NDS
        key = self.dq[q][i]
        d = self._deps(reads, writes)
        if self.cnt[key] > 0 and d.get(key, 0) < self.cnt[key]:
            d[key] = self.cnt[key]
        self._wait(q, d)
        self.cnt[key] += 16
        c = self.cnt[key]
        self.prog[q].append(("o", lambda eng: eng.dma_start(out=out_ap, in_=in_ap, **kw), key, 16))
        for w in writes:
            w.lw = (key, c)
            w.rd = {}
        for r in reads:
            r.rd[key] = c

    def barrier(self):
        d = {k: v for k, v in self.cnt.items() if v > 0}
        for e in self.ENG:
            self._wait(e, d)

    def replay(self):
        nc = self.nc
        with nc.Block() as block:
            def mk(e):
                prog = self.prog[e]
                sem = self.sem

                def body(eng):
                    for it in prog:
                        if it[0] == "w":
                            eng.wait_ge(sem[it[1]], it[2])
                        else:
                            it[1](eng).then_inc(sem[it[2]], it[3])
                return body
            block.tensor(mk("pe"))
            block.scalar(mk("act"))
            block.vector(mk("dve"))
            block.gpsimd(mk("pool"))
            block.sync(mk("sp"))


class Arena:
    def __init__(self, ap, words):
        self.ap = ap
        self.words = words
        self.off = 0
        self.base = 0

    def alloc(self, shape):
        n = int(np.prod(shape))
        assert self.off + n <= self.words, ("arena overflow", self.off, n, self.words)
        v = self.ap[:, self.off:self.off + n]
        self.off += n
        if len(shape) == 2:
            v = v.rearrange("p (a b) -> p a b", b=shape[1])
        elif len(shape) == 3:
            v = v.rearrange("p (a b c) -> p a b c", b=shape[1], c=shape[2])
        return v

    def alloc_bf(self, shape):
        n = int(np.prod(shape))
        assert n % 2 == 0 and self.off + n // 2 <= self.words, ("arena overflow", self.off, n, self.words)
        v = self.ap[:, self.off:self.off + n // 2].bitcast(BF16)
        self.off += n // 2
        if len(shape) == 2:
            v = v.rearrange("p (a b) -> p a b", b=shape[1])
        elif len(shape) == 3:
            v = v.rearrange("p (a b c) -> p a b c", b=shape[1], c=shape[2])
        return v

    def mark(self):
        self.base = self.off

    def reset(self):
        self.off = self.base


def host_consts():
    c = {}
    c["ident"] = np.eye(128, dtype=np.float32)
    s = np.arange(128)
    c["tri_incl"] = (s[:, None] <= s[None, :]).astype(np.float32)
    c["tri_strict"] = (s[:, None] < s[None, :]).astype(np.float32)
    c["ones"] = np.ones((128, 128), np.float32)
    c["tril_strict"] = (s[:, None] > s[None, :]).astype(np.float32)
    c["m05"] = np.full((128, 1), -0.5, np.float32)
    c["caus_neg"] = np.where(s[None, :] > s[:, None], -1e30, 0.0).astype(np.float32)
    hc = np.arange(128) // 32
    hv = np.arange(256) // 64
    c["bm_c"] = (hc[:, None] == hv[None, :]).astype(np.float32)
    c.update(rpb_consts())
    return c


class Ctx:
    pass


def build(nlayers=DEPTH, nseq=4, stages=("P",), dbg=(), extra=None):
    nc = bass.Bass("TRN2", target_bir_lowering=False)
    g = Ctx()
    g.nc = nc
    NTOK = nseq * SEQ

    def din(name, shape):
        return nc.dram_tensor(name, list(shape), F32, kind="ExternalInput").ap()

    def dscr(name, shape, out=False):
        return nc.dram_tensor(name, list(shape), F32, kind="ExternalOutput" if out else "Internal").ap()

    g.x_in = din("x", [NTOK, D])
    g.w_in = din("w_in", [DEPTH, D, INW])
    g.w = {n: din(n, shp) for n, shp in WSHAPES.items()}
    consts = host_consts()
    if extra:
        consts.update(extra)
    g.c = {n: din("c_" + n, a.shape) for n, a in consts.items()}
    g.out = dscr("out", [NTOK, D], out=True)
    g.P = dscr("P", [SEQ, INW], out=("P" in dbg))
    g.PT = dscr("PT", [2048, SEQ], out=("PT" in dbg))
    g.Y = dscr("Y", [SEQ, D], out=("Y" in dbg))
    g.X1 = dscr("X1", [SEQ, D], out=("X1" in dbg))
    g.t_P, g.t_PT, g.t_Y, g.t_X1, g.t_G, g.t_out = T(), T(), T(), T(), T(), T()
    g.G = dscr("G", [8, 128, GP])

    with ExitStack() as es:
        k = K(nc, es)
        g.k = k
        AW = 46 * 1024
        arena_t = es.enter_context(nc.sbuf_tensor("arena", [128, AW], F32))
        g.ar = Arena(arena_t[:, :], AW)
        g.banks = [es.enter_context(nc.psum_tensor("bank%d" % i, [128, 512], F32))[:, :] for i in range(8)]
        g.t_bank = [T() for _ in range(8)]
        ar = g.ar
        g.ident = ar.alloc([128])
        g.t_ident = T()
        k.dma("sp", g.ident, g.c["ident"], writes=[g.t_ident])
        g.cm05 = ar.alloc([1])
        k.dma("sp", g.cm05, g.c["m05"], writes=[g.t_ident])
        ar.mark()

        if "A" in stages or "B" in stages:
            stage_setup(g)
        for l in range(nlayers):
            for s in range(nseq):
                xsrc = g.x_in if l == 0 else g.out
                if "P" in stages:
                    stage_proj(g, l, s, xsrc)
                if "C" in stages:
                    stage_c(g, l)
                if "A" in stages:
                    stage_a(g, l)
                if "D" in stages:
                    stage_d(g, l)
                if "B" in stages:
                    stage_b(g, l)
                if "Yref" in stages:
                    k.barrier()
                    k.dma("sp", g.Y, g.c["yref"], writes=[g.t_Y])
                if "M" in stages:
                    stage_m(g, l, s, xsrc)
                if "X1ref" in stages:
                    k.barrier()
                    k.dma("sp", g.X1, g.c["x1ref"], writes=[g.t_X1])
                if "E" in stages:
                    stage_e(g, l, s)
        k.barrier()
        k.replay()
    return nc, consts


def stage_proj(g, l, s, xsrc):
    k, ar, nc = g.k, g.ar, g.nc
    k.barrier()
    ar.reset()
    xT = ar.alloc_bf([8, SEQ])
    t_xT = [T() for _ in range(NT)]
    xin = [ar.alloc([D]) for _ in range(2)]
    t_xin = [T(), T()]
    t_bank = [T() for _ in range(8)]
    for tt in range(NT):
        b = tt % 2
        r0 = s * SEQ + tt * 128
        k.dma("sp", xin[b], xsrc[r0:r0 + 128, :], writes=[t_xin[b]])
        for hb in range(2):
            bk = 2 * b + hb
            for kc4 in range(4):
                kc = hb * 4 + kc4
                k.op("pe", lambda e, o=g.banks[bk][:, kc4 * 128:(kc4 + 1) * 128], i=xin[b][:, kc * 128:(kc + 1) * 128]:
                     e.transpose(o, i, g.ident), reads=[t_xin[b], g.t_ident], writes=[t_bank[bk]])
            eng = "dve" if hb == 0 else "act"
            o = xT[:, hb * 4:hb * 4 + 4, tt * 128:(tt + 1) * 128]
            i = g.banks[bk].rearrange("p (a b) -> p a b", b=128)
            if eng == "dve":
                k.op("dve", lambda e, o=o, i=i: e.tensor_copy(out=o, in_=i), reads=[t_bank[bk]], writes=[t_xT[tt]])
            else:
                k.op("act", lambda e, o=o, i=i: e.copy(out=o, in_=i), reads=[t_bank[bk]], writes=[t_xT[tt]])
    wtf = [ar.alloc([8, 512]) for _ in range(2)]
    t_wtf = [T(), T()]
    wt = [ar.alloc_bf([8, 512]) for _ in range(2)]
    t_wt = [T(), T()]
    ot = [ar.alloc([512]) for _ in range(4)]
    t_ot = [T() for _ in range(4)]
    t_P = [g.t_P] * NT
    t_PT = g.t_PT
    oi = 0
    bi = 0
    ncb = (INW + 511) // 512
    for cb in range(ncb):
        c0 = cb * 512
        ncol = min(512, INW - c0)
        wb = cb % 2
        k.dma("sp", wtf[wb][:, :, :ncol], g.w_in[l][:, c0:c0 + ncol].rearrange("(a p) n -> p a n", p=128),
              writes=[t_wtf[wb]])
        cp(k, "pool", wt[wb][:, :, :ncol], wtf[wb][:, :, :ncol], [t_wtf[wb]], [t_wt[wb]])
        for tt in range(NT):
            bk = 4 + (bi % 4)
            bi += 1
            for kc in range(8):
                k.op("pe", lambda e, o=g.banks[bk][:, :ncol], a=xT[:, kc, tt * 128:(tt + 1) * 128], b=wt[wb][:, kc, :ncol], kc=kc:
                     e.matmul(o, a, b, start=(kc == 0), stop=(kc == 7)), reads=[t_xT[tt], t_wt[wb]], writes=[t_bank[bk]])
            ob = oi % 4
            oi += 1
            if oi % 2 == 0:
                k.op("dve", lambda e, o=ot[ob][:, :ncol], i=g.banks[bk][:, :ncol]: e.tensor_copy(out=o, in_=i),
                     reads=[t_bank[bk]], writes=[t_ot[ob]])
            else:
                k.op("act", lambda e, o=ot[ob][:, :ncol], i=g.banks[bk][:, :ncol]: e.copy(out=o, in_=i),
                     reads=[t_bank[bk]], writes=[t_ot[ob]])
            k.dma("pool", g.P[tt * 128:(tt + 1) * 128, c0:c0 + ncol], ot[ob][:, :ncol], reads=[t_ot[ob]], writes=[t_P[tt]])
        for sub in range(4):
            r0 = c0 + sub * 128
            if not (r0 < 1408 or r0 == 1792):
                continue
            for tb in range(4):
                bk = 4 + (bi % 4)
                bi += 1
                for kc in range(8):
                    k.op("pe", lambda e, o=g.banks[bk], a=wt[wb][:, kc, sub * 128:(sub + 1) * 128], b=xT[:, kc, tb * 512:(tb + 1) * 512], kc=kc:
                         e.matmul(o, a, b, start=(kc == 0), stop=(kc == 7)),
                         reads=t_xT[tb * 4:tb * 4 + 4] + [t_wt[wb]], writes=[t_bank[bk]])
                ob = oi % 4
                oi += 1
                if oi % 2 == 0:
                    k.op("dve", lambda e, o=ot[ob], i=g.banks[bk]: e.tensor_copy(out=o, in_=i), reads=[t_bank[bk]], writes=[t_ot[ob]])
                else:
                    k.op("act", lambda e, o=ot[ob], i=g.banks[bk]: e.copy(out=o, in_=i), reads=[t_bank[bk]], writes=[t_ot[ob]])
                k.dma("pool", g.PT[r0:r0 + 128, tb * 512:(tb + 1) * 512], ot[ob], reads=[t_ot[ob]], writes=[t_PT])


def mm(k, out, lhsT, rhs, R, W, start=True, stop=True):
    k.op("pe", lambda e: e.matmul(out, lhsT, rhs, start=start, stop=stop), R, W)


def tp(g, out, in_, R, W):
    n = in_.shape[0]
    g.k.op("pe", lambda e: e.transpose(out, in_, g.ident[:n, :n]), list(R) + [g.t_ident], W)


def tt(k, eng, out, a, b, op, R, W):
    k.op(eng, lambda e: e.tensor_tensor(out=out, in0=a, in1=b, op=op), R, W)


def ts(k, eng, out, a, s1, s2, op0, op1, R, W, accum=None):
    if s2 is None:
        k.op(eng, lambda e: e.tensor_scalar(out=out, in0=a, scalar1=s1, scalar2=None, op0=op0), R, W)
    elif accum is None:
        k.op(eng, lambda e: e.tensor_scalar(out=out, in0=a, scalar1=s1, scalar2=s2, op0=op0, op1=op1), R, W)
    else:
        k.op(eng, lambda e: e.tensor_scalar(out=out, in0=a, scalar1=s1, scalar2=s2, op0=op0, op1=op1, accum_out=accum), R, W)


def stt(k, eng, out, a, sc, b, op0, op1, R, W):
    k.op(eng, lambda e: e.scalar_tensor_tensor(out=out, in0=a, scalar=sc, in1=b, op0=op0, op1=op1), R, W)


def act(k, out, in_, func, R, W, bias=None, scale=1.0, accum=None):
    kw = {}
    if bias is not None:
        kw["bias"] = bias
    if accum is not None:
        kw["accum_out"] = accum
    k.op("act", lambda e: e.activation(out=out, in_=in_, func=func, scale=scale, **kw), R, W)


def rsqrt(k, out, in_, scale, eps, R, W):
    ts(k, "dve", out, in_, scale, eps, ALU.mult, ALU.add, R, W)
    k.op("act", lambda e: e.activation(out=out, in_=out, func=AF.Sqrt), [], W)
    k.op("dve", lambda e: e.reciprocal(out=out, in_=out), [], W)


def cp(k, eng, out, in_, R, W):
    if eng == "act":
        k.op("act", lambda e: e.copy(out=out, in_=in_), R, W)
    else:
        k.op(eng, lambda e: e.tensor_copy(out=out, in_=in_), R, W)


def bcast_rows(ap1d, n):
    return bass.AP(ap1d.tensor, ap1d.offset, [[0, 128], [1, n]])


class Rot:
    def __init__(self, ar, shape, n, bf=False):
        self.b = [((ar.alloc_bf(shape) if bf else ar.alloc(shape)), T()) for _ in range(n)]
        self.i = 0

    def get(self):
        r = self.b[self.i % len(self.b)]
        self.i += 1
        return r


class Banks:
    def __init__(self, g, ids):
        self.b = [(g.banks[i], g.t_bank[i]) for i in ids]
        self.i = 0

    def get(self):
        r = self.b[self.i % len(self.b)]
        self.i += 1
        return r


def stage_c(g, l):
    k, ar = g.k, g.ar
    k.barrier()
    ar.reset()
    W = g.w
    t_c = T()
    aup = ar.alloc([128])
    abias = ar.alloc([128])
    gain = ar.alloc([256])
    bm = ar.alloc([512])
    tri = ar.alloc([128])
    ones = ar.alloc([128])
    k.dma("sp", aup[0:16, :], W["c_a_up"][l], writes=[t_c])
    k.dma("sp", abias, bcast_rows(W["c_a_bias"][l], 128), writes=[t_c])
    k.dma("sp", gain, bcast_rows(W["c_norm_gain"][l], 256), writes=[t_c])
    for p_ in range(2):
        k.dma("sp", bm[0:64, p_ * 256:(p_ + 1) * 256], g.c["bm_c"][p_ * 64:(p_ + 1) * 64, :], writes=[t_c])
    k.dma("sp", tri, g.c["tri_incl"], writes=[t_c])
    k.dma("sp", ones, g.c["ones"], writes=[t_c])
    state = ar.alloc([256])
    t_state = T()
    k.op("dve", lambda e: e.memset(state, 0.0), [], [t_state])
    pin = Rot(ar, [784], 2)
    alT = Rot(ar, [128], 2)
    sb = lambda n, w: Rot(ar, [w], n)
    r_zb, r_sp, r_cum, r_eq, r_ek, r_el, r_dec = sb(2, 128), sb(2, 128), sb(2, 128), sb(2, 128), sb(2, 128), sb(2, 128), sb(2, 4)
    r_qd, r_ki, r_kl, r_qkT, r_att, r_o, r_tmp, r_sq, r_ss, r_sg, r_y = (sb(2, 128), sb(2, 128), sb(2, 128), sb(2, 1024), sb(2, 512),
                                                                        sb(2, 256), sb(2, 256), sb(2, 256), sb(2, 4), sb(2, 256), sb(2, 256))
    pb = g.banks
    ps_z, ps_cum, ps_last, ps_lt = pb[0][:, 0:128], pb[1][:, 0:128], pb[2][:, 0:128], pb[5][:, 0:4]
    ps_tr = pb[3]
    ps_att = pb[4]
    ps_o = pb[6][:, 0:256]
    ps_upd = pb[7][:, 0:256]
    tb_ = g.t_bank
    t_z, t_cum, t_last, t_lt, t_tr, t_att, t_o, t_upd = tb_[0], tb_[1], tb_[2], tb_[5], tb_[3], tb_[4], tb_[6], tb_[7]
    for tt_ in range(NT):
        r0 = tt_ * 128
        x, t_x = pin.get()
        k.dma("sp", x, g.P[r0:r0 + 128, O_CQ:O_CQ + 784], reads=[g.t_P], writes=[t_x])
        al, t_al = alT.get()
        k.dma("sp", al[0:16, :], g.PT[O_CA:O_CA + 16, r0:r0 + 128], reads=[g.t_PT], writes=[t_al])
        q, kk, v, og = x[:, 0:128], x[:, 128:256], x[:, 256:512], x[:, 528:784]
        mm(k, ps_z, al[0:16, :], aup[0:16, :], [t_al, t_c], [t_z])
        zb, t_zb = r_zb.get()
        tt(k, "dve", zb, ps_z, abias, ALU.add, [t_c], [t_zb, t_z])
        sp_, t_sp = r_sp.get()
        act(k, sp_, zb, AF.Exp, [t_zb], [t_sp], scale=-1.0)
        act(k, sp_, sp_, AF.Ln, [], [t_sp], bias=1.0)
        if CCUT == 1:
            continue
        mm(k, ps_cum, tri, sp_, [t_c, t_sp], [t_cum])
        mm(k, ps_last, ones, sp_, [t_c, t_sp], [t_last])
        for h in range(4):
            mm(k, ps_lt[0:32, h:h + 1], sp_[:, h * 32:(h + 1) * 32], ones[:, 0:1], [t_c, t_sp], [t_lt])
        cum, t_cs = r_cum.get()
        cp(k, "dve", cum, ps_cum, [], [t_cs, t_cum])
        eq, t_eq = r_eq.get()
        act(k, eq, cum, AF.Exp, [t_cs], [t_eq], scale=-1.0 / 16)
        ek, t_ek = r_ek.get()
        act(k, ek, cum, AF.Exp, [t_cs], [t_ek], scale=1.0 / 16)
        el, t_el = r_el.get()
        tt(k, "dve", el, ps_last, cum, ALU.subtract, [t_cs], [t_el, t_last])
        act(k, el, el, AF.Exp, [], [t_el], scale=-1.0 / 16)
        dec, t_dec = r_dec.get()
        act(k, dec[0:32, :], ps_lt[0:32, :], AF.Exp, [], [t_dec, t_lt], scale=-1.0 / 16)
        if CCUT == 2:
            continue
        qd, t_qd = r_qd.get()
        stt(k, "dve", qd, q, 32 ** -0.5, eq, ALU.mult, ALU.mult, [t_x, t_eq], [t_qd])
        ki, t_ki = r_ki.get()
        tt(k, "pool", ki, kk, ek, ALU.mult, [t_x, t_ek], [t_ki])
        kl, t_kl = r_kl.get()
        tt(k, "pool", kl, kk, el, ALU.mult, [t_x, t_el], [t_kl])
        if CCUT == 5:
            continue
        for h in range(4):
            tp(g, ps_tr[0:32, h * 128:(h + 1) * 128], qd[:, h * 32:(h + 1) * 32], [t_qd], [t_tr])
        tp4 = g.banks[2]
        for h in range(4):
            tp(g, tp4[0:32, h * 128:(h + 1) * 128], ki[:, h * 32:(h + 1) * 32], [t_ki], [t_last])
        qkT, t_qkT = r_qkT.get()
        cp(k, "act", qkT[0:32, 0:512], ps_tr[0:32, :], [], [t_qkT, t_tr])
        cp(k, "act", qkT[0:32, 512:1024], tp4[0:32, :], [], [t_qkT, t_last])
        for h in range(4):
            mm(k, ps_att[:, h * 128:(h + 1) * 128], qkT[0:32, 512 + h * 128:512 + (h + 1) * 128],
               qkT[0:32, h * 128:(h + 1) * 128], [t_qkT], [t_att])
        att, t_at = r_att.get()
        for h in range(4):
            tt(k, "dve", att[:, h * 128:(h + 1) * 128], ps_att[:, h * 128:(h + 1) * 128], tri, ALU.mult, [t_c], [t_at, t_att])
        for h in range(4):
            mm(k, ps_o[:, h * 64:(h + 1) * 64], qkT[0:32, h * 128:(h + 1) * 128], state[0:32, h * 64:(h + 1) * 64],
               [t_qkT, t_state], [t_o], start=True, stop=False)
            mm(k, ps_o[:, h * 64:(h + 1) * 64], att[:, h * 128:(h + 1) * 128], v[:, h * 64:(h + 1) * 64], [t_at, t_x], [t_o],
               start=False, stop=True)
        for h in range(4):
            mm(k, ps_upd[0:32, h * 64:(h + 1) * 64], kl[:, h * 32:(h + 1) * 32], v[:, h * 64:(h + 1) * 64], [t_kl, t_x], [t_upd])
        tmp, t_tmp = r_tmp.get()
        cp(k, "act", tmp[0:32, :], ps_upd[0:32, :], [], [t_tmp, t_upd])
        for h in range(4):
            stt(k, "dve", state[0:32, h * 64:(h + 1) * 64], state[0:32, h * 64:(h + 1) * 64], dec[0:32, h:h + 1],
                tmp[0:32, h * 64:(h + 1) * 64], ALU.mult, ALU.add, [t_dec, t_tmp], [t_state])
        if CCUT == 4:
            continue
        o, t_os = r_o.get()
        cp(k, "act", o, ps_o, [], [t_os, t_o])
        sq, t_sq = r_sq.get()
        tt(k, "pool", sq, o, o, ALU.mult, [t_os], [t_sq])
        ss, t_ss = r_ss.get()
        k.op("dve", lambda e, o_=ss, i_=sq.rearrange("p (h v) -> p h v", v=64): e.tensor_reduce(out=o_, in_=i_, axis=AX.X, op=ALU.add),
             [t_sq], [t_ss])
        rsqrt(k, ss, ss, 1.0 / 64, 1e-6, [], [t_ss])
        sg, t_sg = r_sg.get()
        act(k, sg, og, AF.Silu, [t_x], [t_sg])
        tt(k, "pool", sg, sg, gain, ALU.mult, [t_c], [t_sg])
        y, t_y = r_y.get()
        for h in range(4):
            stt(k, "dve", y[:, h * 64:(h + 1) * 64], o[:, h * 64:(h + 1) * 64], ss[:, h:h + 1], sg[:, h * 64:(h + 1) * 64],
                ALU.mult, ALU.mult, [t_os, t_ss, t_sg], [t_y])
        k.dma("pool", g.Y[r0:r0 + 128, 512:768], y, reads=[t_y], writes=[g.t_Y])


def make_inputs(inputs, consts, core, nseq=4, small=True):
    m = {}
    m["x"] = np.ascontiguousarray(inputs["x"][core * nseq:(core + 1) * nseq]).reshape(nseq * SEQ, D)
    m["w_in"] = np.ascontiguousarray(inputs["w_in"])
    for n in WSHAPES:
        m[n] = np.ascontiguousarray(inputs[n])
    for n, a in consts.items():
        m["c_" + n] = a
    return m


NEV = 2944
GP = NEV + 128


def t5_bucket_np(dist):
    d = np.maximum(dist, 0)
    df = np.maximum(d, 1).astype(np.float32)
    large = 16 + (np.log(df / np.float32(16)) / np.float32(math.log(2048 / 16)) * np.float32(16)).astype(np.int32)
    large = np.minimum(large, 31)
    return np.where(d < 16, d, large)


def rpb_consts():
    i = np.arange(NEV)
    dist = i - 511
    valid = (dist >= 0) & (dist <= 2047)
    b = t5_bucket_np(dist)
    oht = np.zeros((32, NEV), np.float32)
    oht[b[valid], i[valid]] = 1.0
    cA = ((dist <= 128).astype(np.float32) + ((dist % 4 == 0) & (dist <= 512)) + ((dist % 16 == 0) & (dist <= 2048))) * valid
    cB = valid.astype(np.float32)
    return {"oht": oht, "cmul": np.stack([cA, cB]).astype(np.float32)}


def stage_setup(g):
    k, ar = g.k, g.ar
    k.barrier()
    ar.reset()
    t_c = T()
    oht = ar.alloc([NEV])
    cm = [ar.alloc([NEV]), ar.alloc([NEV])]
    tab = ar.alloc([8])
    ones = ar.alloc([128])
    k.dma("sp", oht[0:32, :], g.c["oht"], writes=[t_c])
    for a in range(2):
        k.dma("sp", cm[a], bcast_rows(g.c["cmul"][a], NEV), writes=[t_c])
    k.dma("sp", tab[0:32, :], g.w["rpb_table"], writes=[t_c])
    k.dma("sp", ones, g.c["ones"], writes=[t_c])
    tabb = Rot(ar, [128], 2)
    eb = Rot(ar, [NEV], 2)
    bi = 0
    for hh in range(8):
        tb, t_tb = tabb.get()
        ts(k, "dve", tb[0:32, :], ones[0:32, :], tab[0:32, hh:hh + 1], None, ALU.mult, None, [t_c], [t_tb])
        e_, t_e = eb.get()
        for c0 in range(0, NEV, 512):
            n = min(512, NEV - c0)
            bk = bi % 4
            bi += 1
            mm(k, g.banks[bk][:, :n], tb[0:32, :], oht[0:32, c0:c0 + n], [t_tb, t_c], [g.t_bank[bk]])
            act(k, e_[:, c0:c0 + n], g.banks[bk][:, :n], AF.Exp, [], [t_e, g.t_bank[bk]])
        tt(k, "dve", e_, e_, cm[0 if hh < 4 else 1], ALU.mult, [t_c], [t_e])
        gh = g.G[hh]
        dst = bass.AP(gh.tensor, gh.offset, [[GP + 1, 128], [1, NEV]])
        k.dma("pool", dst, e_, reads=[t_e], writes=[g.t_G])


def attn_block(g, I, kT, t_kT, qT, t_q, strips, t_st, vaug, t_v, vw, ysb, t_y, ycol, st, mask=None, t_mask=None):
    k = g.k
    for j in range(4 * I + 4):
        bk = st["sb"] % 2
        st["sb"] += 1
        ps = g.banks[bk]
        mm(k, ps, kT[0:64, j * 128:(j + 1) * 128], qT, [t_kT, t_q], [g.t_bank[bk]])
        pt, t_pt = st["pt"].get()
        act(k, pt, ps, AF.Exp, [], [t_pt, g.t_bank[bk]], scale=0.125)
        o = 4 * I - j
        eng = "dve" if st["sb"] % 2 == 0 else "pool"
        tt(k, eng, pt, pt, strips[:, (o + 3) * 128:(o + 3) * 128 + 512], ALU.mult, [t_st], [t_pt])
        if mask is not None:
            tt(k, "pool" if eng == "dve" else "dve", pt, pt, mask[:, j, :], ALU.mult, [t_mask], [t_pt])
        for qi in range(4):
            i = 4 * I + qi
            if j > i:
                continue
            ob = 4 + qi
            mm(k, g.banks[ob][:, 0:vw + 1], pt[:, qi * 128:(qi + 1) * 128], vaug(j), [t_pt, t_v], [g.t_bank[ob]],
               start=(j == 0), stop=(j == i))
            if j == i:
                rd, t_rd = st["rd"].get()
                k.op("dve", lambda e, o_=rd, i_=g.banks[ob][:, vw:vw + 1]: e.reciprocal(out=o_, in_=i_), [], [t_rd, g.t_bank[ob]])
                ts(k, "dve", ysb[:, i, ycol:ycol + vw], g.banks[ob][:, 0:vw], rd[:, 0:1], None, ALU.mult, None,
                   [t_rd], [t_y, g.t_bank[ob]])


def stage_a(g, l):
    k, ar = g.k, g.ar
    k.barrier()
    ar.reset()
    ysb = ar.alloc([NT, 256])
    t_y = T()
    st = {"sb": 0, "pt": Rot(ar, [512], 3), "rd": Rot(ar, [1], 4)}
    qTs, kTs, sts, vas = Rot(ar, [SEQ], 2), Rot(ar, [SEQ], 2), Rot(ar, [2816], 2), Rot(ar, [NT, 65], 2)
    for h in range(4):
        qT, t_q = qTs.get()
        kT, t_kT = kTs.get()
        strips, t_st = sts.get()
        va, t_v = vas.get()
        k.dma("sp", qT[0:64, :], g.PT[O_AQ + h * 64:O_AQ + (h + 1) * 64, :], reads=[g.t_PT], writes=[t_q])
        k.dma("sp", kT[0:64, :], g.PT[O_AK + h * 64:O_AK + (h + 1) * 64, :], reads=[g.t_PT], writes=[t_kT])
        k.dma("sp", strips, g.G[h][:, 127:127 + 2816], reads=[g.t_G], writes=[t_st])
        k.dma("sp", va[:, :, 0:64], g.P[:, O_AV + h * 64:O_AV + (h + 1) * 64].rearrange("(t p) c -> p t c", p=128),
              reads=[g.t_P], writes=[t_v])
        k.op("dve", lambda e, o_=va[:, :, 64:65]: e.memset(o_, 1.0), [], [t_v])
        for I in range(4):
            attn_block(g, I, kT, t_kT, qT[0:64, I * 512:(I + 1) * 512], t_q, strips, t_st, lambda j, va=va: va[:, j, :], t_v, 64, ysb, t_y, h * 64, st)
    k.dma("pool", g.Y[:, 0:256].rearrange("(t p) c -> p t c", p=128), ysb, reads=[t_y], writes=[g.t_Y])


def ln_tile(k, r, t_r, gbc, bbc, t_c, sm, out, t_out):
    s1, t_s1 = sm.get()
    k.op("dve", lambda e: e.tensor_reduce(out=s1[:, 0:1], in_=r, axis=AX.X, op=ALU.add), [t_r], [t_s1])
    ts(k, "dve", s1[:, 0:1], s1[:, 0:1], -1.0 / D, None, ALU.mult, None, [], [t_s1])
    ts(k, "dve", r, r, s1[:, 0:1], None, ALU.add, None, [t_s1], [t_r])
    act(k, out, r, AF.Square, [t_r], [t_out, t_s1], accum=s1[:, 1:2])
    rsqrt(k, s1[:, 1:2], s1[:, 1:2], 1.0 / D, LN_EPS, [], [t_s1])
    stt(k, "dve", out, r, s1[:, 1:2], gbc, ALU.mult, ALU.mult, [t_r, t_s1, t_c], [t_out])
    tt(k, "pool", out, out, bbc, ALU.add, [t_c], [t_out])


def stage_m(g, l, s, xsrc):
    k, ar = g.k, g.ar
    k.barrier()
    ar.reset()
    W = g.w
    t_c = T()
    wb = ar.alloc_bf([8, D])
    wo = ar.alloc_bf([8, D])
    gbc = ar.alloc([D])
    bbc = ar.alloc([D])
    stg = Rot(ar, [2, D], 2)
    for c2 in range(4):
        sb_, t_sb = stg.get()
        k.dma("sp", sb_, W["w_branch"][l, c2].rearrange("(c p) d -> p c d", p=128), writes=[t_sb])
        cp(k, "pool" if c2 % 2 else "act", wb[:, 2 * c2:2 * c2 + 2, :], sb_, [t_sb], [t_c])
    for c2 in range(4):
        sb_, t_sb = stg.get()
        k.dma("sp", sb_, W["w_out"][l][c2 * 256:(c2 + 1) * 256, :].rearrange("(c p) d -> p c d", p=128), writes=[t_sb])
        cp(k, "pool" if c2 % 2 else "act", wo[:, 2 * c2:2 * c2 + 2, :], sb_, [t_sb], [t_c])
    k.dma("sp", gbc, bcast_rows(W["ln_g"][l, 0], D), writes=[t_c])
    k.dma("sp", bbc, bcast_rows(W["ln_b"][l, 0], D), writes=[t_c])
    r_y, r_gate, r_yT, r_mg, r_mT, r_x, r_tmp, r_out, sm = (Rot(ar, [D], 2), Rot(ar, [4 * D], 2), Rot(ar, [D], 2, bf=True), Rot(ar, [D], 2),
                                                          Rot(ar, [D], 2, bf=True), Rot(ar, [D], 2), Rot(ar, [512], 3), Rot(ar, [D], 2), Rot(ar, [2], 4))
    B, tB = g.banks, g.t_bank
    bi = 0
    for tt_ in range(NT):
        r0 = tt_ * 128
        y, t_y = r_y.get()
        k.dma("sp", y, g.Y[r0:r0 + 128, :], reads=[g.t_Y], writes=[t_y])
        gt, t_gt = r_gate.get()
        k.dma("sp", gt, g.P[r0:r0 + 128, O_GATE:O_GATE + 4 * D], reads=[g.t_P], writes=[t_gt])
        x, t_x = r_x.get()
        k.dma("sp", x, xsrc[s * SEQ + r0:s * SEQ + r0 + 128, :], writes=[t_x])
        act(k, gt, gt, AF.Sigmoid, [], [t_gt])
        yT, t_yT = r_yT.get()
        for hb in range(2):
            for c4 in range(4):
                c = hb * 4 + c4
                tp(g, B[hb][:, c4 * 128:(c4 + 1) * 128], y[:, c * 128:(c + 1) * 128], [t_y], [tB[hb]])
            cp(k, "act" if hb else "dve", yT[:, hb * 512:(hb + 1) * 512], B[hb], [], [t_yT, tB[hb]])
        mg, t_mg = r_mg.get()
        for n in range(4):
            for dh in range(2):
                bk = 2 + bi % 2
                bi += 1
                for cc in range(2):
                    c = 2 * n + cc
                    mm(k, B[bk], yT[:, c * 128:(c + 1) * 128], wb[:, c, dh * 512:(dh + 1) * 512], [t_yT, t_c], [tB[bk]],
                       start=(cc == 0), stop=(cc == 1))
                gsl = gt[:, n * D + dh * 512:n * D + (dh + 1) * 512]
                msl = mg[:, dh * 512:(dh + 1) * 512]
                if n == 0:
                    tt(k, "dve", msl, B[bk], gsl, ALU.mult, [t_gt], [t_mg, tB[bk]])
                else:
                    tmp, t_tmp = r_tmp.get()
                    tt(k, "dve", tmp, B[bk], gsl, ALU.mult, [t_gt], [t_tmp, tB[bk]])
                    tt(k, "pool", msl, msl, tmp, ALU.add, [t_tmp], [t_mg])
        mT, t_mT = r_mT.get()
        for hb in range(2):
            for c4 in range(4):
                c = hb * 4 + c4
                tp(g, B[4 + hb][:, c4 * 128:(c4 + 1) * 128], mg[:, c * 128:(c + 1) * 128], [t_mg], [tB[4 + hb]])
            cp(k, "act" if hb else "dve", mT[:, hb * 512:(hb + 1) * 512], B[4 + hb], [], [t_mT, tB[4 + hb]])
        for dh in range(2):
            bk = 6 + dh
            for c in range(8):
                mm(k, B[bk], mT[:, c * 128:(c + 1) * 128], wo[:, c, dh * 512:(dh + 1) * 512], [t_mT, t_c], [tB[bk]],
                   start=(c == 0), stop=(c == 7))
            stt(k, "dve", x[:, dh * 512:(dh + 1) * 512], x[:, dh * 512:(dh + 1) * 512], DN_ALPHA, B[bk], ALU.mult, ALU.add,
                [], [t_x, tB[bk]])
        o, t_o = r_out.get()
        ln_tile(k, x, t_x, gbc, bbc, t_c, sm, o, t_o)
        k.dma("pool", g.X1[r0:r0 + 128, :], o, reads=[t_o], writes=[g.t_X1])


def stage_e(g, l, s):
    k, ar = g.k, g.ar
    W = g.w
    B, tB = g.banks, g.t_bank
    for half in range(2):
        k.barrier()
        ar.reset()
        t_c = T()
        wr = ar.alloc([8, 36])
        rb = ar.alloc([36])
        gbc = ar.alloc([D])
        bbc = ar.alloc([D])
        k.dma("sp", wr[:, :, 0:4], W["router_g"][l].rearrange("(c p) e -> p c e", p=128), writes=[t_c])
        k.dma("sp", wr[:, :, 4:36], W["router_e"][l].rearrange("(c p) e -> p c e", p=128), writes=[t_c])
        k.dma("sp", rb[:, 0:4], bcast_rows(W["router_g_bias"][l], 4), writes=[t_c])
        k.dma("sp", rb[:, 4:36], bcast_rows(W["router_e_bias"][l], 32), writes=[t_c])
        k.dma("sp", gbc, bcast_rows(W["ln_g"][l, 1], D), writes=[t_c])
        k.dma("sp", bbc, bcast_rows(W["ln_b"][l, 1], D), writes=[t_c])
        r_x1 = Rot(ar, [D], 2)
        xT = ar.alloc_bf([8, 1024])
        t_xT = [T() for _ in range(8)]
        r_xf = Rot(ar, [8, 128], 2)
        yacc = ar.alloc([8, D])
        t_ya = [T() for _ in range(8)]
        gates = ar.alloc([8, 32])
        t_g = [T() for _ in range(8)]
        sm = Rot(ar, [40], 4)
        sm2 = Rot(ar, [8], 4)
        sm3 = Rot(ar, [32], 6)
        for ti in range(8):
            r0 = half * 1024 + ti * 128
            x1t, t_x1t = r_x1.get()
            k.dma("sp", x1t, g.X1[r0:r0 + 128, :], reads=[g.t_X1], writes=[t_x1t])
            for hb in range(2):
                for c4 in range(4):
                    c = hb * 4 + c4
                    tp(g, B[hb][:, c4 * 128:(c4 + 1) * 128], x1t[:, c * 128:(c + 1) * 128], [t_x1t], [tB[hb]])
                if hb == 0:
                    xf, t_xf = r_xf.get()
                cp(k, "act" if hb else "dve", xf[:, hb * 4:hb * 4 + 4, :], B[hb].rearrange("p (a b) -> p a b", b=128), [], [t_xf, tB[hb]])
            cp(k, "pool", xT[:, :, ti * 128:(ti + 1) * 128], xf, [t_xf], [t_xT[ti]])
            for c in range(8):
                mm(k, B[2][:, 0:36], xf[:, c, :], wr[:, c, :], [t_xf, t_c], [tB[2]], start=(c == 0), stop=(c == 7))
            lg, t_lg = sm.get()
            tt(k, "dve", lg[:, 0:36], B[2][:, 0:36], rb, ALU.add, [t_c], [t_lg, tB[2]])
            a, t_a = sm2.get()
            k.op("dve", lambda e, o_=a[:, 0:1], i_=lg[:, 0:4]: e.tensor_reduce(out=o_, in_=i_, axis=AX.X, op=ALU.max), [t_lg], [t_a])
            ts(k, "dve", a[:, 1:2], a[:, 0:1], -1.0, None, ALU.mult, None, [], [t_a])
            e4, t_e4 = sm3.get()
            act(k, e4[:, 0:4], lg[:, 0:4], AF.Exp, [t_lg, t_a], [t_e4, t_a], bias=a[:, 1:2], accum=a[:, 2:3])
            ohg, t_ohg = sm3.get()
            ts(k, "dve", ohg[:, 0:4], lg[:, 0:4], a[:, 0:1], None, ALU.is_equal, None, [t_lg, t_a], [t_ohg])
            ts(k, "dve", ohg[:, 0:4], ohg[:, 0:4], -1.0, 1e30, ALU.add, ALU.mult, [], [t_ohg])
            lem, t_lem = sm3.get()
            for gi in range(4):
                ts(k, "dve", lem[:, gi * 8:(gi + 1) * 8], lg[:, 4 + gi * 8:4 + (gi + 1) * 8], ohg[:, gi:gi + 1], None, ALU.add, None,
                   [t_lg, t_ohg], [t_lem])
            k.op("dve", lambda e, o_=a[:, 3:4], i_=lem: e.tensor_reduce(out=o_, in_=i_, axis=AX.X, op=ALU.max), [t_lem], [t_a])
            oh1, t_oh1 = sm3.get()
            ts(k, "dve", oh1, lem, a[:, 3:4], None, ALU.is_equal, None, [t_lem, t_a], [t_oh1])
            stt(k, "dve", lem, oh1, -1e30, lem, ALU.mult, ALU.add, [t_oh1], [t_lem])
            k.op("dve", lambda e, o_=a[:, 4:5], i_=lem: e.tensor_reduce(out=o_, in_=i_, axis=AX.X, op=ALU.max), [t_lem], [t_a])
            oh2, t_oh2 = sm3.get()
            ts(k, "dve", oh2, lem, a[:, 4:5], None, ALU.is_equal, None, [t_lem, t_a], [t_oh2])
            tt(k, "dve", a[:, 5:6], a[:, 4:5], a[:, 3:4], ALU.subtract, [], [t_a])
            act(k, a[:, 5:6], a[:, 5:6], AF.Exp, [], [t_a])
            ts(k, "dve", a[:, 6:7], a[:, 5:6], 1.0, None, ALU.add, None, [], [t_a])
            tt(k, "dve", a[:, 6:7], a[:, 6:7], a[:, 2:3], ALU.mult, [], [t_a])
            k.op("dve", lambda e, o_=a[:, 6:7]: e.reciprocal(out=o_, in_=o_), [], [t_a])
            tt(k, "dve", a[:, 7:8], a[:, 6:7], a[:, 5:6], ALU.mult, [], [t_a])
            ts(k, "dve", gates[:, ti, :], oh1, a[:, 6:7], None, ALU.mult, None, [t_oh1, t_a], [t_g[ti]])
            stt(k, "dve", gates[:, ti, :], oh2, a[:, 7:8], gates[:, ti, :], ALU.mult, ALU.add, [t_oh2, t_a], [t_g[ti]])
        r_wg, r_wu, r_wd = Rot(ar, [8, 256], 2, bf=True), Rot(ar, [8, 256], 2, bf=True), Rot(ar, [2, D], 2, bf=True)
        f_wg, f_wu, f_wd = Rot(ar, [8, 256], 2), Rot(ar, [8, 256], 2), Rot(ar, [2, D], 2)
        r_sg, r_h = Rot(ar, [512], 2), Rot(ar, [2, 512], 2, bf=True)
        bi = 0
        for ex in range(32):
            wg, t_wg = r_wg.get()
            wu, t_wu = r_wu.get()
            wd, t_wd = r_wd.get()
            fg, t_fg = f_wg.get()
            fu, t_fu = f_wu.get()
            fd, t_fd = f_wd.get()
            k.dma("sp", fg, W["moe_w_gate"][l, ex].rearrange("(c p) h -> p c h", p=128), writes=[t_fg])
            k.dma("sp", fu, W["moe_w_up"][l, ex].rearrange("(c p) h -> p c h", p=128), writes=[t_fu])
            k.dma("sp", fd, W["moe_w_down"][l, ex].rearrange("(c p) d -> p c d", p=128), writes=[t_fd])
            cp(k, "pool", wg, fg, [t_fg], [t_wg])
            cp(k, "pool", wu, fu, [t_fu], [t_wu])
            cp(k, "act", wd, fd, [t_fd], [t_wd])
            for tb in range(2):
                hT, t_h = r_h.get()
                for hc in range(2):
                    for c in range(8):
                        mm(k, B[0], wg[:, c, hc * 128:(hc + 1) * 128], xT[:, c, tb * 512:(tb + 1) * 512], [t_wg] + t_xT[tb * 4:tb * 4 + 4], [tB[0]],
                           start=(c == 0), stop=(c == 7))
                    for c in range(8):
                        mm(k, B[1], wu[:, c, hc * 128:(hc + 1) * 128], xT[:, c, tb * 512:(tb + 1) * 512], [t_wu] + t_xT[tb * 4:tb * 4 + 4], [tB[1]],
                           start=(c == 0), stop=(c == 7))
                    sg, t_sg = r_sg.get()
                    act(k, sg, B[0], AF.Silu, [], [t_sg, tB[0]])
                    tt(k, "dve", hT[:, hc, :], B[1], sg, ALU.mult, [t_sg], [t_h, tB[1]])
                for q4 in range(4):
                    ti = tb * 4 + q4
                    for dh in range(2):
                        bk = 2 + bi % 6
                        bi += 1
                        for hc in range(2):
                            mm(k, B[bk], hT[:, hc, q4 * 128:(q4 + 1) * 128], wd[:, hc, dh * 512:(dh + 1) * 512], [t_h, t_wd], [tB[bk]],
                               start=(hc == 0), stop=(hc == 1))
                        ysl = yacc[:, ti, dh * 512:(dh + 1) * 512]
                        if ex == 0:
                            ts(k, "dve", ysl, B[bk], gates[:, ti, ex:ex + 1], None, ALU.mult, None, [t_g[ti]], [t_ya[ti], tB[bk]])
                        else:
                            stt(k, "dve", ysl, B[bk], gates[:, ti, ex:ex + 1], ysl, ALU.mult, ALU.add, [t_g[ti]], [t_ya[ti], tB[bk]])
        r_out = Rot(ar, [D], 2)
        for ti in range(8):
            r0 = s * SEQ + half * 1024 + ti * 128
            x1t, t_x1t = r_x1.get()
            k.dma("sp", x1t, g.X1[half * 1024 + ti * 128:half * 1024 + (ti + 1) * 128, :], reads=[g.t_X1], writes=[t_x1t])
            stt(k, "dve", yacc[:, ti, :], x1t, DN_ALPHA, yacc[:, ti, :], ALU.mult, ALU.add, [t_x1t], [t_ya[ti]])
            o, t_o = r_out.get()
            ln_tile(k, yacc[:, ti, :], t_ya[ti], gbc, bbc, t_c, sm2, o, t_o)
            k.dma("pool", g.out[r0:r0 + 128, :], o, reads=[t_o], writes=[g.t_out])


def bc_last(ap, n):
    return bass.AP(ap.tensor, ap.offset, [list(x) for x in ap.ap] + [[0, n]])


def v3(ap, inner):
    return ap.rearrange("p (a b) -> p a b", b=inner)


def stage_d(g, l):
    k, ar = g.k, g.ar
    k.barrier()
    ar.reset()
    W = g.w
    B, tB = g.banks, g.t_bank
    st = {"b": 0}

    def nb():
        st["b"] = (st["b"] + 1) % 8
        return st["b"]
    t_c = T()
    mu = ar.alloc([832])
    wcat = ar.alloc([768])
    vec = {n: ar.alloc([256]) for n in ("d_w0", "d_a0", "d_k_k", "d_k_a", "d_r_k", "d_gn_w", "d_gn_b")}
    tri_i, tri_s, tril_s, ones, idn = ar.alloc([128]), ar.alloc([128]), ar.alloc([128]), ar.alloc([128]), g.ident
    k.dma("sp", mu, bcast_rows(W["d_mu"][l], 832), writes=[t_c])
    k.op("dve", lambda e: e.memset(wcat, 0.0), [], [t_c])
    k.dma("sp", wcat[0:16, 0:256], W["d_w2"][l], writes=[t_c])
    k.dma("sp", wcat[16:32, 256:512], W["d_a2"][l], writes=[t_c])
    k.dma("sp", wcat[32:64, 512:768], W["d_g2"][l], writes=[t_c])
    for n, a in vec.items():
        k.dma("sp", a, bcast_rows(W[n][l], 256), writes=[t_c])
    k.dma("sp", tri_i, g.c["tri_incl"], writes=[t_c])
    k.dma("sp", tri_s, g.c["tri_strict"], writes=[t_c])
    k.dma("sp", tril_s, g.c["tril_strict"], writes=[t_c])
    k.dma("sp", ones, g.c["ones"], writes=[t_c])
    S = ar.alloc([256])
    t_S = T()
    k.op("dve", lambda e: e.memset(S, 0.0), [], [t_S])
    R2 = lambda w: Rot(ar, [w], 2)
    r_cur, r_prev, r_seg, r_z, r_zT = R2(832), R2(832), R2(832), R2(64), R2(128)
    r_nlw, r_a, r_g, r_kk, r_kp, r_sm, r_tmp = R2(256), R2(256), R2(256), R2(256), R2(256), Rot(ar, [16], 4), Rot(ar, [256], 4)
    r_en, r_ep, r_ex = R2(256), R2(256), R2(256)
    r_at, r_bt, r_kt, r_rt = R2(256), R2(256), R2(256), R2(256)
    r_FT = Rot(ar, [4, 512], 2)
    r_wT = R2(4)
    r_M = Rot(ar, [5, 512], 1)
    r_P, r_PT, r_XT = Rot(ar, [512], 2), Rot(ar, [512], 2), Rot(ar, [512], 2)
    r_rhs0, r_U, r_y, r_o = R2(256), R2(256), R2(256), R2(256)
    for tt_ in range(NT):
        r0 = tt_ * 128
        cur, t_cur = r_cur.get()
        prev, t_prev = r_prev.get()
        k.dma("sp", cur, g.P[r0:r0 + 128, O_D:O_D + 832], reads=[g.t_P], writes=[t_cur])
        if tt_ == 0:
            k.op("dve", lambda e, o_=prev[0:1, :]: e.memset(o_, 0.0), [], [t_prev])
            k.dma("sp", prev[1:128, :], g.P[0:127, O_D:O_D + 832], reads=[g.t_P], writes=[t_prev])
        else:
            k.dma("sp", prev, g.P[r0 - 1:r0 + 127, O_D:O_D + 832], reads=[g.t_P], writes=[t_prev])
        seg, t_seg = r_seg.get()
        tt(k, "pool", prev, prev, cur, ALU.subtract, [t_cur], [t_prev])
        tt(k, "pool", prev, prev, mu, ALU.mult, [t_c], [t_prev])
        tt(k, "dve", seg, cur, prev, ALU.add, [t_cur, t_prev], [t_seg])
        r, kraw, v = seg[:, 0:256], seg[:, 256:512], seg[:, 512:768]
        z, t_z = r_z.get()
        act(k, z[:, 0:16], seg[:, 768:784], AF.Tanh, [t_seg], [t_z])
        cp(k, "dve", z[:, 16:32], seg[:, 784:800], [t_seg], [t_z])
        act(k, z[:, 32:64], seg[:, 800:832], AF.Sigmoid, [t_seg], [t_z])
        b0 = nb()
        tp(g, B[b0][0:64, 0:128], z, [t_z], [tB[b0]])
        zT, t_zT = r_zT.get()
        cp(k, "act", zT[0:64, :], B[b0][0:64, 0:128], [], [t_zT, tB[b0]])
        bw, bg_ = nb(), nb()
        mm(k, B[bw], zT[0:64, :], wcat[0:64, 0:512], [t_zT, t_c], [tB[bw]])
        mm(k, B[bg_][:, 0:256], zT[0:64, :], wcat[0:64, 512:768], [t_zT, t_c], [tB[bg_]])
        nlw, t_nlw = r_nlw.get()
        tt(k, "dve", nlw, B[bw][:, 0:256], vec["d_w0"], ALU.add, [t_c], [t_nlw, tB[bw]])
        a, t_a = r_a.get()
        tt(k, "dve", a, B[bw][:, 256:512], vec["d_a0"], ALU.add, [t_c], [t_a, tB[bw]])
        gsb, t_gs = r_g.get()
        cp(k, "act", gsb, B[bg_][:, 0:256], [], [t_gs, tB[bg_]])
        act(k, nlw, nlw, AF.Exp, [], [t_nlw], scale=-1.0)
        act(k, nlw, nlw, AF.Ln, [], [t_nlw], bias=1.0)
        act(k, nlw, nlw, AF.Exp, [], [t_nlw], scale=-1.0, bias=g.cm05[:, 0:1])
        act(k, a, a, AF.Sigmoid, [], [t_a])
        kk, t_kk = r_kk.get()
        tt(k, "pool", kk, kraw, vec["d_k_k"], ALU.mult, [t_seg, t_c], [t_kk])
        tmp, t_tmp = r_tmp.get()
        tt(k, "pool", tmp, kk, kk, ALU.mult, [t_kk], [t_tmp])
        sm, t_sm = r_sm.get()
        k.op("dve", lambda e, o_=sm[:, 0:4], i_=v3(tmp, 64): e.tensor_reduce(out=o_, in_=i_, axis=AX.X, op=ALU.add), [t_tmp], [t_sm])
        k.op("act", lambda e, o_=sm[:, 0:4]: e.activation(out=o_, in_=o_, func=AF.Sqrt), [], [t_sm])
        ts(k, "dve", sm[:, 0:4], sm[:, 0:4], 1e-12, None, ALU.max, None, [], [t_sm])
        k.op("dve", lambda e, o_=sm[:, 0:4]: e.reciprocal(out=o_, in_=o_), [], [t_sm])
        tt(k, "dve", v3(kk, 64), v3(kk, 64), bc_last(sm[:, 0:4], 64), ALU.mult, [t_sm], [t_kk])
        kp, t_kp = r_kp.get()
        stt(k, "dve", kp, a, -1.0, vec["d_k_a"], ALU.add, ALU.mult, [t_a, t_c], [t_kp])
        stt(k, "dve", kp, kp, 1.0, kraw, ALU.add, ALU.mult, [t_seg], [t_kp])
        bc = nb()
        mm(k, B[bc][:, 0:256], tri_i, nlw, [t_c, t_nlw], [tB[bc]])
        for h in range(4):
            mm(k, B[bc][0:64, 256 + h:257 + h], nlw[:, h * 64:(h + 1) * 64], ones[:, 0:1], [t_nlw, t_c], [tB[bc]])
        en, t_en = r_en.get()
        ep, t_ep = r_ep.get()
        ex, t_ex = r_ex.get()
        wT, t_wT = r_wT.get()
        act(k, en, B[bc][:, 0:256], AF.Exp, [], [t_en, tB[bc]], scale=-1.0)
        act(k, ep, B[bc][:, 0:256], AF.Exp, [], [t_ep, tB[bc]])
        tt(k, "dve", ex, B[bc][:, 0:256], nlw, ALU.subtract, [t_nlw], [t_ex, tB[bc]])
        act(k, wT[0:64, :], B[bc][0:64, 256:260], AF.Exp, [], [t_wT, tB[bc]], scale=-1.0)
        act(k, ex, ex, AF.Exp, [], [t_ex], scale=-1.0)
        at, t_at = r_at.get()
        bt, t_bt = r_bt.get()
        kt, t_kt = r_kt.get()
        rt, t_rt = r_rt.get()
        stt(k, "dve", at, kk, -1.0, ex, ALU.mult, ALU.mult, [t_kk, t_ex], [t_at])
        tt(k, "pool", bt, kk, a, ALU.mult, [t_kk, t_a], [t_bt])
        tt(k, "pool", bt, bt, ep, ALU.mult, [t_ep], [t_bt])
        tt(k, "dve", kt, kp, ep, ALU.mult, [t_kp, t_ep], [t_kt])
        tt(k, "pool", rt, r, en, ALU.mult, [t_seg, t_en], [t_rt])
        FT, t_FT = r_FT.get()
        for qi, (src, t_src) in enumerate(((at, t_at), (bt, t_bt), (kt, t_kt), (rt, t_rt))):
            bq = nb()
            for h in range(4):
                tp(g, B[bq][0:64, h * 128:(h + 1) * 128], src[:, h * 64:(h + 1) * 64], [t_src], [tB[bq]])
            cp(k, "act" if qi % 2 else "dve", FT[0:64, qi, :], B[bq][0:64, :], [], [t_FT, tB[bq]])
        aT = lambda h: FT[0:64, 0, h * 128:(h + 1) * 128]
        bT = lambda h: FT[0:64, 1, h * 128:(h + 1) * 128]
        kT = lambda h: FT[0:64, 2, h * 128:(h + 1) * 128]
        rT = lambda h: FT[0:64, 3, h * 128:(h + 1) * 128]
        M, t_M = r_M.get()
        specs = ((bT, aT, tri_s), (aT, bT, tril_s), (kT, aT, tri_s), (bT, rT, tri_i), (kT, rT, tri_i))
        for mi, (lf, rf, msk) in enumerate(specs):
            bq = nb()
            for h in range(4):
                mm(k, B[bq][:, h * 128:(h + 1) * 128], lf(h), rf(h), [t_FT], [tB[bq]])
            for h in range(4):
                tt(k, "dve", M[:, mi, h * 128:(h + 1) * 128], B[bq][:, h * 128:(h + 1) * 128], msk, ALU.mult, [t_c], [t_M, tB[bq]])
        LT, L, LakT, ArbT, ArkT = (M[:, i, :] for i in range(5))
        XT, t_XT = r_XT.get()
        for h in range(4):
            tt(k, "pool", XT[:, h * 128:(h + 1) * 128], LT[:, h * 128:(h + 1) * 128], idn, ALU.add, [t_M, g.t_ident], [t_XT])
        P_, t_P_ = L, t_M
        PT_, t_PT_ = LT, t_M
        for step in range(1, 7):
            b1 = nb()
            for h in range(4):
                hs = slice(h * 128, (h + 1) * 128)
                mm(k, B[b1][:, hs], PT_[:, hs], P_[:, hs], [t_P_, t_PT_], [tB[b1]])
            Pn, t_Pn = r_P.get()
            cp(k, "act", Pn, B[b1], [], [t_Pn, tB[b1]])
            if step < 6:
                b2 = nb()
                for h in range(4):
                    hs = slice(h * 128, (h + 1) * 128)
                    mm(k, B[b2][:, hs], P_[:, hs], PT_[:, hs], [t_P_, t_PT_], [tB[b2]])
                PTn, t_PTn = r_PT.get()
                cp(k, "dve", PTn, B[b2], [], [t_PTn, tB[b2]])
            b3 = nb()
            for h in range(4):
                hs = slice(h * 128, (h + 1) * 128)
                mm(k, B[b3][:, hs], Pn[:, hs], XT[:, hs], [t_Pn, t_XT], [tB[b3]])
            XTn, t_XTn = r_XT.get()
            tt(k, "dve", XTn, XT, B[b3], ALU.add, [t_XT], [t_XTn, tB[b3]])
            XT, t_XT = XTn, t_XTn
            P_, t_P_ = Pn, t_Pn
            if step < 6:
                PT_, t_PT_ = PTn, t_PTn
        b1 = nb()
        for h in range(4):
            vs = slice(h * 64, (h + 1) * 64)
            mm(k, B[b1][:, vs], aT(h), S[0:64, vs], [t_FT, t_S], [tB[b1]], start=True, stop=False)
            mm(k, B[b1][:, vs], LakT[:, h * 128:(h + 1) * 128], v[:, vs], [t_M, t_seg], [tB[b1]], start=False, stop=True)
        rhs0, t_rhs0 = r_rhs0.get()
        cp(k, "act", rhs0, B[b1][:, 0:256], [], [t_rhs0, tB[b1]])
        b2 = nb()
        for h in range(4):
            vs = slice(h * 64, (h + 1) * 64)
            mm(k, B[b2][:, vs], XT[:, h * 128:(h + 1) * 128], rhs0[:, vs], [t_XT, t_rhs0], [tB[b2]])
        U, t_U = r_U.get()
        cp(k, "dve", U, B[b2][:, 0:256], [], [t_U, tB[b2]])
        b3 = nb()
        for h in range(4):
            vs = slice(h * 64, (h + 1) * 64)
            hs = slice(h * 128, (h + 1) * 128)
            mm(k, B[b3][:, vs], rT(h), S[0:64, vs], [t_FT, t_S], [tB[b3]], start=True, stop=False)
            mm(k, B[b3][:, vs], ArbT[:, hs], U[:, vs], [t_M, t_U], [tB[b3]], start=False, stop=False)
            mm(k, B[b3][:, vs], ArkT[:, hs], v[:, vs], [t_M, t_seg], [tB[b3]], start=False, stop=True)
        y, t_y = r_y.get()
        cp(k, "act", y, B[b3][:, 0:256], [], [t_y, tB[b3]])
        b4 = nb()
        for h in range(4):
            vs = slice(h * 64, (h + 1) * 64)
            mm(k, B[b4][0:64, vs], bt[:, vs], U[:, vs], [t_bt, t_U], [tB[b4]], start=True, stop=False)
            mm(k, B[b4][0:64, vs], kt[:, vs], v[:, vs], [t_kt, t_seg], [tB[b4]], start=False, stop=True)
        tt(k, "dve", S[0:64, :], S[0:64, :], B[b4][0:64, 0:256], ALU.add, [], [t_S, tB[b4]])
        tt(k, "dve", v3(S[0:64, :], 64), v3(S[0:64, :], 64), bc_last(wT[0:64, :], 64), ALU.mult, [t_wT], [t_S])
        sm2, t_sm2 = r_sm.get()
        k.op("dve", lambda e, o_=sm2[:, 0:4], i_=v3(y, 64): e.tensor_reduce(out=o_, in_=i_, axis=AX.X, op=ALU.add), [t_y], [t_sm2])
        ts(k, "dve", sm2[:, 0:4], sm2[:, 0:4], -1.0 / 64, None, ALU.mult, None, [], [t_sm2])
        tt(k, "dve", v3(y, 64), v3(y, 64), bc_last(sm2[:, 0:4], 64), ALU.add, [t_sm2], [t_y])
        tmp2, t_tmp2 = r_tmp.get()
        tt(k, "pool", tmp2, y, y, ALU.mult, [t_y], [t_tmp2])
        k.op("dve", lambda e, o_=sm2[:, 4:8], i_=v3(tmp2, 64): e.tensor_reduce(out=o_, in_=i_, axis=AX.X, op=ALU.add), [t_tmp2], [t_sm2])
        rsqrt(k, sm2[:, 4:8], sm2[:, 4:8], 1.0 / 64, 64e-5, [], [t_sm2])
        tt(k, "dve", v3(y, 64), v3(y, 64), bc_last(sm2[:, 4:8], 64), ALU.mult, [t_sm2], [t_y])
        tt(k, "pool", y, y, vec["d_gn_w"], ALU.mult, [t_c], [t_y])
        tt(k, "pool", y, y, vec["d_gn_b"], ALU.add, [t_c], [t_y])
        tmp3, t_tmp3 = r_tmp.get()
        tt(k, "dve", tmp3, r, kp, ALU.mult, [t_seg, t_kp], [t_tmp3])
        tt(k, "dve", tmp3, tmp3, vec["d_r_k"], ALU.mult, [t_c], [t_tmp3])
        k.op("dve", lambda e, o_=sm2[:, 8:12], i_=v3(tmp3, 64): e.tensor_reduce(out=o_, in_=i_, axis=AX.X, op=ALU.add), [t_tmp3], [t_sm2])
        tt(k, "dve", v3(tmp3, 64), v3(v, 64), bc_last(sm2[:, 8:12], 64), ALU.mult, [t_seg, t_sm2], [t_tmp3])
        tt(k, "pool", y, y, tmp3, ALU.add, [t_tmp3], [t_y])
        o, t_o = r_o.get()
        tt(k, "dve", o, y, gsb, ALU.mult, [t_y, t_gs], [t_o])
        k.dma("pool", g.Y[r0:r0 + 128, 768:1024], o, reads=[t_o], writes=[g.t_Y])


def bc_mid(ap, n):
    a = [list(x) for x in ap.ap]
    return bass.AP(ap.tensor, ap.offset, [a[0], [0, n]] + a[1:])


def stage_b(g, l):
    k, ar = g.k, g.ar
    k.barrier()
    ar.reset()
    W = g.w
    B, tB = g.banks, g.t_bank
    t_c = T()
    gain = ar.alloc([64])
    wuv = ar.alloc([256])
    cneg = ar.alloc([128])
    k.dma("sp", gain, bcast_rows(W["b_kv_gain"][l], 64), writes=[t_c])
    k.dma("sp", v3(wuv[0:64, :], 64), W["b_w_uv"][l].rearrange("h c d -> c h d"), writes=[t_c])
    k.dma("sp", cneg, g.c["caus_neg"], writes=[t_c])
    ckv = ar.alloc([NT, 64])
    t_ckv = T()
    k.dma("sp", ckv, g.P[:, O_CKV:O_CKV + 64].rearrange("(t p) c -> p t c", p=128), reads=[g.t_P], writes=[t_ckv])
    iw = ar.alloc([NT, 8])
    t_iw = T()
    k.dma("sp", iw, g.P[:, O_IW:O_IW + 8].rearrange("(t p) c -> p t c", p=128), reads=[g.t_P], writes=[t_iw])
    ikT = ar.alloc([SEQ])
    t_ik = T()
    k.dma("sp", ikT[0:32, :], g.PT[O_IK:O_IK + 32, :], reads=[g.t_PT], writes=[t_ik])
    sq = ar.alloc([NT, 64])
    t_sq = T()
    ss = ar.alloc([NT])
    tt(k, "pool", sq, ckv, ckv, ALU.mult, [t_ckv], [t_sq])
    k.op("dve", lambda e: e.tensor_reduce(out=ss, in_=sq, axis=AX.X, op=ALU.add), [t_sq], [t_sq])
    rsqrt(k, ss, ss, 1.0 / 64, 1e-6, [], [t_sq])
    tt(k, "dve", ckv, ckv, bc_last(ss, 64), ALU.mult, [t_sq], [t_ckv])
    tt(k, "dve", ckv, ckv, bc_mid(gain, NT), ALU.mult, [t_c], [t_ckv])
    ckvT = ar.alloc([SEQ])
    t_cT = T()
    for j4 in range(4):
        for jj in range(4):
            j = j4 * 4 + jj
            tp(g, B[j4][0:64, jj * 128:(jj + 1) * 128], ckv[:, j, :], [t_ckv], [tB[j4]])
        cp(k, "act" if j4 % 2 else "dve", ckvT[0:64, j4 * 512:(j4 + 1) * 512], B[j4][0:64, :], [], [t_cT, tB[j4]])
    vaug = ar.alloc([NT, 4, 65])
    t_v = T()
    k.op("dve", lambda e: e.memset(vaug[:, :, :, 64:65], 1.0), [], [t_v])
    for j in range(NT):
        bk = 4 + j % 4
        mm(k, B[bk][:, 0:256], ckvT[0:64, j * 128:(j + 1) * 128], wuv[0:64, :], [t_cT, t_c], [tB[bk]])
        cp(k, "act" if j % 2 else "dve", vaug[:, j, :, 0:64], v3(B[bk][:, 0:256], 64), [], [t_v, tB[bk]])
    ysb = ar.alloc([NT, 256])
    t_y = T()
    iqT = ar.alloc([8, 512])
    t_iq = T()
    qT = ar.alloc([4, 512])
    t_q = T()
    strips = ar.alloc([2816])
    t_st = T()
    MT = ar.alloc([NT, 512])
    t_MT = T()
    r_sc, r_wk, r_tmp, r_m8 = Rot(ar, [SEQ], 2), Rot(ar, [SEQ], 1), Rot(ar, [512], 3), Rot(ar, [8], 2)
    st = {"sb": 0, "pt": Rot(ar, [512], 3), "rd": Rot(ar, [1], 4)}
    bi = 0
    for I in range(4):
        for ih in range(8):
            k.dma("sp", iqT[0:32, ih, :], g.PT[O_IQ + ih * 32:O_IQ + (ih + 1) * 32, I * 512:(I + 1) * 512], reads=[g.t_PT], writes=[t_iq])
        for h in range(4):
            k.dma("sp", qT[0:64, h, :], g.PT[O_BQ + h * 64:O_BQ + (h + 1) * 64, I * 512:(I + 1) * 512], reads=[g.t_PT], writes=[t_q])
        k.op("pool", lambda e: e.memset(MT, 0.0), [], [t_MT])
        for qi in range(4):
            i = 4 * I + qi
            nk = (i + 1) * 128
            sc, t_sc = r_sc.get()
            for ih in range(8):
                for kb in range(0, nk, 512):
                    n = min(512, nk - kb)
                    bk = bi % 2
                    bi += 1
                    mm(k, B[bk][:, 0:n], iqT[0:32, ih, qi * 128:(qi + 1) * 128], ikT[0:32, kb:kb + n], [t_iq, t_ik], [tB[bk]])
                    if ih == 0:
                        act(k, sc[:, kb:kb + n], B[bk][:, 0:n], AF.Relu, [], [t_sc, tB[bk]])
                        ts(k, "dve", sc[:, kb:kb + n], sc[:, kb:kb + n], iw[:, i, 0:1], None, ALU.mult, None, [t_iw], [t_sc])
                    else:
                        tmp, t_tmp = r_tmp.get()
                        act(k, tmp[:, 0:n], B[bk][:, 0:n], AF.Relu, [], [t_tmp, tB[bk]])
                        stt(k, "dve", sc[:, kb:kb + n], tmp[:, 0:n], iw[:, i, ih:ih + 1], sc[:, kb:kb + n], ALU.mult, ALU.add,
                            [t_tmp, t_iw], [t_sc])
            tt(k, "dve", sc[:, i * 128:nk], sc[:, i * 128:nk], cneg, ALU.add, [t_c], [t_sc])
            m8, t_m8 = r_m8.get()
            if i >= 2:
                wk, t_wk = r_wk.get()
                cp(k, "pool", wk[:, 0:nk], sc[:, 0:nk], [t_sc], [t_wk])
                for rnd in range(32):
                    k.op("dve", lambda e, o_=m8, i_=wk[:, 0:nk]: e.max(out=o_, in_=i_), [t_wk], [t_m8])
                    if rnd < 31:
                        k.op("dve", lambda e, o_=wk[:, 0:nk], r_=m8: e.match_replace(out=o_, in_to_replace=r_, in_values=o_, imm_value=-1e30),
                             [t_m8], [t_wk])
                thr = m8[:, 7:8]
            else:
                k.op("dve", lambda e, o_=m8: e.memset(o_, -1e29), [], [t_m8])
                thr = m8[:, 7:8]
            ts(k, "dve", sc[:, 0:nk], sc[:, 0:nk], thr, None, ALU.is_ge, None, [t_m8], [t_sc])
            for j4 in range(0, i + 1, 4):
                nj = min(4, i + 1 - j4)
                bk = 2 + (bi % 2)
                bi += 1
                for jj in range(nj):
                    j = j4 + jj
                    tp(g, B[bk][:, jj * 128:(jj + 1) * 128], sc[:, j * 128:(j + 1) * 128], [t_sc], [tB[bk]])
                cp(k, "act", MT[:, j4:j4 + nj, qi * 128:(qi + 1) * 128], v3(B[bk][:, 0:nj * 128], 128), [], [t_MT, tB[bk]])
        for h in range(4):
            k.dma("sp", strips, g.G[4 + h][:, 127:127 + 2816], reads=[g.t_G], writes=[t_st])
            attn_block(g, I, ckvT, t_cT, qT[0:64, h, :], t_q, strips, t_st, lambda j, h=h: vaug[:, j, h, :], t_v, 64, ysb, t_y, h * 64, st,
                       mask=MT, t_mask=t_MT)
    k.dma("pool", g.Y[:, 256:512].rearrange("(t p) c -> p t c", p=128), ysb, reads=[t_y], writes=[g.t_Y])


_CACHE = {}


def kernel(**inputs):
    if "nc" not in _CACHE:
        _CACHE["nc"] = build(nlayers=DEPTH, nseq=4, stages=("P", "C", "A", "D", "B", "M", "E"))
    nc, consts = _CACHE["nc"]
    inputs = {n: np.asarray(a, dtype=np.float32) for n, a in inputs.items()}
    in_maps = [make_inputs(inputs, consts, c) for c in range(NCORES)]
    res = run_bass_kernel_spmd(nc, in_maps, core_ids=list(range(NCORES)))
    out = np.concatenate([np.asarray(r["out"]).reshape(4, SEQ, D) for r in res.results], axis=0)
    return out.astype(np.float32)
```

```python
import math
from contextlib import ExitStack
import numpy as np
import concourse.bass as bass
import concourse.mybir as mybir
from concourse.bass_utils import run_bass_kernel_spmd

F32 = mybir.dt.float32
BF16 = mybir.dt.bfloat16
AF = mybir.ActivationFunctionType
ALU = mybir.AluOpType
AX = mybir.AxisListType

NCORES = 8
D = 1024
SEQ = 2048
DEPTH = 4
NT = SEQ // 128
INW = 7096
BW = 256
DN_ALPHA = (2 * DEPTH) ** 0.25
LN_EPS = 1e-5
O_AQ, O_AK, O_AV, O_BQ, O_CKV, O_IQ, O_IK, O_IW = 0, 256, 512, 768, 1024, 1088, 1344, 1376
O_CQ, O_CK, O_CV, O_CA, O_CG, O_D, O_GATE = 1384, 1512, 1640, 1896, 1912, 2168, 3000
import os
CCUT = int(os.environ.get('CCUT', '0'))
NDS = 24
NE = 2048 + 128
WSHAPES = {
    "rpb_table": [32, 8], "b_kv_gain": [4, 64], "b_w_uv": [4, 4, 64, 64], "c_a_up": [4, 16, 128], "c_a_bias": [4, 128],
    "c_norm_gain": [4, 256], "d_mu": [4, 832], "d_w0": [4, 256], "d_w2": [4, 16, 256], "d_a0": [4, 256], "d_a2": [4, 16, 256],
    "d_g2": [4, 32, 256], "d_k_k": [4, 256], "d_k_a": [4, 256], "d_r_k": [4, 256], "d_gn_w": [4, 256], "d_gn_b": [4, 256],
    "w_branch": [4, 4, 256, 1024], "w_out": [4, 1024, 1024], "ln_g": [4, 2, 1024], "ln_b": [4, 2, 1024],
    "router_g": [4, 1024, 4], "router_g_bias": [4, 4], "router_e": [4, 1024, 32], "router_e_bias": [4, 32],
    "moe_w_gate": [4, 32, 1024, 256], "moe_w_up": [4, 32, 1024, 256], "moe_w_down": [4, 32, 256, 1024],
}


class T:
    __slots__ = ("lw", "rd")

    def __init__(self):
        self.lw = None
        self.rd = {}


class K:
    ENG = ("pe", "act", "dve", "pool", "sp")

    def __init__(self, nc, es):
        self.nc = nc
        self.prog = {e: [] for e in self.ENG}
        self.sem = {}
        self.cnt = {}
        for e in self.ENG:
            self.sem[e] = es.enter_context(nc.semaphore("s_" + e))
            self.cnt[e] = 0
        self.known = {e: {} for e in self.ENG}
        self.dq = {}
        self.dqi = {}
        for q in ("sp", "pool", "act"):
            self.dq[q] = []
            self.dqi[q] = 0
            for i in range(NDS):
                key = "d_%s%d" % (q, i)
                self.sem[key] = es.enter_context(nc.semaphore(key))
                self.cnt[key] = 0
                self.dq[q].append(key)

    def _deps(self, reads, writes):
        d = {}
        for r in reads:
            if r.lw is not None:
                k, v = r.lw
                if d.get(k, 0) < v:
                    d[k] = v
        for w in writes:
            if w.lw is not None:
                k, v = w.lw
                if d.get(k, 0) < v:
                    d[k] = v
            for k, v in w.rd.items():
                if d.get(k, 0) < v:
                    d[k] = v
        return d

    def _wait(self, e, d):
        kn = self.known[e]
        for k, v in d.items():
            if k == e and e == "pe":
                continue
            if kn.get(k, 0) < v:
                self.prog[e].append(("w", k, v))
                kn[k] = v

    def op(self, e, fn, reads=(), writes=()):
        self._wait(e, self._deps(reads, writes))
        self.cnt[e] += 1
        c = self.cnt[e]
        self.prog[e].append(("o", fn, e, 1))
        for w in writes:
            w.lw = (e, c)
            w.rd = {}
        for r in reads:
            if r not in writes:
                r.rd[e] = c

    def dma(self, q, out_ap, in_ap, reads=(), writes=(), **kw):
        i = self.dqi[q]
        self.dqi[q] = (i + 1) % NDS
        key = self.dq[q][i]
        d = self._deps(reads, writes)
        if self.cnt[key] > 0 and d.get(key, 0) < self.cnt[key]:
            d[key] = self.cnt[key]
        self._wait(q, d)
        self.cnt[key] += 16
        c = self.cnt[key]
        self.prog[q].append(("o", lambda eng: eng.dma_start(out=out_ap, in_=in_ap, **kw), key, 16))
        for w in writes:
            w.lw = (key, c)
            w.rd = {}
        for r in reads:
            r.rd[key] = c

    def barrier(self):
        d = {k: v for k, v in self.cnt.items() if v > 0}
        for e in self.ENG:
            self._wait(e, d)

    def replay(self):
        nc = self.nc
        with nc.Block() as block:
            def mk(e):
                prog = self.prog[e]
                sem = self.sem

                def body(eng):
                    for it in prog:
                        if it[0] == "w":
                            eng.wait_ge(sem[it[1]], it[2])
                        else:
                            it[1](eng).then_inc(sem[it[2]], it[3])
                return body
            block.tensor(mk("pe"))
            block.scalar(mk("act"))
            block.vector(mk("dve"))
            block.gpsimd(mk("pool"))
            block.sync(mk("sp"))


class Arena:
    def __init__(self, ap, words):
        self.ap = ap
        self.words = words
        self.off = 0
        self.base = 0

    def alloc(self, shape):
        n = int(np.prod(shape))
        assert self.off + n <= self.words, ("arena overflow", self.off, n, self.words)
        v = self.ap[:, self.off:self.off + n]
        self.off += n
        if len(shape) == 2:
            v = v.rearrange("p (a b) -> p a b", b=shape[1])
        elif len(shape) == 3:
            v = v.rearrange("p (a b c) -> p a b c", b=shape[1], c=shape[2])
        return v

    def alloc_bf(self, shape):
        n = int(np.prod(shape))
        assert n % 2 == 0 and self.off + n // 2 <= self.words, ("arena overflow", self.off, n, self.words)
        v = self.ap[:, self.off:self.off + n // 2].bitcast(BF16)
        self.off += n // 2
        if len(shape) == 2:
            v = v.rearrange("p (a b) -> p a b", b=shape[1])
        elif len(shape) == 3:
            v = v.rearrange("p (a b c) -> p a b c", b=shape[1], c=shape[2])
        return v

    def mark(self):
        self.base = self.off

    def reset(self):
        self.off = self.base


def host_consts():
    c = {}
    c["ident"] = np.eye(128, dtype=np.float32)
    s = np.arange(128)
    c["tri_incl"] = (s[:, None] <= s[None, :]).astype(np.float32)
    c["tri_strict"] = (s[:, None] < s[None, :]).astype(np.float32)
    c["ones"] = np.ones((128, 128), np.float32)
    c["tril_strict"] = (s[:, None] > s[None, :]).astype(np.float32)
    c["m05"] = np.full((128, 1), -0.5, np.float32)
    c["caus_neg"] = np.where(s[None, :] > s[:, None], -1e30, 0.0).astype(np.float32)
    hc = np.arange(128) // 32
    hv = np.arange(256) // 64
    c["bm_c"] = (hc[:, None] == hv[None, :]).astype(np.float32)
    c.update(rpb_consts())
    return c


class Ctx:
    pass


def build(nlayers=DEPTH, nseq=4, stages=("P",), dbg=(), extra=None):
    nc = bass.Bass("TRN2", target_bir_lowering=False)
    g = Ctx()
    g.nc = nc
    NTOK = nseq * SEQ

    def din(name, shape):
        return nc.dram_tensor(name, list(shape), F32, kind="ExternalInput").ap()

    def dscr(name, shape, out=False):
        return nc.dram_tensor(name, list(shape), F32, kind="ExternalOutput" if out else "Internal").ap()

    g.x_in = din("x", [NTOK, D])
    g.w_in = din("w_in", [DEPTH, D, INW])
    g.w = {n: din(n, shp) for n, shp in WSHAPES.items()}
    consts = host_consts()
    if extra:
        consts.update(extra)
    g.c = {n: din("c_" + n, a.shape) for n, a in consts.items()}
    g.out = dscr("out", [NTOK, D], out=True)
    g.P = dscr("P", [SEQ, INW], out=("P" in dbg))
    g.PT = dscr("PT", [2048, SEQ], out=("PT" in dbg))
    g.Y = dscr("Y", [SEQ, D], out=("Y" in dbg))
    g.X1 = dscr("X1", [SEQ, D], out=("X1" in dbg))
    g.t_P, g.t_PT, g.t_Y, g.t_X1, g.t_G, g.t_out = T(), T(), T(), T(), T(), T()
    g.G = dscr("G", [8, 128, GP])

    with ExitStack() as es:
        k = K(nc, es)
        g.k = k
        AW = 46 * 1024
        arena_t = es.enter_context(nc.sbuf_tensor("arena", [128, AW], F32))
        g.ar = Arena(arena_t[:, :], AW)
        g.banks = [es.enter_context(nc.psum_tensor("bank%d" % i, [128, 512], F32))[:, :] for i in range(8)]
        g.t_bank = [T() for _ in range(8)]
        ar = g.ar
        g.ident = ar.alloc([128])
        g.t_ident = T()
        k.dma("sp", g.ident, g.c["ident"], writes=[g.t_ident])
        g.cm05 = ar.alloc([1])
        k.dma("sp", g.cm05, g.c["m05"], writes=[g.t_ident])
        ar.mark()

        if "A" in stages or "B" in stages:
            stage_setup(g)
        for l in range(nlayers):
            for s in range(nseq):
                xsrc = g.x_in if l == 0 else g.out
                if "P" in stages:
                    stage_proj(g, l, s, xsrc)
                if "C" in stages:
                    stage_c(g, l)
                if "A" in stages:
                    stage_a(g, l)
                if "D" in stages:
                    stage_d(g, l)
                if "B" in stages:
                    stage_b(g, l)
                if "Yref" in stages:
                    k.barrier()
                    k.dma("sp", g.Y, g.c["yref"], writes=[g.t_Y])
                if "M" in stages:
                    stage_m(g, l, s, xsrc)
                if "X1ref" in stages:
                    k.barrier()
                    k.dma("sp", g.X1, g.c["x1ref"], writes=[g.t_X1])
                if "E" in stages:
                    stage_e(g, l, s)
        k.barrier()
        k.replay()
    return nc, consts


def stage_proj(g, l, s, xsrc):
    k, ar, nc = g.k, g.ar, g.nc
    k.barrier()
    ar.reset()
    xT = ar.alloc_bf([8, SEQ])
    t_xT = [T() for _ in range(NT)]
    xin = [ar.alloc([D]) for _ in range(2)]
    t_xin = [T(), T()]
    t_bank = [T() for _ in range(8)]
    for tt in range(NT):
        b = tt % 2
        r0 = s * SEQ + tt * 128
        k.dma("sp", xin[b], xsrc[r0:r0 + 128, :], writes=[t_xin[b]])
        for hb in range(2):
            bk = 2 * b + hb
            for kc4 in range(4):
                kc = hb * 4 + kc4
                k.op("pe", lambda e, o=g.banks[bk][:, kc4 * 128:(kc4 + 1) * 128], i=xin[b][:, kc * 128:(kc + 1) * 128]:
                     e.transpose(o, i, g.ident), reads=[t_xin[b], g.t_ident], writes=[t_bank[bk]])
            eng = "dve" if hb == 0 else "act"
            o = xT[:, hb * 4:hb * 4 + 4, tt * 128:(tt + 1) * 128]
            i = g.banks[bk].rearrange("p (a b) -> p a b", b=128)
            if eng == "dve":
                k.op("dve", lambda e, o=o, i=i: e.tensor_copy(out=o, in_=i), reads=[t_bank[bk]], writes=[t_xT[tt]])
            else:
                k.op("act", lambda e, o=o, i=i: e.copy(out=o, in_=i), reads=[t_bank[bk]], writes=[t_xT[tt]])
    wtf = [ar.alloc([8, 512]) for _ in range(2)]
    t_wtf = [T(), T()]
    wt = [ar.alloc_bf([8, 512]) for _ in range(2)]
    t_wt = [T(), T()]
    ot = [ar.alloc([512]) for _ in range(4)]
    t_ot = [T() for _ in range(4)]
    t_P = [g.t_P] * NT
    t_PT = g.t_PT
    oi = 0
    bi = 0
    ncb = (INW + 511) // 512
    for cb in range(ncb):
        c0 = cb * 512
        ncol = min(512, INW - c0)
        wb = cb % 2
        k.dma("sp", wtf[wb][:, :, :ncol], g.w_in[l][:, c0:c0 + ncol].rearrange("(a p) n -> p a n", p=128),
              writes=[t_wtf[wb]])
        cp(k, "pool", wt[wb][:, :, :ncol], wtf[wb][:, :, :ncol], [t_wtf[wb]], [t_wt[wb]])
        for tt in range(NT):
            bk = 4 + (bi % 4)
            bi += 1
            for kc in range(8):
                k.op("pe", lambda e, o=g.banks[bk][:, :ncol], a=xT[:, kc, tt * 128:(tt + 1) * 128], b=wt[wb][:, kc, :ncol], kc=kc:
                     e.matmul(o, a, b, start=(kc == 0), stop=(kc == 7)), reads=[t_xT[tt], t_wt[wb]], writes=[t_bank[bk]])
            ob = oi % 4
            oi += 1
            if oi % 2 == 0:
                k.op("dve", lambda e, o=ot[ob][:, :ncol], i=g.banks[bk][:, :ncol]: e.tensor_copy(out=o, in_=i),
                     reads=[t_bank[bk]], writes=[t_ot[ob]])
            else:
                k.op("act", lambda e, o=ot[ob][:, :ncol], i=g.banks[bk][:, :ncol]: e.copy(out=o, in_=i),
                     reads=[t_bank[bk]], writes=[t_ot[ob]])
            k.dma("pool", g.P[tt * 128:(tt + 1) * 128, c0:c0 + ncol], ot[ob][:, :ncol], reads=[t_ot[ob]], writes=[t_P[tt]])
        for sub in range(4):
            r0 = c0 + sub * 128
            if not (r0 < 1408 or r0 == 1792):
                continue
            for tb in range(4):
                bk = 4 + (bi % 4)
                bi += 1
                for kc in range(8):
                    k.op("pe", lambda e, o=g.banks[bk], a=wt[wb][:, kc, sub * 128:(sub + 1) * 128], b=xT[:, kc, tb * 512:(tb + 1) * 512], kc=kc:
                         e.matmul(o, a, b, start=(kc == 0), stop=(kc == 7)),
                         reads=t_xT[tb * 4:tb * 4 + 4] + [t_wt[wb]], writes=[t_bank[bk]])
                ob = oi % 4
                oi += 1
                if oi % 2 == 0:
                    k.op("dve", lambda e, o=ot[ob], i=g.banks[bk]: e.tensor_copy(out=o, in_=i), reads=[t_bank[bk]], writes=[t_ot[ob]])
                else:
                    k.op("act", lambda e, o=ot[ob], i=g.banks[bk]: e.copy(out=o, in_=i), reads=[t_bank[bk]], writes=[t_ot[ob]])
                k.dma("pool", g.PT[r0:r0 + 128, tb * 512:(tb + 1) * 512], ot[ob], reads=[t_ot[ob]], writes=[t_PT])


def mm(k, out, lhsT, rhs, R, W, start=True, stop=True):
    k.op("pe", lambda e: e.matmul(out, lhsT, rhs, start=start, stop=stop), R, W)


def tp(g, out, in_, R, W):
    n = in_.shape[0]
    g.k.op("pe", lambda e: e.transpose(out, in_, g.ident[:n, :n]), list(R) + [g.t_ident], W)


def tt(k, eng, out, a, b, op, R, W):
    k.op(eng, lambda e: e.tensor_tensor(out=out, in0=a, in1=b, op=op), R, W)


def ts(k, eng, out, a, s1, s2, op0, op1, R, W, accum=None):
    if s2 is None:
        k.op(eng, lambda e: e.tensor_scalar(out=out, in0=a, scalar1=s1, scalar2=None, op0=op0), R, W)
    elif accum is None:
        k.op(eng, lambda e: e.tensor_scalar(out=out, in0=a, scalar1=s1, scalar2=s2, op0=op0, op1=op1), R, W)
    else:
        k.op(eng, lambda e: e.tensor_scalar(out=out, in0=a, scalar1=s1, scalar2=s2, op0=op0, op1=op1, accum_out=accum), R, W)


def stt(k, eng, out, a, sc, b, op0, op1, R, W):
    k.op(eng, lambda e: e.scalar_tensor_tensor(out=out, in0=a, scalar=sc, in1=b, op0=op0, op1=op1), R, W)


def act(k, out, in_, func, R, W, bias=None, scale=1.0, accum=None):
    kw = {}
    if bias is not None:
        kw["bias"] = bias
    if accum is not None:
        kw["accum_out"] = accum
    k.op("act", lambda e: e.activation(out=out, in_=in_, func=func, scale=scale, **kw), R, W)


def rsqrt(k, out, in_, scale, eps, R, W):
    ts(k, "dve", out, in_, scale, eps, ALU.mult, ALU.add, R, W)
    k.op("act", lambda e: e.activation(out=out, in_=out, func=AF.Sqrt), [], W)
    k.op("dve", lambda e: e.reciprocal(out=out, in_=out), [], W)


def cp(k, eng, out, in_, R, W):
    if eng == "act":
        k.op("act", lambda e: e.copy(out=out, in_=in_), R, W)
    else:
        k.op(eng, lambda e: e.tensor_copy(out=out, in_=in_), R, W)


def bcast_rows(ap1d, n):
    return bass.AP(ap1d.tensor, ap1d.offset, [[0, 128], [1, n]])


class Rot:
    def __init__(self, ar, shape, n, bf=False):
        self.b = [((ar.alloc_bf(shape) if bf else ar.alloc(shape)), T()) for _ in range(n)]
        self.i = 0

    def get(self):
        r = self.b[self.i % len(self.b)]
        self.i += 1
        return r


class Banks:
    def __init__(self, g, ids):
        self.b = [(g.banks[i], g.t_bank[i]) for i in ids]
        self.i = 0

    def get(self):
        r = self.b[self.i % len(self.b)]
        self.i += 1
        return r


def stage_c(g, l):
    k, ar = g.k, g.ar
    k.barrier()
    ar.reset()
    W = g.w
    t_c = T()
    aup = ar.alloc([128])
    abias = ar.alloc([128])
    gain = ar.alloc([256])
    bm = ar.alloc([512])
    tri = ar.alloc([128])
    ones = ar.alloc([128])
    k.dma("sp", aup[0:16, :], W["c_a_up"][l], writes=[t_c])
    k.dma("sp", abias, bcast_rows(W["c_a_bias"][l], 128), writes=[t_c])
    k.dma("sp", gain, bcast_rows(W["c_norm_gain"][l], 256), writes=[t_c])
    for p_ in range(2):
        k.dma("sp", bm[0:64, p_ * 256:(p_ + 1) * 256], g.c["bm_c"][p_ * 64:(p_ + 1) * 64, :], writes=[t_c])
    k.dma("sp", tri, g.c["tri_incl"], writes=[t_c])
    k.dma("sp", ones, g.c["ones"], writes=[t_c])
    state = ar.alloc([256])
    t_state = T()
    k.op("dve", lambda e: e.memset(state, 0.0), [], [t_state])
    pin = Rot(ar, [784], 2)
    alT = Rot(ar, [128], 2)
    sb = lambda n, w: Rot(ar, [w], n)
    r_zb, r_sp, r_cum, r_eq, r_ek, r_el, r_dec = sb(2, 128), sb(2, 128), sb(2, 128), sb(2, 128), sb(2, 128), sb(2, 128), sb(2, 4)
    r_qd, r_ki, r_kl, r_qkT, r_att, r_o, r_tmp, r_sq, r_ss, r_sg, r_y = (sb(2, 128), sb(2, 128), sb(2, 128), sb(2, 1024), sb(2, 512),
                                                                        sb(2, 256), sb(2, 256), sb(2, 256), sb(2, 4), sb(2, 256), sb(2, 256))
    pb = g.banks
    ps_z, ps_cum, ps_last, ps_lt = pb[0][:, 0:128], pb[1][:, 0:128], pb[2][:, 0:128], pb[5][:, 0:4]
    ps_tr = pb[3]
    ps_att = pb[4]
    ps_o = pb[6][:, 0:256]
    ps_upd = pb[7][:, 0:256]
    tb_ = g.t_bank
    t_z, t_cum, t_last, t_lt, t_tr, t_att, t_o, t_upd = tb_[0], tb_[1], tb_[2], tb_[5], tb_[3], tb_[4], tb_[6], tb_[7]
    for tt_ in range(NT):
        r0 = tt_ * 128
        x, t_x = pin.get()
        k.dma("sp", x, g.P[r0:r0 + 128, O_CQ:O_CQ + 784], reads=[g.t_P], writes=[t_x])
        al, t_al = alT.get()
        k.dma("sp", al[0:16, :], g.PT[O_CA:O_CA + 16, r0:r0 + 128], reads=[g.t_PT], writes=[t_al])
        q, kk, v, og = x[:, 0:128], x[:, 128:256], x[:, 256:512], x[:, 528:784]
        mm(k, ps_z, al[0:16, :], aup[0:16, :], [t_al, t_c], [t_z])
        zb, t_zb = r_zb.get()
        tt(k, "dve", zb, ps_z, abias, ALU.add, [t_c], [t_zb, t_z])
        sp_, t_sp = r_sp.get()
        act(k, sp_, zb, AF.Exp, [t_zb], [t_sp], scale=-1.0)
        act(k, sp_, sp_, AF.Ln, [], [t_sp], bias=1.0)
        if CCUT == 1:
            continue
        mm(k, ps_cum, tri, sp_, [t_c, t_sp], [t_cum])
        mm(k, ps_last, ones, sp_, [t_c, t_sp], [t_last])
        for h in range(4):
            mm(k, ps_lt[0:32, h:h + 1], sp_[:, h * 32:(h + 1) * 32], ones[:, 0:1], [t_c, t_sp], [t_lt])
        cum, t_cs = r_cum.get()
        cp(k, "dve", cum, ps_cum, [], [t_cs, t_cum])
        eq, t_eq = r_eq.get()
        act(k, eq, cum, AF.Exp, [t_cs], [t_eq], scale=-1.0 / 16)
        ek, t_ek = r_ek.get()
        act(k, ek, cum, AF.Exp, [t_cs], [t_ek], scale=1.0 / 16)
        el, t_el = r_el.get()
        tt(k, "dve", el, ps_last, cum, ALU.subtract, [t_cs], [t_el, t_last])
        act(k, el, el, AF.Exp, [], [t_el], scale=-1.0 / 16)
        dec, t_dec = r_dec.get()
        act(k, dec[0:32, :], ps_lt[0:32, :], AF.Exp, [], [t_dec, t_lt], scale=-1.0 / 16)
        if CCUT == 2:
            continue
        qd, t_qd = r_qd.get()
        stt(k, "dve", qd, q, 32 ** -0.5, eq, ALU.mult, ALU.mult, [t_x, t_eq], [t_qd])
        ki, t_ki = r_ki.get()
        tt(k, "pool", ki, kk, ek, ALU.mult, [t_x, t_ek], [t_ki])
        kl, t_kl = r_kl.get()
        tt(k, "pool", kl, kk, el, ALU.mult, [t_x, t_el], [t_kl])
        if CCUT == 5:
            continue
        for h in range(4):
            tp(g, ps_tr[0:32, h * 128:(h + 1) * 128], qd[:, h * 32:(h + 1) * 32], [t_qd], [t_tr])
        tp4 = g.banks[2]
        for h in range(4):
            tp(g, tp4[0:32, h * 128:(h + 1) * 128], ki[:, h * 32:(h + 1) * 32], [t_ki], [t_last])
        qkT, t_qkT = r_qkT.get()
        cp(k, "act", qkT[0:32, 0:512], ps_tr[0:32, :], [], [t_qkT, t_tr])
        cp(k, "act", qkT[0:32, 512:1024], tp4[0:32, :], [], [t_qkT, t_last])
        for h in range(4):
            mm(k, ps_att[:, h * 128:(h + 1) * 128], qkT[0:32, 512 + h * 128:512 + (h + 1) * 128],
               qkT[0:32, h * 128:(h + 1) * 128], [t_qkT], [t_att])
        att, t_at = r_att.get()
        for h in range(4):
            tt(k, "dve", att[:, h * 128:(h + 1) * 128], ps_att[:, h * 128:(h + 1) * 128], tri, ALU.mult, [t_c], [t_at, t_att])
        for h in range(4):
            mm(k, ps_o[:, h * 64:(h + 1) * 64], qkT[0:32, h * 128:(h + 1) * 128], state[0:32, h * 64:(h + 1) * 64],
               [t_qkT, t_state], [t_o], start=True, stop=False)
            mm(k, ps_o[:, h * 64:(h + 1) * 64], att[:, h * 128:(h + 1) * 128], v[:, h * 64:(h + 1) * 64], [t_at, t_x], [t_o],
               start=False, stop=True)
        for h in range(4):
            mm(k, ps_upd[0:32, h * 64:(h + 1) * 64], kl[:, h * 32:(h + 1) * 32], v[:, h * 64:(h + 1) * 64], [t_kl, t_x], [t_upd])
        tmp, t_tmp = r_tmp.get()
        cp(k, "act", tmp[0:32, :], ps_upd[0:32, :], [], [t_tmp, t_upd])
        for h in range(4):
            stt(k, "dve", state[0:32, h * 64:(h + 1) * 64], state[0:32, h * 64:(h + 1) * 64], dec[0:32, h:h + 1],
                tmp[0:32, h * 64:(h + 1) * 64], ALU.mult, ALU.add, [t_dec, t_tmp], [t_state])
        if CCUT == 4:
            continue
        o, t_os = r_o.get()
        cp(k, "act", o, ps_o, [], [t_os, t_o])
        sq, t_sq = r_sq.get()
        tt(k, "pool", sq, o, o, ALU.mult, [t_os], [t_sq])
        ss, t_ss = r_ss.get()
        k.op("dve", lambda e, o_=ss, i_=sq.rearrange("p (h v) -> p h v", v=64): e.tensor_reduce(out=o_, in_=i_, axis=AX.X, op=ALU.add),
             [t_sq], [t_ss])
        rsqrt(k, ss, ss, 1.0 / 64, 1e-6, [], [t_ss])
        sg, t_sg = r_sg.get()
        act(k, sg, og, AF.Silu, [t_x], [t_sg])
        tt(k, "pool", sg, sg, gain, ALU.mult, [t_c], [t_sg])
        y, t_y = r_y.get()
        for h in range(4):
            stt(k, "dve", y[:, h * 64:(h + 1) * 64], o[:, h * 64:(h + 1) * 64], ss[:, h:h + 1], sg[:, h * 64:(h + 1) * 64],
                ALU.mult, ALU.mult, [t_os, t_ss, t_sg], [t_y])
        k.dma("pool", g.Y[r0:r0 + 128, 512:768], y, reads=[t_y], writes=[g.t_Y])


def make_inputs(inputs, consts, core, nseq=4, small=True):
    m = {}
    m["x"] = np.ascontiguousarray(inputs["x"][core * nseq:(core + 1) * nseq]).reshape(nseq * SEQ, D)
    m["w_in"] = np.ascontiguousarray(inputs["w_in"])
    for n in WSHAPES:
        m[n] = np.ascontiguousarray(inputs[n])
    for n, a in consts.items():
        m["c_" + n] = a
    return m


NEV = 2944
GP = NEV + 128


def t5_bucket_np(dist):
    d = np.maximum(dist, 0)
    df = np.maximum(d, 1).astype(np.float32)
    large = 16 + (np.log(df / np.float32(16)) / np.float32(math.log(2048 / 16)) * np.float32(16)).astype(np.int32)
    large = np.minimum(large, 31)
    return np.where(d < 16, d, large)


def rpb_consts():
    i = np.arange(NEV)
    dist = i - 511
    valid = (dist >= 0) & (dist <= 2047)
    b = t5_bucket_np(dist)
    oht = np.zeros((32, NEV), np.float32)
    oht[b[valid], i[valid]] = 1.0
    cA = ((dist <= 128).astype(np.float32) + ((dist % 4 == 0) & (dist <= 512)) + ((dist % 16 == 0) & (dist <= 2048))) * valid
    cB = valid.astype(np.float32)
    return {"oht": oht, "cmul": np.stack([cA, cB]).astype(np.float32)}


def stage_setup(g):
    k, ar = g.k, g.ar
    k.barrier()
    ar.reset()
    t_c = T()
    oht = ar.alloc([NEV])
    cm = [ar.alloc([NEV]), ar.alloc([NEV])]
    tab = ar.alloc([8])
    ones = ar.alloc([128])
    k.dma("sp", oht[0:32, :], g.c["oht"], writes=[t_c])
    for a in range(2):
        k.dma("sp", cm[a], bcast_rows(g.c["cmul"][a], NEV), writes=[t_c])
    k.dma("sp", tab[0:32, :], g.w["rpb_table"], writes=[t_c])
    k.dma("sp", ones, g.c["ones"], writes=[t_c])
    tabb = Rot(ar, [128], 2)
    eb = Rot(ar, [NEV], 2)
    bi = 0
    for hh in range(8):
        tb, t_tb = tabb.get()
        ts(k, "dve", tb[0:32, :], ones[0:32, :], tab[0:32, hh:hh + 1], None, ALU.mult, None, [t_c], [t_tb])
        e_, t_e = eb.get()
        for c0 in range(0, NEV, 512):
            n = min(512, NEV - c0)
            bk = bi % 4
            bi += 1
            mm(k, g.banks[bk][:, :n], tb[0:32, :], oht[0:32, c0:c0 + n], [t_tb, t_c], [g.t_bank[bk]])
            act(k, e_[:, c0:c0 + n], g.banks[bk][:, :n], AF.Exp, [], [t_e, g.t_bank[bk]])
        tt(k, "dve", e_, e_, cm[0 if hh < 4 else 1], ALU.mult, [t_c], [t_e])
        gh = g.G[hh]
        dst = bass.AP(gh.tensor, gh.offset, [[GP + 1, 128], [1, NEV]])
        k.dma("pool", dst, e_, reads=[t_e], writes=[g.t_G])


def attn_block(g, I, kT, t_kT, qT, t_q, strips, t_st, vaug, t_v, vw, ysb, t_y, ycol, st, mask=None, t_mask=None, after_pair=None):
    k = g.k

    def scores(j):
        bk = st["sb"] % 2
        st["sb"] += 1
        ps = g.banks[bk]
        mm(k, ps, kT[0:64, j * 128:(j + 1) * 128], qT, [t_kT, t_q], [g.t_bank[bk]])
        pt, t_pt = st["pt"].get()
        act(k, pt, ps, AF.Exp, [], [t_pt, g.t_bank[bk]], scale=0.125)
        o = 4 * I - j
        eng = "dve" if st["sb"] % 2 == 0 else "pool"
        tt(k, eng, pt, pt, strips[:, (o + 3) * 128:(o + 3) * 128 + 512], ALU.mult, [t_st], [t_pt])
        if mask is not None:
            tt(k, "pool" if eng == "dve" else "dve", pt, pt, mask[:, j, :], ALU.mult, [t_mask], [t_pt])
        return pt, t_pt

    def pv(j, pt, t_pt):
        for qi in range(4):
            i = 4 * I + qi
            if j > i:
                continue
            ob = 4 + qi
            mm(k, g.banks[ob][:, 0:vw + 1], pt[:, qi * 128:(qi + 1) * 128], vaug(j), [t_pt, t_v], [g.t_bank[ob]],
               start=(j == 0), stop=(j == i))
            if j == i:
                rd, t_rd = st["rd"].get()
                k.op("dve", lambda e, o_=rd, i_=g.banks[ob][:, vw:vw + 1]: e.reciprocal(out=o_, in_=i_), [], [t_rd, g.t_bank[ob]])
                ts(k, "dve", ysb[:, i, ycol:ycol + vw], g.banks[ob][:, 0:vw], rd[:, 0:1], None, ALU.mult, None,
                   [t_rd], [t_y, g.t_bank[ob]])
        if after_pair is not None:
            after_pair()

    prev = None
    for j in range(4 * I + 4):
        cur = (j,) + scores(j)
        if prev is not None:
            pv(*prev)
        prev = cur
    pv(*prev)


def gen_a(g, l, fresh=True):
    k, ar = g.k, g.ar
    if fresh:
        k.barrier()
        ar.reset()
    ysb = ar.alloc([NT, 256])
    t_y = T()
    st = {"sb": 0, "pt": Rot(ar, [512], 4, bf=True), "rd": Rot(ar, [1], 4)}
    qTs, kTs, sts, vas = Rot(ar, [SEQ], 2, bf=True), Rot(ar, [SEQ], 2, bf=True), Rot(ar, [2816], 2), Rot(ar, [NT, 66], 2, bf=True)
    ldq, ldk, ldv = Rot(ar, [SEQ], 1), Rot(ar, [SEQ], 1), Rot(ar, [NT, 64], 1)
    yield
    pend = []
    for h in range(4):
        qT, t_q = qTs.get()
        kT, t_kT = kTs.get()
        strips, t_st = sts.get()
        va, t_v = vas.get()
        fq, t_fq = ldq.get()
        fk, t_fk = ldk.get()
        fv, t_fv = ldv.get()
        k.dma("sp", fq[0:64, :], g.PT[O_AQ + h * 64:O_AQ + (h + 1) * 64, :], reads=[g.t_PT], writes=[t_fq])
        k.dma("sp", fk[0:64, :], g.PT[O_AK + h * 64:O_AK + (h + 1) * 64, :], reads=[g.t_PT], writes=[t_fk])
        k.dma("sp", strips, g.G[h][:, 127:127 + 2816], reads=[g.t_G], writes=[t_st])
        k.dma("sp", fv, g.P[:, O_AV + h * 64:O_AV + (h + 1) * 64].rearrange("(t p) c -> p t c", p=128),
              reads=[g.t_P], writes=[t_fv])
        cp(k, "pool", qT[0:64, :], fq[0:64, :], [t_fq], [t_q])
        cp(k, "act", kT[0:64, :], fk[0:64, :], [t_fk], [t_kT])
        cp(k, "pool", va[:, :, 0:64], fv, [t_fv], [t_v])
        k.op("dve", lambda e, o_=va[:, :, 64:65]: e.memset(o_, 1.0), [], [t_v])
        for I in range(4):
            attn_block(g, I, kT, t_kT, qT[0:64, I * 512:(I + 1) * 512], t_q, strips, t_st, lambda j, va=va: va[:, j, 0:65], t_v, 64, ysb, t_y,
                       h * 64, st, after_pair=lambda: pend.append(1))
            while pend:
                pend.pop()
                yield
    k.dma("pool", g.Y[:, 0:256].rearrange("(t p) c -> p t c", p=128), ysb, reads=[t_y], writes=[g.t_Y])


def stage_a(g, l):
    for _ in gen_a(g, l):
        pass


def stage_ad(g, l):
    k, ar = g.k, g.ar
    k.barrier()
    ar.reset()
    ga = gen_a(g, l, fresh=False)
    next(ga)
    gd = gen_d(g, l, fresh=False, banks=(2, 3))
    next(gd)
    da = dd = False
    while not (da and dd):
        if not da:
            try:
                next(ga)
            except StopIteration:
                da = True
        for _ in range(2):
            if not dd:
                try:
                    next(gd)
                except StopIteration:
                    dd = True


def ln_tile(k, r, t_r, gbc, bbc, t_c, sm, out, t_out):
    s1, t_s1 = sm.get()
    k.op("dve", lambda e: e.tensor_reduce(out=s1[:, 0:1], in_=r, axis=AX.X, op=ALU.add), [t_r], [t_s1])
    ts(k, "dve", s1[:, 0:1], s1[:, 0:1], -1.0 / D, None, ALU.mult, None, [], [t_s1])
    ts(k, "dve", r, r, s1[:, 0:1], None, ALU.add, None, [t_s1], [t_r])
    act(k, out, r, AF.Square, [t_r], [t_out, t_s1], accum=s1[:, 1:2])
    rsqrt(k, s1[:, 1:2], s1[:, 1:2], 1.0 / D, LN_EPS, [], [t_s1])
    stt(k, "dve", out, r, s1[:, 1:2], gbc, ALU.mult, ALU.mult, [t_r, t_s1, t_c], [t_out])
    tt(k, "pool", out, out, bbc, ALU.add, [t_c], [t_out])


def stage_m(g, l, s, xsrc):
    k, ar = g.k, g.ar
    k.barrier()
    ar.reset()
    W = g.w
    t_c = T()
    wb = ar.alloc_bf([8, D])
    wo = ar.alloc_bf([8, D])
    gbc = ar.alloc([D])
    bbc = ar.alloc([D])
    stg = Rot(ar, [2, D], 2)
    for c2 in range(4):
        sb_, t_sb = stg.get()
        k.dma("sp", sb_, W["w_branch"][l, c2].rearrange("(c p) d -> p c d", p=128), writes=[t_sb])
        cp(k, "pool" if c2 % 2 else "act", wb[:, 2 * c2:2 * c2 + 2, :], sb_, [t_sb], [t_c])
    for c2 in range(4):
        sb_, t_sb = stg.get()
        k.dma("sp", sb_, W["w_out"][l][c2 * 256:(c2 + 1) * 256, :].rearrange("(c p) d -> p c d", p=128), writes=[t_sb])
        cp(k, "pool" if c2 % 2 else "act", wo[:, 2 * c2:2 * c2 + 2, :], sb_, [t_sb], [t_c])
    k.dma("sp", gbc, bcast_rows(W["ln_g"][l, 0], D), writes=[t_c])
    k.dma("sp", bbc, bcast_rows(W["ln_b"][l, 0], D), writes=[t_c])
    r_y, r_gate, r_yT, r_mg, r_mT, r_x, r_tmp, r_out, sm = (Rot(ar, [D], 2), Rot(ar, [4 * D], 2), Rot(ar, [D], 2, bf=True), Rot(ar, [D], 2),
                                                          Rot(ar, [D], 2, bf=True), Rot(ar, [D], 2), Rot(ar, [512], 3), Rot(ar, [D], 2), Rot(ar, [2], 4))
    B, tB = g.banks, g.t_bank
    bi = 0
    for tt_ in range(NT):
        r0 = tt_ * 128
        y, t_y = r_y.get()
        k.dma("sp", y, g.Y[r0:r0 + 128, :], reads=[g.t_Y], writes=[t_y])
        gt, t_gt = r_gate.get()
        k.dma("sp", gt, g.P[r0:r0 + 128, O_GATE:O_GATE + 4 * D], reads=[g.t_P], writes=[t_gt])
        x, t_x = r_x.get()
        k.dma("sp", x, xsrc[s * SEQ + r0:s * SEQ + r0 + 128, :], writes=[t_x])
        act(k, gt, gt, AF.Sigmoid, [], [t_gt])
        yT, t_yT = r_yT.get()
        for hb in range(2):
            for c4 in range(4):
                c = hb * 4 + c4
                tp(g, B[hb][:, c4 * 128:(c4 + 1) * 128], y[:, c * 128:(c + 1) * 128], [t_y], [tB[hb]])
            cp(k, "act" if hb else "dve", yT[:, hb * 512:(hb + 1) * 512], B[hb], [], [t_yT, tB[hb]])
        mg, t_mg = r_mg.get()
        for n in range(4):
            for dh in range(2):
                bk = 2 + bi % 2
                bi += 1
                for cc in range(2):
                    c = 2 * n + cc
                    mm(k, B[bk], yT[:, c * 128:(c + 1) * 128], wb[:, c, dh * 512:(dh + 1) * 512], [t_yT, t_c], [tB[bk]],
                       start=(cc == 0), stop=(cc == 1))
                gsl = gt[:, n * D + dh * 512:n * D + (dh + 1) * 512]
                msl = mg[:, dh * 512:(dh + 1) * 512]
                if n == 0:
                    tt(k, "dve", msl, B[bk], gsl, ALU.mult, [t_gt], [t_mg, tB[bk]])
                else:
                    tmp, t_tmp = r_tmp.get()
                    tt(k, "dve", tmp, B[bk], gsl, ALU.mult, [t_gt], [t_tmp, tB[bk]])
                    tt(k, "pool", msl, msl, tmp, ALU.add, [t_tmp], [t_mg])
        mT, t_mT = r_mT.get()
        for hb in range(2):
            for c4 in range(4):
                c = hb * 4 + c4
                tp(g, B[4 + hb][:, c4 * 128:(c4 + 1) * 128], mg[:, c * 128:(c + 1) * 128], [t_mg], [tB[4 + hb]])
            cp(k, "act" if hb else "dve", mT[:, hb * 512:(hb + 1) * 512], B[4 + hb], [], [t_mT, tB[4 + hb]])
        for dh in range(2):
            bk = 6 + dh
            for c in range(8):
                mm(k, B[bk], mT[:, c * 128:(c + 1) * 128], wo[:, c, dh * 512:(dh + 1) * 512], [t_mT, t_c], [tB[bk]],
                   start=(c == 0), stop=(c == 7))
            stt(k, "dve", x[:, dh * 512:(dh + 1) * 512], x[:, dh * 512:(dh + 1) * 512], DN_ALPHA, B[bk], ALU.mult, ALU.add,
                [], [t_x, tB[bk]])
        o, t_o = r_out.get()
        ln_tile(k, x, t_x, gbc, bbc, t_c, sm, o, t_o)
        k.dma("pool", g.X1[r0:r0 + 128, :], o, reads=[t_o], writes=[g.t_X1])


def stage_e(g, l, s):
    k, ar = g.k, g.ar
    W = g.w
    B, tB = g.banks, g.t_bank
    for half in range(2):
        k.barrier()
        ar.reset()
        t_c = T()
        wr = ar.alloc([8, 36])
        rb = ar.alloc([36])
        gbc = ar.alloc([D])
        bbc = ar.alloc([D])
        k.dma("sp", wr[:, :, 0:4], W["router_g"][l].rearrange("(c p) e -> p c e", p=128), writes=[t_c])
        k.dma("sp", wr[:, :, 4:36], W["router_e"][l].rearrange("(c p) e -> p c e", p=128), writes=[t_c])
        k.dma("sp", rb[:, 0:4], bcast_rows(W["router_g_bias"][l], 4), writes=[t_c])
        k.dma("sp", rb[:, 4:36], bcast_rows(W["router_e_bias"][l], 32), writes=[t_c])
        k.dma("sp", gbc, bcast_rows(W["ln_g"][l, 1], D), writes=[t_c])
        k.dma("sp", bbc, bcast_rows(W["ln_b"][l, 1], D), writes=[t_c])
        r_x1 = Rot(ar, [D], 2)
        xT = ar.alloc_bf([8, 1024])
        t_xT = [T() for _ in range(8)]
        r_xf = Rot(ar, [8, 128], 2)
        yacc = ar.alloc([8, D])
        t_ya = [T() for _ in range(8)]
        gates = ar.alloc([8, 32])
        t_g = [T() for _ in range(8)]
        sm = Rot(ar, [40], 4)
        sm2 = Rot(ar, [8], 4)
        sm3 = Rot(ar, [32], 6)
        for ti in range(8):
            r0 = half * 1024 + ti * 128
            x1t, t_x1t = r_x1.get()
            k.dma("sp", x1t, g.X1[r0:r0 + 128, :], reads=[g.t_X1], writes=[t_x1t])
            for hb in range(2):
                for c4 in range(4):
                    c = hb * 4 + c4
                    tp(g, B[hb][:, c4 * 128:(c4 + 1) * 128], x1t[:, c * 128:(c + 1) * 128], [t_x1t], [tB[hb]])
                if hb == 0:
                    xf, t_xf = r_xf.get()
                cp(k, "act" if hb else "dve", xf[:, hb * 4:hb * 4 + 4, :], B[hb].rearrange("p (a b) -> p a b", b=128), [], [t_xf, tB[hb]])
            cp(k, "pool", xT[:, :, ti * 128:(ti + 1) * 128], xf, [t_xf], [t_xT[ti]])
            for c in range(8):
                mm(k, B[2][:, 0:36], xf[:, c, :], wr[:, c, :], [t_xf, t_c], [tB[2]], start=(c == 0), stop=(c == 7))
            lg, t_lg = sm.get()
            tt(k, "dve", lg[:, 0:36], B[2][:, 0:36], rb, ALU.add, [t_c], [t_lg, tB[2]])
            a, t_a = sm2.get()
            k.op("dve", lambda e, o_=a[:, 0:1], i_=lg[:, 0:4]: e.tensor_reduce(out=o_, in_=i_, axis=AX.X, op=ALU.max), [t_lg], [t_a])
            ts(k, "dve", a[:, 1:2], a[:, 0:1], -1.0, None, ALU.mult, None, [], [t_a])
            e4, t_e4 = sm3.get()
            act(k, e4[:, 0:4], lg[:, 0:4], AF.Exp, [t_lg, t_a], [t_e4, t_a], bias=a[:, 1:2], accum=a[:, 2:3])
            ohg, t_ohg = sm3.get()
            ts(k, "dve", ohg[:, 0:4], lg[:, 0:4], a[:, 0:1], None, ALU.is_equal, None, [t_lg, t_a], [t_ohg])
            ts(k, "dve", ohg[:, 0:4], ohg[:, 0:4], -1.0, 1e30, ALU.add, ALU.mult, [], [t_ohg])
            lem, t_lem = sm3.get()
            for gi in range(4):
                ts(k, "dve", lem[:, gi * 8:(gi + 1) * 8], lg[:, 4 + gi * 8:4 + (gi + 1) * 8], ohg[:, gi:gi + 1], None, ALU.add, None,
                   [t_lg, t_ohg], [t_lem])
            k.op("dve", lambda e, o_=a[:, 3:4], i_=lem: e.tensor_reduce(out=o_, in_=i_, axis=AX.X, op=ALU.max), [t_lem], [t_a])
            oh1, t_oh1 = sm3.get()
            ts(k, "dve", oh1, lem, a[:, 3:4], None, ALU.is_equal, None, [t_lem, t_a], [t_oh1])
            stt(k, "dve", lem, oh1, -1e30, lem, ALU.mult, ALU.add, [t_oh1], [t_lem])
            k.op("dve", lambda e, o_=a[:, 4:5], i_=lem: e.tensor_reduce(out=o_, in_=i_, axis=AX.X, op=ALU.max), [t_lem], [t_a])
            oh2, t_oh2 = sm3.get()
            ts(k, "dve", oh2, lem, a[:, 4:5], None, ALU.is_equal, None, [t_lem, t_a], [t_oh2])
            tt(k, "dve", a[:, 5:6], a[:, 4:5], a[:, 3:4], ALU.subtract, [], [t_a])
            act(k, a[:, 5:6], a[:, 5:6], AF.Exp, [], [t_a])
            ts(k, "dve", a[:, 6:7], a[:, 5:6], 1.0, None, ALU.add, None, [], [t_a])
            tt(k, "dve", a[:, 6:7], a[:, 6:7], a[:, 2:3], ALU.mult, [], [t_a])
            k.op("dve", lambda e, o_=a[:, 6:7]: e.reciprocal(out=o_, in_=o_), [], [t_a])
            tt(k, "dve", a[:, 7:8], a[:, 6:7], a[:, 5:6], ALU.mult, [], [t_a])
            ts(k, "dve", gates[:, ti, :], oh1, a[:, 6:7], None, ALU.mult, None, [t_oh1, t_a], [t_g[ti]])
            stt(k, "dve", gates[:, ti, :], oh2, a[:, 7:8], gates[:, ti, :], ALU.mult, ALU.add, [t_oh2, t_a], [t_g[ti]])
        r_wg, r_wu, r_wd = Rot(ar, [8, 256], 2, bf=True), Rot(ar, [8, 256], 2, bf=True), Rot(ar, [2, D], 2, bf=True)
        f_wg, f_wu, f_wd = Rot(ar, [8, 256], 2), Rot(ar, [8, 256], 2), Rot(ar, [2, D], 2)
        r_sg, r_h = Rot(ar, [512], 2), Rot(ar, [2, 512], 2, bf=True)
        bi = 0
        for ex in range(32):
            wg, t_wg = r_wg.get()
            wu, t_wu = r_wu.get()
            wd, t_wd = r_wd.get()
            fg, t_fg = f_wg.get()
            fu, t_fu = f_wu.get()
            fd, t_fd = f_wd.get()
            k.dma("sp", fg, W["moe_w_gate"][l, ex].rearrange("(c p) h -> p c h", p=128), writes=[t_fg])
            k.dma("sp", fu, W["moe_w_up"][l, ex].rearrange("(c p) h -> p c h", p=128), writes=[t_fu])
            k.dma("sp", fd, W["moe_w_down"][l, ex].rearrange("(c p) d -> p c d", p=128), writes=[t_fd])
            cp(k, "pool", wg, fg, [t_fg], [t_wg])
            cp(k, "pool", wu, fu, [t_fu], [t_wu])
            cp(k, "act", wd, fd, [t_fd], [t_wd])
            for tb in range(2):
                hT, t_h = r_h.get()
                for hc in range(2):
                    for c in range(8):
                        mm(k, B[0], wg[:, c, hc * 128:(hc + 1) * 128], xT[:, c, tb * 512:(tb + 1) * 512], [t_wg] + t_xT[tb * 4:tb * 4 + 4], [tB[0]],
                           start=(c == 0), stop=(c == 7))
                    for c in range(8):
                        mm(k, B[1], wu[:, c, hc * 128:(hc + 1) * 128], xT[:, c, tb * 512:(tb + 1) * 512], [t_wu] + t_xT[tb * 4:tb * 4 + 4], [tB[1]],
                           start=(c == 0), stop=(c == 7))
                    sg, t_sg = r_sg.get()
                    act(k, sg, B[0], AF.Silu, [], [t_sg, tB[0]])
                    tt(k, "dve", hT[:, hc, :], B[1], sg, ALU.mult, [t_sg], [t_h, tB[1]])
                for q4 in range(4):
                    ti = tb * 4 + q4
                    for dh in range(2):
                        bk = 2 + bi % 6
                        bi += 1
                        for hc in range(2):
                            mm(k, B[bk], hT[:, hc, q4 * 128:(q4 + 1) * 128], wd[:, hc, dh * 512:(dh + 1) * 512], [t_h, t_wd], [tB[bk]],
                               start=(hc == 0), stop=(hc == 1))
                        ysl = yacc[:, ti, dh * 512:(dh + 1) * 512]
                        if ex == 0:
                            ts(k, "dve", ysl, B[bk], gates[:, ti, ex:ex + 1], None, ALU.mult, None, [t_g[ti]], [t_ya[ti], tB[bk]])
                        else:
                            stt(k, "dve", ysl, B[bk], gates[:, ti, ex:ex + 1], ysl, ALU.mult, ALU.add, [t_g[ti]], [t_ya[ti], tB[bk]])
        r_out = Rot(ar, [D], 2)
        for ti in range(8):
            r0 = s * SEQ + half * 1024 + ti * 128
            x1t, t_x1t = r_x1.get()
            k.dma("sp", x1t, g.X1[half * 1024 + ti * 128:half * 1024 + (ti + 1) * 128, :], reads=[g.t_X1], writes=[t_x1t])
            stt(k, "dve", yacc[:, ti, :], x1t, DN_ALPHA, yacc[:, ti, :], ALU.mult, ALU.add, [t_x1t], [t_ya[ti]])
            o, t_o = r_out.get()
            ln_tile(k, yacc[:, ti, :], t_ya[ti], gbc, bbc, t_c, sm2, o, t_o)
            k.dma("pool", g.out[r0:r0 + 128, :], o, reads=[t_o], writes=[g.t_out])


def bc_last(ap, n):
    return bass.AP(ap.tensor, ap.offset, [list(x) for x in ap.ap] + [[0, n]])


def v3(ap, inner):
    return ap.rearrange("p (a b) -> p a b", b=inner)


def stage_d(g, l):
    for _ in gen_d(g, l):
        pass


def gen_d(g, l, fresh=True, banks=tuple(range(8))):
    k, ar = g.k, g.ar
    if fresh:
        k.barrier()
        ar.reset()
    W = g.w
    B, tB = g.banks, g.t_bank
    st = {"b": 0}

    def nb():
        st["b"] = (st["b"] + 1) % len(banks)
        return banks[st["b"]]
    t_c = T()
    mu = ar.alloc([832])
    wcat = ar.alloc([768])
    vec = {n: ar.alloc([256]) for n in ("d_w0", "d_a0", "d_k_k", "d_k_a", "d_r_k", "d_gn_w", "d_gn_b")}
    tri_i, tri_s, tril_s, ones, idn = ar.alloc([128]), ar.alloc([128]), ar.alloc([128]), ar.alloc([128]), g.ident
    k.dma("sp", mu, bcast_rows(W["d_mu"][l], 832), writes=[t_c])
    k.op("dve", lambda e: e.memset(wcat, 0.0), [], [t_c])
    k.dma("sp", wcat[0:16, 0:256], W["d_w2"][l], writes=[t_c])
    k.dma("sp", wcat[16:32, 256:512], W["d_a2"][l], writes=[t_c])
    k.dma("sp", wcat[32:64, 512:768], W["d_g2"][l], writes=[t_c])
    for n, a in vec.items():
        k.dma("sp", a, bcast_rows(W[n][l], 256), writes=[t_c])
    k.dma("sp", tri_i, g.c["tri_incl"], writes=[t_c])
    k.dma("sp", tri_s, g.c["tri_strict"], writes=[t_c])
    k.dma("sp", tril_s, g.c["tril_strict"], writes=[t_c])
    k.dma("sp", ones, g.c["ones"], writes=[t_c])
    S = ar.alloc([256])
    t_S = T()
    k.op("dve", lambda e: e.memset(S, 0.0), [], [t_S])
    R2 = lambda w: Rot(ar, [w], 2)
    r_cur, r_prev, r_seg, r_z, r_zT = R2(832), R2(832), R2(832), R2(64), R2(128)
    r_nlw, r_a, r_g, r_kk, r_kp, r_sm, r_tmp = R2(256), R2(256), R2(256), R2(256), R2(256), Rot(ar, [16], 4), Rot(ar, [256], 4)
    r_en, r_ep, r_ex = R2(256), R2(256), R2(256)
    r_at, r_bt, r_kt, r_rt = R2(256), R2(256), R2(256), R2(256)
    r_FT = Rot(ar, [4, 512], 2)
    r_wT = R2(4)
    r_M = Rot(ar, [5, 512], 1)
    r_P, r_PT, r_XT = Rot(ar, [512], 2), Rot(ar, [512], 2), Rot(ar, [512], 2)
    r_rhs0, r_U, r_y, r_o = R2(256), R2(256), R2(256), R2(256)
    yield
    for tt_ in range(NT):
        r0 = tt_ * 128
        cur, t_cur = r_cur.get()
        prev, t_prev = r_prev.get()
        k.dma("sp", cur, g.P[r0:r0 + 128, O_D:O_D + 832], reads=[g.t_P], writes=[t_cur])
        if tt_ == 0:
            k.op("dve", lambda e, o_=prev[0:1, :]: e.memset(o_, 0.0), [], [t_prev])
            k.dma("sp", prev[1:128, :], g.P[0:127, O_D:O_D + 832], reads=[g.t_P], writes=[t_prev])
        else:
            k.dma("sp", prev, g.P[r0 - 1:r0 + 127, O_D:O_D + 832], reads=[g.t_P], writes=[t_prev])
        seg, t_seg = r_seg.get()
        tt(k, "pool", prev, prev, cur, ALU.subtract, [t_cur], [t_prev])
        tt(k, "pool", prev, prev, mu, ALU.mult, [t_c], [t_prev])
        tt(k, "dve", seg, cur, prev, ALU.add, [t_cur, t_prev], [t_seg])
        r, kraw, v = seg[:, 0:256], seg[:, 256:512], seg[:, 512:768]
        z, t_z = r_z.get()
        act(k, z[:, 0:16], seg[:, 768:784], AF.Tanh, [t_seg], [t_z])
        cp(k, "dve", z[:, 16:32], seg[:, 784:800], [t_seg], [t_z])
        act(k, z[:, 32:64], seg[:, 800:832], AF.Sigmoid, [t_seg], [t_z])
        b0 = nb()
        tp(g, B[b0][0:64, 0:128], z, [t_z], [tB[b0]])
        zT, t_zT = r_zT.get()
        cp(k, "act", zT[0:64, :], B[b0][0:64, 0:128], [], [t_zT, tB[b0]])
        bw, bg_ = nb(), nb()
        mm(k, B[bw], zT[0:64, :], wcat[0:64, 0:512], [t_zT, t_c], [tB[bw]])
        mm(k, B[bg_][:, 0:256], zT[0:64, :], wcat[0:64, 512:768], [t_zT, t_c], [tB[bg_]])
        nlw, t_nlw = r_nlw.get()
        tt(k, "dve", nlw, B[bw][:, 0:256], vec["d_w0"], ALU.add, [t_c], [t_nlw, tB[bw]])
        a, t_a = r_a.get()
        tt(k, "dve", a, B[bw][:, 256:512], vec["d_a0"], ALU.add, [t_c], [t_a, tB[bw]])
        gsb, t_gs = r_g.get()
        cp(k, "act", gsb, B[bg_][:, 0:256], [], [t_gs, tB[bg_]])
        act(k, nlw, nlw, AF.Exp, [], [t_nlw], scale=-1.0)
        act(k, nlw, nlw, AF.Ln, [], [t_nlw], bias=1.0)
        act(k, nlw, nlw, AF.Exp, [], [t_nlw], scale=-1.0, bias=g.cm05[:, 0:1])
        act(k, a, a, AF.Sigmoid, [], [t_a])
        yield
        kk, t_kk = r_kk.get()
        tt(k, "pool", kk, kraw, vec["d_k_k"], ALU.mult, [t_seg, t_c], [t_kk])
        tmp, t_tmp = r_tmp.get()
        tt(k, "pool", tmp, kk, kk, ALU.mult, [t_kk], [t_tmp])
        sm, t_sm = r_sm.get()
        k.op("dve", lambda e, o_=sm[:, 0:4], i_=v3(tmp, 64): e.tensor_reduce(out=o_, in_=i_, axis=AX.X, op=ALU.add), [t_tmp], [t_sm])
        k.op("act", lambda e, o_=sm[:, 0:4]: e.activation(out=o_, in_=o_, func=AF.Sqrt), [], [t_sm])
        ts(k, "dve", sm[:, 0:4], sm[:, 0:4], 1e-12, None, ALU.max, None, [], [t_sm])
        k.op("dve", lambda e, o_=sm[:, 0:4]: e.reciprocal(out=o_, in_=o_), [], [t_sm])
        tt(k, "dve", v3(kk, 64), v3(kk, 64), bc_last(sm[:, 0:4], 64), ALU.mult, [t_sm], [t_kk])
        kp, t_kp = r_kp.get()
        stt(k, "dve", kp, a, -1.0, vec["d_k_a"], ALU.add, ALU.mult, [t_a, t_c], [t_kp])
        stt(k, "dve", kp, kp, 1.0, kraw, ALU.add, ALU.mult, [t_seg], [t_kp])
        yield
        bc = nb()
        mm(k, B[bc][:, 0:256], tri_i, nlw, [t_c, t_nlw], [tB[bc]])
        for h in range(4):
            mm(k, B[bc][0:64, 256 + h:257 + h], nlw[:, h * 64:(h + 1) * 64], ones[:, 0:1], [t_nlw, t_c], [tB[bc]])
        en, t_en = r_en.get()
        ep, t_ep = r_ep.get()
        ex, t_ex = r_ex.get()
        wT, t_wT = r_wT.get()
        act(k, en, B[bc][:, 0:256], AF.Exp, [], [t_en, tB[bc]], scale=-1.0)
        act(k, ep, B[bc][:, 0:256], AF.Exp, [], [t_ep, tB[bc]])
        tt(k, "dve", ex, B[bc][:, 0:256], nlw, ALU.subtract, [t_nlw], [t_ex, tB[bc]])
        act(k, wT[0:64, :], B[bc][0:64, 256:260], AF.Exp, [], [t_wT, tB[bc]], scale=-1.0)
        act(k, ex, ex, AF.Exp, [], [t_ex], scale=-1.0)
        at, t_at = r_at.get()
        bt, t_bt = r_bt.get()
        kt, t_kt = r_kt.get()
        rt, t_rt = r_rt.get()
        stt(k, "dve", at, kk, -1.0, ex, ALU.mult, ALU.mult, [t_kk, t_ex], [t_at])
        tt(k, "pool", bt, kk, a, ALU.mult, [t_kk, t_a], [t_bt])
        tt(k, "pool", bt, bt, ep, ALU.mult, [t_ep], [t_bt])
        tt(k, "dve", kt, kp, ep, ALU.mult, [t_kp, t_ep], [t_kt])
        tt(k, "pool", rt, r, en, ALU.mult, [t_seg, t_en], [t_rt])
        yield
        FT, t_FT = r_FT.get()
        for qi, (src, t_src) in enumerate(((at, t_at), (bt, t_bt), (kt, t_kt), (rt, t_rt))):
            bq = nb()
            for h in range(4):
                tp(g, B[bq][0:64, h * 128:(h + 1) * 128], src[:, h * 64:(h + 1) * 64], [t_src], [tB[bq]])
            cp(k, "act" if qi % 2 else "dve", FT[0:64, qi, :], B[bq][0:64, :], [], [t_FT, tB[bq]])
        aT = lambda h: FT[0:64, 0, h * 128:(h + 1) * 128]
        bT = lambda h: FT[0:64, 1, h * 128:(h + 1) * 128]
        kT = lambda h: FT[0:64, 2, h * 128:(h + 1) * 128]
        rT = lambda h: FT[0:64, 3, h * 128:(h + 1) * 128]
        yield
        M, t_M = r_M.get()
        specs = ((bT, aT, tri_s), (aT, bT, tril_s), (kT, aT, tri_s), (bT, rT, tri_i), (kT, rT, tri_i))
        for mi, (lf, rf, msk) in enumerate(specs):
            bq = nb()
            for h in range(4):
                mm(k, B[bq][:, h * 128:(h + 1) * 128], lf(h), rf(h), [t_FT], [tB[bq]])
            for h in range(4):
                tt(k, "dve", M[:, mi, h * 128:(h + 1) * 128], B[bq][:, h * 128:(h + 1) * 128], msk, ALU.mult, [t_c], [t_M, tB[bq]])
        LT, L, LakT, ArbT, ArkT = (M[:, i, :] for i in range(5))
        yield
        XT, t_XT = r_XT.get()
        for h in range(4):
            tt(k, "pool", XT[:, h * 128:(h + 1) * 128], LT[:, h * 128:(h + 1) * 128], idn, ALU.add, [t_M, g.t_ident], [t_XT])
        P_, t_P_ = L, t_M
        PT_, t_PT_ = LT, t_M
        for step in range(1, 7):
            b1 = nb()
            for h in range(4):
                hs = slice(h * 128, (h + 1) * 128)
                mm(k, B[b1][:, hs], PT_[:, hs], P_[:, hs], [t_P_, t_PT_], [tB[b1]])
            Pn, t_Pn = r_P.get()
            cp(k, "act", Pn, B[b1], [], [t_Pn, tB[b1]])
            if step < 6:
                b2 = nb()
                for h in range(4):
                    hs = slice(h * 128, (h + 1) * 128)
                    mm(k, B[b2][:, hs], P_[:, hs], PT_[:, hs], [t_P_, t_PT_], [tB[b2]])
                PTn, t_PTn = r_PT.get()
                cp(k, "dve", PTn, B[b2], [], [t_PTn, tB[b2]])
            b3 = nb()
            for h in range(4):
                hs = slice(h * 128, (h + 1) * 128)
                mm(k, B[b3][:, hs], Pn[:, hs], XT[:, hs], [t_Pn, t_XT], [tB[b3]])
            XTn, t_XTn = r_XT.get()
            tt(k, "dve", XTn, XT, B[b3], ALU.add, [t_XT], [t_XTn, tB[b3]])
            XT, t_XT = XTn, t_XTn
            yield
            P_, t_P_ = Pn, t_Pn
            if step < 6:
                PT_, t_PT_ = PTn, t_PTn
        yield
        b1 = nb()
        for h in range(4):
            vs = slice(h * 64, (h + 1) * 64)
            mm(k, B[b1][:, vs], aT(h), S[0:64, vs], [t_FT, t_S], [tB[b1]], start=True, stop=False)
            mm(k, B[b1][:, vs], LakT[:, h * 128:(h + 1) * 128], v[:, vs], [t_M, t_seg], [tB[b1]], start=False, stop=True)
        rhs0, t_rhs0 = r_rhs0.get()
        cp(k, "act", rhs0, B[b1][:, 0:256], [], [t_rhs0, tB[b1]])
        b2 = nb()
        for h in range(4):
            vs = slice(h * 64, (h + 1) * 64)
            mm(k, B[b2][:, vs], XT[:, h * 128:(h + 1) * 128], rhs0[:, vs], [t_XT, t_rhs0], [tB[b2]])
        U, t_U = r_U.get()
        cp(k, "dve", U, B[b2][:, 0:256], [], [t_U, tB[b2]])
        yield
        b3 = nb()
        for h in range(4):
            vs = slice(h * 64, (h + 1) * 64)
            hs = slice(h * 128, (h + 1) * 128)
            mm(k, B[b3][:, vs], rT(h), S[0:64, vs], [t_FT, t_S], [tB[b3]], start=True, stop=False)
            mm(k, B[b3][:, vs], ArbT[:, hs], U[:, vs], [t_M, t_U], [tB[b3]], start=False, stop=False)
            mm(k, B[b3][:, vs], ArkT[:, hs], v[:, vs], [t_M, t_seg], [tB[b3]], start=False, stop=True)
        y, t_y = r_y.get()
        cp(k, "act", y, B[b3][:, 0:256], [], [t_y, tB[b3]])
        yield
        b4 = nb()
        for h in range(4):
            vs = slice(h * 64, (h + 1) * 64)
            mm(k, B[b4][0:64, vs], bt[:, vs], U[:, vs], [t_bt, t_U], [tB[b4]], start=True, stop=False)
            mm(k, B[b4][0:64, vs], kt[:, vs], v[:, vs], [t_kt, t_seg], [tB[b4]], start=False, stop=True)
        tt(k, "dve", S[0:64, :], S[0:64, :], B[b4][0:64, 0:256], ALU.add, [], [t_S, tB[b4]])
        tt(k, "dve", v3(S[0:64, :], 64), v3(S[0:64, :], 64), bc_last(wT[0:64, :], 64), ALU.mult, [t_wT], [t_S])
        yield
        sm2, t_sm2 = r_sm.get()
        k.op("dve", lambda e, o_=sm2[:, 0:4], i_=v3(y, 64): e.tensor_reduce(out=o_, in_=i_, axis=AX.X, op=ALU.add), [t_y], [t_sm2])
        ts(k, "dve", sm2[:, 0:4], sm2[:, 0:4], -1.0 / 64, None, ALU.mult, None, [], [t_sm2])
        tt(k, "dve", v3(y, 64), v3(y, 64), bc_last(sm2[:, 0:4], 64), ALU.add, [t_sm2], [t_y])
        tmp2, t_tmp2 = r_tmp.get()
        tt(k, "pool", tmp2, y, y, ALU.mult, [t_y], [t_tmp2])
        k.op("dve", lambda e, o_=sm2[:, 4:8], i_=v3(tmp2, 64): e.tensor_reduce(out=o_, in_=i_, axis=AX.X, op=ALU.add), [t_tmp2], [t_sm2])
        rsqrt(k, sm2[:, 4:8], sm2[:, 4:8], 1.0 / 64, 64e-5, [], [t_sm2])
        tt(k, "dve", v3(y, 64), v3(y, 64), bc_last(sm2[:, 4:8], 64), ALU.mult, [t_sm2], [t_y])
        tt(k, "pool", y, y, vec["d_gn_w"], ALU.mult, [t_c], [t_y])
        tt(k, "pool", y, y, vec["d_gn_b"], ALU.add, [t_c], [t_y])
        tmp3, t_tmp3 = r_tmp.get()
        tt(k, "dve", tmp3, r, kp, ALU.mult, [t_seg, t_kp], [t_tmp3])
        tt(k, "dve", tmp3, tmp3, vec["d_r_k"], ALU.mult, [t_c], [t_tmp3])
        k.op("dve", lambda e, o_=sm2[:, 8:12], i_=v3(tmp3, 64): e.tensor_reduce(out=o_, in_=i_, axis=AX.X, op=ALU.add), [t_tmp3], [t_sm2])
        tt(k, "dve", v3(tmp3, 64), v3(v, 64), bc_last(sm2[:, 8:12], 64), ALU.mult, [t_seg, t_sm2], [t_tmp3])
        tt(k, "pool", y, y, tmp3, ALU.add, [t_tmp3], [t_y])
        o, t_o = r_o.get()
        tt(k, "dve", o, y, gsb, ALU.mult, [t_y, t_gs], [t_o])
        k.dma("pool", g.Y[r0:r0 + 128, 768:1024], o, reads=[t_o], writes=[g.t_Y])


def bc_mid(ap, n):
    a = [list(x) for x in ap.ap]
    return bass.AP(ap.tensor, ap.offset, [a[0], [0, n]] + a[1:])


def stage_b(g, l):
    k, ar = g.k, g.ar
    k.barrier()
    ar.reset()
    W = g.w
    B, tB = g.banks, g.t_bank
    t_c = T()
    gain = ar.alloc([64])
    wuv = ar.alloc([256])
    cneg = ar.alloc([128])
    k.dma("sp", gain, bcast_rows(W["b_kv_gain"][l], 64), writes=[t_c])
    k.dma("sp", v3(wuv[0:64, :], 64), W["b_w_uv"][l].rearrange("h c d -> c h d"), writes=[t_c])
    k.dma("sp", cneg, g.c["caus_neg"], writes=[t_c])
    ckv = ar.alloc([NT, 64])
    t_ckv = T()
    k.dma("sp", ckv, g.P[:, O_CKV:O_CKV + 64].rearrange("(t p) c -> p t c", p=128), reads=[g.t_P], writes=[t_ckv])
    iw = ar.alloc([NT, 8])
    t_iw = T()
    k.dma("sp", iw, g.P[:, O_IW:O_IW + 8].rearrange("(t p) c -> p t c", p=128), reads=[g.t_P], writes=[t_iw])
    ikT = ar.alloc([SEQ])
    t_ik = T()
    k.dma("sp", ikT[0:32, :], g.PT[O_IK:O_IK + 32, :], reads=[g.t_PT], writes=[t_ik])
    sq = ar.alloc([NT, 64])
    t_sq = T()
    ss = ar.alloc([NT])
    tt(k, "pool", sq, ckv, ckv, ALU.mult, [t_ckv], [t_sq])
    k.op("dve", lambda e: e.tensor_reduce(out=ss, in_=sq, axis=AX.X, op=ALU.add), [t_sq], [t_sq])
    rsqrt(k, ss, ss, 1.0 / 64, 1e-6, [], [t_sq])
    tt(k, "dve", ckv, ckv, bc_last(ss, 64), ALU.mult, [t_sq], [t_ckv])
    tt(k, "dve", ckv, ckv, bc_mid(gain, NT), ALU.mult, [t_c], [t_ckv])
    ckvT = ar.alloc_bf([SEQ])
    ckvTf = ar.alloc([SEQ])
    t_cT = T()
    for j4 in range(4):
        for jj in range(4):
            j = j4 * 4 + jj
            tp(g, B[j4][0:64, jj * 128:(jj + 1) * 128], ckv[:, j, :], [t_ckv], [tB[j4]])
        cp(k, "act" if j4 % 2 else "dve", ckvTf[0:64, j4 * 512:(j4 + 1) * 512], B[j4][0:64, :], [], [t_cT, tB[j4]])
        cp(k, "pool", ckvT[0:64, j4 * 512:(j4 + 1) * 512], ckvTf[0:64, j4 * 512:(j4 + 1) * 512], [], [t_cT])
    vaug = ar.alloc_bf([NT, 4, 66])
    t_v = T()
    k.op("dve", lambda e: e.memset(vaug[:, :, :, 64:65], 1.0), [], [t_v])
    for j in range(NT):
        bk = 4 + j % 4
        mm(k, B[bk][:, 0:256], ckvTf[0:64, j * 128:(j + 1) * 128], wuv[0:64, :], [t_cT, t_c], [tB[bk]])
        cp(k, "act" if j % 2 else "dve", vaug[:, j, :, 0:64], v3(B[bk][:, 0:256], 64), [], [t_v, tB[bk]])
    ysb = ar.alloc([NT, 256])
    t_y = T()
    iqT = ar.alloc([8, 512])
    t_iq = T()
    qT = ar.alloc_bf([4, 512])
    qTf = ar.alloc([4, 512])
    t_q = T()
    t_qf = T()
    strips = ar.alloc([2816])
    t_st = T()
    MTs = [(ar.alloc_bf([NT, 512]), T()) for _ in range(2)]
    r_sc, r_wk, r_tmp, r_m8 = Rot(ar, [SEQ], 2), Rot(ar, [SEQ], 1), Rot(ar, [512], 3), Rot(ar, [8], 2)
    st = {"sb": 0, "pt": Rot(ar, [512], 4, bf=True), "rd": Rot(ar, [1], 4)}
    cnt = {"bi": 0}

    def prep_yields(I):
        n = 1
        for qi in range(4):
            i = 4 * I + qi
            n += 8 + (16 if i >= 2 else 0) + 1
        return n

    def prep(I):
        MT, t_MT = MTs[I % 2]
        for ih in range(8):
            k.dma("sp", iqT[0:32, ih, :], g.PT[O_IQ + ih * 32:O_IQ + (ih + 1) * 32, I * 512:(I + 1) * 512], reads=[g.t_PT], writes=[t_iq])
        k.op("pool", lambda e: e.memset(MT, 0.0), [], [t_MT])
        yield
        for qi in range(4):
            i = 4 * I + qi
            nk = (i + 1) * 128
            sc, t_sc = r_sc.get()
            for ih in range(8):
                for kb in range(0, nk, 512):
                    n = min(512, nk - kb)
                    bk = 2 + cnt["bi"] % 2
                    cnt["bi"] += 1
                    mm(k, B[bk][:, 0:n], iqT[0:32, ih, qi * 128:(qi + 1) * 128], ikT[0:32, kb:kb + n], [t_iq, t_ik], [tB[bk]])
                    if ih == 0:
                        act(k, sc[:, kb:kb + n], B[bk][:, 0:n], AF.Relu, [], [t_sc, tB[bk]])
                        ts(k, "dve", sc[:, kb:kb + n], sc[:, kb:kb + n], iw[:, i, 0:1], None, ALU.mult, None, [t_iw], [t_sc])
                    else:
                        tmp, t_tmp = r_tmp.get()
                        act(k, tmp[:, 0:n], B[bk][:, 0:n], AF.Relu, [], [t_tmp, tB[bk]])
                        stt(k, "dve", sc[:, kb:kb + n], tmp[:, 0:n], iw[:, i, ih:ih + 1], sc[:, kb:kb + n], ALU.mult, ALU.add,
                            [t_tmp, t_iw], [t_sc])
                yield
            tt(k, "dve", sc[:, i * 128:nk], sc[:, i * 128:nk], cneg, ALU.add, [t_c], [t_sc])
            m8, t_m8 = r_m8.get()
            if i >= 2:
                wk, t_wk = r_wk.get()
                cp(k, "pool", wk[:, 0:nk], sc[:, 0:nk], [t_sc], [t_wk])
                for rnd in range(32):
                    k.op("dve", lambda e, o_=m8, i_=wk[:, 0:nk]: e.max(out=o_, in_=i_), [t_wk], [t_m8])
                    if rnd < 31:
                        k.op("dve", lambda e, o_=wk[:, 0:nk], r_=m8: e.match_replace(out=o_, in_to_replace=r_, in_values=o_, imm_value=-1e30),
                             [t_m8], [t_wk])
                    if rnd % 2 == 1:
                        yield
            else:
                k.op("dve", lambda e, o_=m8: e.memset(o_, -1e29), [], [t_m8])
            thr = m8[:, 7:8]
            ts(k, "dve", sc[:, 0:nk], sc[:, 0:nk], thr, None, ALU.is_ge, None, [t_m8], [t_sc])
            for j4 in range(0, i + 1, 4):
                nj = min(4, i + 1 - j4)
                bk = 2 + cnt["bi"] % 2
                cnt["bi"] += 1
                for jj in range(nj):
                    j = j4 + jj
                    tp(g, B[bk][:, jj * 128:(jj + 1) * 128], sc[:, j * 128:(j + 1) * 128], [t_sc], [tB[bk]])
                cp(k, "act", MT[:, j4:j4 + nj, qi * 128:(qi + 1) * 128], v3(B[bk][:, 0:nj * 128], 128), [], [t_MT, tB[bk]])
            yield

    for _ in prep(0):
        pass
    for I in range(4):
        MT, t_MT = MTs[I % 2]
        nxt = prep(I + 1) if I < 3 else None
        npairs = (4 * I + 4) * 4
        step = -(-prep_yields(I + 1) // npairs) if nxt is not None else 0
        stn = {"g": nxt}

        def after_pair(stn=stn, step=step):
            for _ in range(step):
                if stn["g"] is not None:
                    try:
                        next(stn["g"])
                    except StopIteration:
                        stn["g"] = None
        for h in range(4):
            k.dma("sp", qTf[0:64, h, :], g.PT[O_BQ + h * 64:O_BQ + (h + 1) * 64, I * 512:(I + 1) * 512], reads=[g.t_PT], writes=[t_qf])
        cp(k, "act", qT[0:64, :, :], qTf[0:64, :, :], [t_qf], [t_q])
        for h in range(4):
            k.dma("sp", strips, g.G[4 + h][:, 127:127 + 2816], reads=[g.t_G], writes=[t_st])
            attn_block(g, I, ckvT, t_cT, qT[0:64, h, :], t_q, strips, t_st, lambda j, h=h: vaug[:, j, h, 0:65], t_v, 64, ysb, t_y, h * 64, st,
                       mask=MT, t_mask=t_MT, after_pair=after_pair)
        if stn["g"] is not None:
            for _ in stn["g"]:
                pass
    k.dma("pool", g.Y[:, 256:512].rearrange("(t p) c -> p t c", p=128), ysb, reads=[t_y], writes=[g.t_Y])


_CACHE = {}


def kernel(**inputs):
    if "nc" not in _CACHE:
        _CACHE["nc"] = build(nlayers=DEPTH, nseq=4, stages=("P", "C", "A", "D", "B", "M", "E"))
    nc, consts = _CACHE["nc"]
    inputs = {n: np.asarray(a, dtype=np.float32) for n, a in inputs.items()}
    in_maps = [make_inputs(inputs, consts, c) for c in range(NCORES)]
    res = run_bass_kernel_spmd(nc, in_maps, core_ids=list(range(NCORES)))
    out = np.concatenate([np.asarray(r["out"]).reshape(4, SEQ, D) for r in res.results], axis=0)
    return out.astype(np.float32)
```

```python
import math
from contextlib import ExitStack
import numpy as np
import concourse.bass as bass
import concourse.mybir as mybir
from concourse.bass_utils import run_bass_kernel_spmd

F32 = mybir.dt.float32
BF16 = mybir.dt.bfloat16
AF = mybir.ActivationFunctionType
ALU = mybir.AluOpType
AX = mybir.AxisListType

NCORES = 8
D = 1024
SEQ = 2048
DEPTH = 4
NT = SEQ // 128
INW = 7096
BW = 256
DN_ALPHA = (2 * DEPTH) ** 0.25
LN_EPS = 1e-5
O_AQ, O_AK, O_AV, O_BQ, O_CKV, O_IQ, O_IK, O_IW = 0, 256, 512, 768, 1024, 1088, 1344, 1376
O_CQ, O_CK, O_CV, O_CA, O_CG, O_D, O_GATE = 1384, 1512, 1640, 1896, 1912, 2168, 3000
import os
CCUT = int(os.environ.get('CCUT', '0'))
NDS = 24
NE = 2048 + 128
WSHAPES = {
    "rpb_table": [32, 8], "b_kv_gain": [4, 64], "b_w_uv": [4, 4, 64, 64], "c_a_up": [4, 16, 128], "c_a_bias": [4, 128],
    "c_norm_gain": [4, 256], "d_mu": [4, 832], "d_w0": [4, 256], "d_w2": [4, 16, 256], "d_a0": [4, 256], "d_a2": [4, 16, 256],
    "d_g2": [4, 32, 256], "d_k_k": [4, 256], "d_k_a": [4, 256], "d_r_k": [4, 256], "d_gn_w": [4, 256], "d_gn_b": [4, 256],
    "w_branch": [4, 4, 256, 1024], "w_out": [4, 1024, 1024], "ln_g": [4, 2, 1024], "ln_b": [4, 2, 1024],
    "router_g": [4, 1024, 4], "router_g_bias": [4, 4], "router_e": [4, 1024, 32], "router_e_bias": [4, 32],
    "moe_w_gate": [4, 32, 1024, 256], "moe_w_up": [4, 32, 1024, 256], "moe_w_down": [4, 32, 256, 1024],
}


class T:
    __slots__ = ("lw", "rd")

    def __init__(self):
        self.lw = None
        self.rd = {}


class K:
    ENG = ("pe", "act", "dve", "pool", "sp")

    def __init__(self, nc, es):
        self.nc = nc
        self.prog = {e: [] for e in self.ENG}
        self.sem = {}
        self.cnt = {}
        for e in self.ENG:
            self.sem[e] = es.enter_context(nc.semaphore("s_" + e))
            self.cnt[e] = 0
        self.known = {e: {} for e in self.ENG}
        self.dq = {}
        self.dqi = {}
        for q in ("sp", "pool", "act"):
            self.dq[q] = []
            self.dqi[q] = 0
            for i in range(NDS):
                key = "d_%s%d" % (q, i)
                self.sem[key] = es.enter_context(nc.semaphore(key))
                self.cnt[key] = 0
                self.dq[q].append(key)

    def _deps(self, reads, writes):
        d = {}
        for r in reads:
            if r.lw is not None:
                k, v = r.lw
                if d.get(k, 0) < v:
                    d[k] = v
        for w in writes:
            if w.lw is not None:
                k, v = w.lw
                if d.get(k, 0) < v:
                    d[k] = v
            for k, v in w.rd.items():
                if d.get(k, 0) < v:
                    d[k] = v
        return d

    def _wait(self, e, d):
        kn = self.known[e]
        for k, v in d.items():
            if k == e and e == "pe":
                continue
            if kn.get(k, 0) < v:
                self.prog[e].append(("w", k, v))
                kn[k] = v

    def op(self, e, fn, reads=(), writes=()):
        self._wait(e, self._deps(reads, writes))
        self.cnt[e] += 1
        c = self.cnt[e]
        self.prog[e].append(("o", fn, e, 1))
        for w in writes:
            w.lw = (e, c)
            w.rd = {}
        for r in reads:
            if r not in writes:
                r.rd[e] = c

    def dma(self, q, out_ap, in_ap, reads=(), writes=(), **kw):
        i = self.dqi[q]
        self.dqi[q] = (i + 1) % NDS
        key = self.dq[q][i]
        d = self._deps(reads, writes)
        if self.cnt[key] > 0 and d.get(key, 0) < self.cnt[key]:
            d[key] = self.cnt[key]
        self._wait(q, d)
        self.cnt[key] += 16
        c = self.cnt[key]
        self.prog[q].append(("o", lambda eng: eng.dma_start(out=out_ap, in_=in_ap, **kw), key, 16))
        for w in writes:
            w.lw = (key, c)
            w.rd = {}
        for r in reads:
            r.rd[key] = c

    def barrier(self):
        d = {k: v for k, v in self.cnt.items() if v > 0}
        for e in self.ENG:
            self._wait(e, d)

    def replay(self):
        nc = self.nc
        with nc.Block() as block:
            def mk(e):
                prog = self.prog[e]
                sem = self.sem

                def body(eng):
                    for it in prog:
                        if it[0] == "w":
                            eng.wait_ge(sem[it[1]], it[2])
                        else:
                            it[1](eng).then_inc(sem[it[2]], it[3])
                return body
            block.tensor(mk("pe"))
            block.scalar(mk("act"))
            block.vector(mk("dve"))
            block.gpsimd(mk("pool"))
            block.sync(mk("sp"))


class Arena:
    def __init__(self, ap, words):
        self.ap = ap
        self.words = words
        self.off = 0
        self.base = 0

    def alloc(self, shape):
        n = int(np.prod(shape))
        assert self.off + n <= self.words, ("arena overflow", self.off, n, self.words)
        v = self.ap[:, self.off:self.off + n]
        self.off += n
        if len(shape) == 2:
            v = v.rearrange("p (a b) -> p a b", b=shape[1])
        elif len(shape) == 3:
            v = v.rearrange("p (a b c) -> p a b c", b=shape[1], c=shape[2])
        return v

    def alloc_bf(self, shape):
        n = int(np.prod(shape))
        assert n % 2 == 0 and self.off + n // 2 <= self.words, ("arena overflow", self.off, n, self.words)
        v = self.ap[:, self.off:self.off + n // 2].bitcast(BF16)
        self.off += n // 2
        if len(shape) == 2:
            v = v.rearrange("p (a b) -> p a b", b=shape[1])
        elif len(shape) == 3:
            v = v.rearrange("p (a b c) -> p a b c", b=shape[1], c=shape[2])
        return v

    def mark(self):
        self.base = self.off

    def reset(self):
        self.off = self.base


def host_consts():
    c = {}
    c["ident"] = np.eye(128, dtype=np.float32)
    s = np.arange(128)
    c["tri_incl"] = (s[:, None] <= s[None, :]).astype(np.float32)
    c["tri_strict"] = (s[:, None] < s[None, :]).astype(np.float32)
    c["ones"] = np.ones((128, 128), np.float32)
    c["tril_strict"] = (s[:, None] > s[None, :]).astype(np.float32)
    c["m05"] = np.full((128, 1), -0.5, np.float32)
    c["m30k"] = np.full((128, 1), -30000.0, np.float32)
    c["caus_neg"] = np.where(s[None, :] > s[:, None], -1e30, 0.0).astype(np.float32)
    hc = np.arange(128) // 32
    hv = np.arange(256) // 64
    c["bm_c"] = (hc[:, None] == hv[None, :]).astype(np.float32)
    c.update(rpb_consts())
    return c


class Ctx:
    pass


def build(nlayers=DEPTH, nseq=4, stages=("P",), dbg=(), extra=None):
    nc = bass.Bass("TRN2", target_bir_lowering=False)
    g = Ctx()
    g.nc = nc
    NTOK = nseq * SEQ

    def din(name, shape):
        return nc.dram_tensor(name, list(shape), F32, kind="ExternalInput").ap()

    def dscr(name, shape, out=False):
        return nc.dram_tensor(name, list(shape), F32, kind="ExternalOutput" if out else "Internal").ap()

    g.x_in = din("x", [NTOK, D])
    g.w_in = din("w_in", [DEPTH, D, INW])
    g.w = {n: din(n, shp) for n, shp in WSHAPES.items()}
    consts = host_consts()
    if extra:
        consts.update(extra)
    g.c = {n: din("c_" + n, a.shape) for n, a in consts.items()}
    g.out = dscr("out", [NTOK, D], out=True)
    g.P = dscr("P", [SEQ, INW], out=("P" in dbg))
    g.PT = dscr("PT", [2048, SEQ], out=("PT" in dbg))
    g.Y = dscr("Y", [SEQ, D], out=("Y" in dbg))
    g.X1 = dscr("X1", [SEQ, D], out=("X1" in dbg))
    g.t_P, g.t_PT, g.t_Y, g.t_X1, g.t_G, g.t_out = T(), T(), T(), T(), T(), T()
    g.G = dscr("G", [8, 128, GP])

    with ExitStack() as es:
        k = K(nc, es)
        g.k = k
        AW = 46 * 1024
        arena_t = es.enter_context(nc.sbuf_tensor("arena", [128, AW], F32))
        g.ar = Arena(arena_t[:, :], AW)
        g.banks = [es.enter_context(nc.psum_tensor("bank%d" % i, [128, 512], F32))[:, :] for i in range(8)]
        g.t_bank = [T() for _ in range(8)]
        ar = g.ar
        g.ident = ar.alloc([128])
        g.t_ident = T()
        k.dma("sp", g.ident, g.c["ident"], writes=[g.t_ident])
        g.cm05 = ar.alloc([1])
        k.dma("sp", g.cm05, g.c["m05"], writes=[g.t_ident])
        g.cm30k = ar.alloc([1])
        k.dma("sp", g.cm30k, g.c["m30k"], writes=[g.t_ident])
        ar.mark()

        if "A" in stages or "B" in stages:
            stage_setup(g)
        for l in range(nlayers):
            for s in range(nseq):
                xsrc = g.x_in if l == 0 else g.out
                if "P" in stages:
                    stage_proj(g, l, s, xsrc)
                if "C" in stages:
                    stage_c(g, l)
                if "A" in stages:
                    stage_a(g, l)
                if "D" in stages:
                    stage_d(g, l)
                if "B" in stages:
                    stage_b(g, l)
                if "Yref" in stages:
                    k.barrier()
                    k.dma("sp", g.Y, g.c["yref"], writes=[g.t_Y])
                if "M" in stages:
                    stage_m(g, l, s, xsrc)
                if "X1ref" in stages:
                    k.barrier()
                    k.dma("sp", g.X1, g.c["x1ref"], writes=[g.t_X1])
                if "E" in stages:
                    stage_e(g, l, s)
        k.barrier()
        k.replay()
    return nc, consts


def stage_proj(g, l, s, xsrc):
    k, ar, nc = g.k, g.ar, g.nc
    k.barrier()
    ar.reset()
    xT = ar.alloc_bf([8, SEQ])
    t_xT = [T() for _ in range(NT)]
    xin = [ar.alloc([D]) for _ in range(2)]
    t_xin = [T(), T()]
    t_bank = [T() for _ in range(8)]
    for tt in range(NT):
        b = tt % 2
        r0 = s * SEQ + tt * 128
        k.dma("sp", xin[b], xsrc[r0:r0 + 128, :], writes=[t_xin[b]])
        for hb in range(2):
            bk = 2 * b + hb
            for kc4 in range(4):
                kc = hb * 4 + kc4
                k.op("pe", lambda e, o=g.banks[bk][:, kc4 * 128:(kc4 + 1) * 128], i=xin[b][:, kc * 128:(kc + 1) * 128]:
                     e.transpose(o, i, g.ident), reads=[t_xin[b], g.t_ident], writes=[t_bank[bk]])
            eng = "dve" if hb == 0 else "act"
            o = xT[:, hb * 4:hb * 4 + 4, tt * 128:(tt + 1) * 128]
            i = g.banks[bk].rearrange("p (a b) -> p a b", b=128)
            if eng == "dve":
                k.op("dve", lambda e, o=o, i=i: e.tensor_copy(out=o, in_=i), reads=[t_bank[bk]], writes=[t_xT[tt]])
            else:
                k.op("act", lambda e, o=o, i=i: e.copy(out=o, in_=i), reads=[t_bank[bk]], writes=[t_xT[tt]])
    wtf = [ar.alloc([8, 512]) for _ in range(2)]
    t_wtf = [T(), T()]
    wt = [ar.alloc_bf([8, 512]) for _ in range(2)]
    t_wt = [T(), T()]
    ot = [ar.alloc([512]) for _ in range(4)]
    t_ot = [T() for _ in range(4)]
    t_P = [g.t_P] * NT
    t_PT = g.t_PT
    oi = 0
    bi = 0
    ncb = (INW + 511) // 512
    for cb in range(ncb):
        c0 = cb * 512
        ncol = min(512, INW - c0)
        wb = cb % 2
        k.dma("sp", wtf[wb][:, :, :ncol], g.w_in[l][:, c0:c0 + ncol].rearrange("(a p) n -> p a n", p=128),
              writes=[t_wtf[wb]])
        cp(k, "pool", wt[wb][:, :, :ncol], wtf[wb][:, :, :ncol], [t_wtf[wb]], [t_wt[wb]])
        for tt in range(NT):
            bk = 4 + (bi % 4)
            bi += 1
            for kc in range(8):
                k.op("pe", lambda e, o=g.banks[bk][:, :ncol], a=xT[:, kc, tt * 128:(tt + 1) * 128], b=wt[wb][:, kc, :ncol], kc=kc:
                     e.matmul(o, a, b, start=(kc == 0), stop=(kc == 7)), reads=[t_xT[tt], t_wt[wb]], writes=[t_bank[bk]])
            ob = oi % 4
            oi += 1
            if oi % 2 == 0:
                k.op("dve", lambda e, o=ot[ob][:, :ncol], i=g.banks[bk][:, :ncol]: e.tensor_copy(out=o, in_=i),
                     reads=[t_bank[bk]], writes=[t_ot[ob]])
            else:
                k.op("act", lambda e, o=ot[ob][:, :ncol], i=g.banks[bk][:, :ncol]: e.copy(out=o, in_=i),
                     reads=[t_bank[bk]], writes=[t_ot[ob]])
            k.dma("pool", g.P[tt * 128:(tt + 1) * 128, c0:c0 + ncol], ot[ob][:, :ncol], reads=[t_ot[ob]], writes=[t_P[tt]])
        for sub in range(4):
            r0 = c0 + sub * 128
            if not (r0 < 1408 or r0 == 1792):
                continue
            for tb in range(4):
                bk = 4 + (bi % 4)
                bi += 1
                for kc in range(8):
                    k.op("pe", lambda e, o=g.banks[bk], a=wt[wb][:, kc, sub * 128:(sub + 1) * 128], b=xT[:, kc, tb * 512:(tb + 1) * 512], kc=kc:
                         e.matmul(o, a, b, start=(kc == 0), stop=(kc == 7)),
                         reads=t_xT[tb * 4:tb * 4 + 4] + [t_wt[wb]], writes=[t_bank[bk]])
                ob = oi % 4
                oi += 1
                if oi % 2 == 0:
                    k.op("dve", lambda e, o=ot[ob], i=g.banks[bk]: e.tensor_copy(out=o, in_=i), reads=[t_bank[bk]], writes=[t_ot[ob]])
                else:
                    k.op("act", lambda e, o=ot[ob], i=g.banks[bk]: e.copy(out=o, in_=i), reads=[t_bank[bk]], writes=[t_ot[ob]])
                k.dma("pool", g.PT[r0:r0 + 128, tb * 512:(tb + 1) * 512], ot[ob], reads=[t_ot[ob]], writes=[t_PT])


def mm(k, out, lhsT, rhs, R, W, start=True, stop=True):
    k.op("pe", lambda e: e.matmul(out, lhsT, rhs, start=start, stop=stop), R, W)


def tp(g, out, in_, R, W):
    n = in_.shape[0]
    g.k.op("pe", lambda e: e.transpose(out, in_, g.ident[:n, :n]), list(R) + [g.t_ident], W)


def tt(k, eng, out, a, b, op, R, W):
    k.op(eng, lambda e: e.tensor_tensor(out=out, in0=a, in1=b, op=op), R, W)


def ts(k, eng, out, a, s1, s2, op0, op1, R, W, accum=None):
    if s2 is None:
        k.op(eng, lambda e: e.tensor_scalar(out=out, in0=a, scalar1=s1, scalar2=None, op0=op0), R, W)
    elif accum is None:
        k.op(eng, lambda e: e.tensor_scalar(out=out, in0=a, scalar1=s1, scalar2=s2, op0=op0, op1=op1), R, W)
    else:
        k.op(eng, lambda e: e.tensor_scalar(out=out, in0=a, scalar1=s1, scalar2=s2, op0=op0, op1=op1, accum_out=accum), R, W)


def stt(k, eng, out, a, sc, b, op0, op1, R, W):
    k.op(eng, lambda e: e.scalar_tensor_tensor(out=out, in0=a, scalar=sc, in1=b, op0=op0, op1=op1), R, W)


def act(k, out, in_, func, R, W, bias=None, scale=1.0, accum=None):
    kw = {}
    if bias is not None:
        kw["bias"] = bias
    if accum is not None:
        kw["accum_out"] = accum
    k.op("act", lambda e: e.activation(out=out, in_=in_, func=func, scale=scale, **kw), R, W)


def rsqrt(k, out, in_, scale, eps, R, W):
    ts(k, "dve", out, in_, scale, eps, ALU.mult, ALU.add, R, W)
    k.op("act", lambda e: e.activation(out=out, in_=out, func=AF.Sqrt), [], W)
    k.op("dve", lambda e: e.reciprocal(out=out, in_=out), [], W)


def cp(k, eng, out, in_, R, W):
    if eng == "act":
        k.op("act", lambda e: e.copy(out=out, in_=in_), R, W)
    else:
        k.op(eng, lambda e: e.tensor_copy(out=out, in_=in_), R, W)


def bcast_rows(ap1d, n):
    return bass.AP(ap1d.tensor, ap1d.offset, [[0, 128], [1, n]])


class Rot:
    def __init__(self, ar, shape, n, bf=False):
        self.b = [((ar.alloc_bf(shape) if bf else ar.alloc(shape)), T()) for _ in range(n)]
        self.i = 0

    def get(self):
        r = self.b[self.i % len(self.b)]
        self.i += 1
        return r


class Banks:
    def __init__(self, g, ids):
        self.b = [(g.banks[i], g.t_bank[i]) for i in ids]
        self.i = 0

    def get(self):
        r = self.b[self.i % len(self.b)]
        self.i += 1
        return r


def stage_c(g, l):
    k, ar = g.k, g.ar
    k.barrier()
    ar.reset()
    W = g.w
    t_c = T()
    aup = ar.alloc([128])
    abias = ar.alloc([128])
    gain = ar.alloc([256])
    bm = ar.alloc([512])
    tri = ar.alloc([128])
    ones = ar.alloc([128])
    k.dma("sp", aup[0:16, :], W["c_a_up"][l], writes=[t_c])
    k.dma("sp", abias, bcast_rows(W["c_a_bias"][l], 128), writes=[t_c])
    k.dma("sp", gain, bcast_rows(W["c_norm_gain"][l], 256), writes=[t_c])
    for p_ in range(2):
        k.dma("sp", bm[0:64, p_ * 256:(p_ + 1) * 256], g.c["bm_c"][p_ * 64:(p_ + 1) * 64, :], writes=[t_c])
    k.dma("sp", tri, g.c["tri_incl"], writes=[t_c])
    k.dma("sp", ones, g.c["ones"], writes=[t_c])
    state = ar.alloc([256])
    t_state = T()
    k.op("dve", lambda e: e.memset(state, 0.0), [], [t_state])
    pin = Rot(ar, [784], 2)
    alT = Rot(ar, [128], 2)
    sb = lambda n, w: Rot(ar, [w], n)
    r_zb, r_sp, r_cum, r_eq, r_ek, r_el, r_dec = sb(2, 128), sb(2, 128), sb(2, 128), sb(2, 128), sb(2, 128), sb(2, 128), sb(2, 4)
    r_qd, r_ki, r_kl, r_qkT, r_att, r_o, r_tmp, r_sq, r_ss, r_sg, r_y = (sb(2, 128), sb(2, 128), sb(2, 128), sb(2, 1024), sb(2, 512),
                                                                        sb(2, 256), sb(2, 256), sb(2, 256), sb(2, 4), sb(2, 256), sb(2, 256))
    pb = g.banks
    ps_z, ps_cum, ps_last, ps_lt = pb[0][:, 0:128], pb[1][:, 0:128], pb[2][:, 0:128], pb[5][:, 0:4]
    ps_tr = pb[3]
    ps_att = pb[4]
    ps_o = pb[6][:, 0:256]
    ps_upd = pb[7][:, 0:256]
    tb_ = g.t_bank
    t_z, t_cum, t_last, t_lt, t_tr, t_att, t_o, t_upd = tb_[0], tb_[1], tb_[2], tb_[5], tb_[3], tb_[4], tb_[6], tb_[7]
    for tt_ in range(NT):
        r0 = tt_ * 128
        x, t_x = pin.get()
        k.dma("sp", x, g.P[r0:r0 + 128, O_CQ:O_CQ + 784], reads=[g.t_P], writes=[t_x])
        al, t_al = alT.get()
        k.dma("sp", al[0:16, :], g.PT[O_CA:O_CA + 16, r0:r0 + 128], reads=[g.t_PT], writes=[t_al])
        q, kk, v, og = x[:, 0:128], x[:, 128:256], x[:, 256:512], x[:, 528:784]
        mm(k, ps_z, al[0:16, :], aup[0:16, :], [t_al, t_c], [t_z])
        zb, t_zb = r_zb.get()
        tt(k, "dve", zb, ps_z, abias, ALU.add, [t_c], [t_zb, t_z])
        sp_, t_sp = r_sp.get()
        act(k, sp_, zb, AF.Exp, [t_zb], [t_sp], scale=-1.0)
        act(k, sp_, sp_, AF.Ln, [], [t_sp], bias=1.0)
        if CCUT == 1:
            continue
        mm(k, ps_cum, tri, sp_, [t_c, t_sp], [t_cum])
        mm(k, ps_last, ones, sp_, [t_c, t_sp], [t_last])
        for h in range(4):
            mm(k, ps_lt[0:32, h:h + 1], sp_[:, h * 32:(h + 1) * 32], ones[:, 0:1], [t_c, t_sp], [t_lt])
        cum, t_cs = r_cum.get()
        cp(k, "dve", cum, ps_cum, [], [t_cs, t_cum])
        eq, t_eq = r_eq.get()
        act(k, eq, cum, AF.Exp, [t_cs], [t_eq], scale=-1.0 / 16)
        ek, t_ek = r_ek.get()
        act(k, ek, cum, AF.Exp, [t_cs], [t_ek], scale=1.0 / 16)
        el, t_el = r_el.get()
        tt(k, "dve", el, ps_last, cum, ALU.subtract, [t_cs], [t_el, t_last])
        act(k, el, el, AF.Exp, [], [t_el], scale=-1.0 / 16)
        dec, t_dec = r_dec.get()
        act(k, dec[0:32, :], ps_lt[0:32, :], AF.Exp, [], [t_dec, t_lt], scale=-1.0 / 16)
        if CCUT == 2:
            continue
        qd, t_qd = r_qd.get()
        stt(k, "dve", qd, q, 32 ** -0.5, eq, ALU.mult, ALU.mult, [t_x, t_eq], [t_qd])
        ki, t_ki = r_ki.get()
        tt(k, "pool", ki, kk, ek, ALU.mult, [t_x, t_ek], [t_ki])
        kl, t_kl = r_kl.get()
        tt(k, "pool", kl, kk, el, ALU.mult, [t_x, t_el], [t_kl])
        if CCUT == 5:
            continue
        for h in range(4):
            tp(g, ps_tr[0:32, h * 128:(h + 1) * 128], qd[:, h * 32:(h + 1) * 32], [t_qd], [t_tr])
        tp4 = g.banks[2]
        for h in range(4):
            tp(g, tp4[0:32, h * 128:(h + 1) * 128], ki[:, h * 32:(h + 1) * 32], [t_ki], [t_last])
        qkT, t_qkT = r_qkT.get()
        cp(k, "act", qkT[0:32, 0:512], ps_tr[0:32, :], [], [t_qkT, t_tr])
        cp(k, "act", qkT[0:32, 512:1024], tp4[0:32, :], [], [t_qkT, t_last])
        for h in range(4):
            mm(k, ps_att[:, h * 128:(h + 1) * 128], qkT[0:32, 512 + h * 128:512 + (h + 1) * 128],
               qkT[0:32, h * 128:(h + 1) * 128], [t_qkT], [t_att])
        att, t_at = r_att.get()
        for h in range(4):
            tt(k, "dve", att[:, h * 128:(h + 1) * 128], ps_att[:, h * 128:(h + 1) * 128], tri, ALU.mult, [t_c], [t_at, t_att])
        for h in range(4):
            mm(k, ps_o[:, h * 64:(h + 1) * 64], qkT[0:32, h * 128:(h + 1) * 128], state[0:32, h * 64:(h + 1) * 64],
               [t_qkT, t_state], [t_o], start=True, stop=False)
            mm(k, ps_o[:, h * 64:(h + 1) * 64], att[:, h * 128:(h + 1) * 128], v[:, h * 64:(h + 1) * 64], [t_at, t_x], [t_o],
               start=False, stop=True)
        for h in range(4):
            mm(k, ps_upd[0:32, h * 64:(h + 1) * 64], kl[:, h * 32:(h + 1) * 32], v[:, h * 64:(h + 1) * 64], [t_kl, t_x], [t_upd])
        tmp, t_tmp = r_tmp.get()
        cp(k, "act", tmp[0:32, :], ps_upd[0:32, :], [], [t_tmp, t_upd])
        for h in range(4):
            stt(k, "dve", state[0:32, h * 64:(h + 1) * 64], state[0:32, h * 64:(h + 1) * 64], dec[0:32, h:h + 1],
                tmp[0:32, h * 64:(h + 1) * 64], ALU.mult, ALU.add, [t_dec, t_tmp], [t_state])
        if CCUT == 4:
            continue
        o, t_os = r_o.get()
        cp(k, "act", o, ps_o, [], [t_os, t_o])
        sq, t_sq = r_sq.get()
        tt(k, "pool", sq, o, o, ALU.mult, [t_os], [t_sq])
        ss, t_ss = r_ss.get()
        k.op("dve", lambda e, o_=ss, i_=sq.rearrange("p (h v) -> p h v", v=64): e.tensor_reduce(out=o_, in_=i_, axis=AX.X, op=ALU.add),
             [t_sq], [t_ss])
        rsqrt(k, ss, ss, 1.0 / 64, 1e-6, [], [t_ss])
        sg, t_sg = r_sg.get()
        act(k, sg, og, AF.Silu, [t_x], [t_sg])
        tt(k, "pool", sg, sg, gain, ALU.mult, [t_c], [t_sg])
        y, t_y = r_y.get()
        for h in range(4):
            stt(k, "dve", y[:, h * 64:(h + 1) * 64], o[:, h * 64:(h + 1) * 64], ss[:, h:h + 1], sg[:, h * 64:(h + 1) * 64],
                ALU.mult, ALU.mult, [t_os, t_ss, t_sg], [t_y])
        k.dma("pool", g.Y[r0:r0 + 128, 512:768], y, reads=[t_y], writes=[g.t_Y])


def make_inputs(inputs, consts, core, nseq=4, small=True):
    m = {}
    m["x"] = np.ascontiguousarray(inputs["x"][core * nseq:(core + 1) * nseq]).reshape(nseq * SEQ, D)
    m["w_in"] = np.ascontiguousarray(inputs["w_in"])
    for n in WSHAPES:
        m[n] = np.ascontiguousarray(inputs[n])
    for n, a in consts.items():
        m["c_" + n] = a
    return m


NEV = 2944
GP = NEV + 128


def t5_bucket_np(dist):
    d = np.maximum(dist, 0)
    df = np.maximum(d, 1).astype(np.float32)
    large = 16 + (np.log(df / np.float32(16)) / np.float32(math.log(2048 / 16)) * np.float32(16)).astype(np.int32)
    large = np.minimum(large, 31)
    return np.where(d < 16, d, large)


def rpb_consts():
    i = np.arange(NEV)
    dist = i - 511
    valid = (dist >= 0) & (dist <= 2047)
    b = t5_bucket_np(dist)
    oht = np.zeros((32, NEV), np.float32)
    oht[b[valid], i[valid]] = 1.0
    cA = ((dist <= 128).astype(np.float32) + ((dist % 4 == 0) & (dist <= 512)) + ((dist % 16 == 0) & (dist <= 2048))) * valid
    cB = valid.astype(np.float32)
    return {"oht": oht, "cmul": np.stack([cA, cB]).astype(np.float32)}


def stage_setup(g):
    k, ar = g.k, g.ar
    k.barrier()
    ar.reset()
    t_c = T()
    oht = ar.alloc([NEV])
    cm = [ar.alloc([NEV]), ar.alloc([NEV])]
    tab = ar.alloc([8])
    ones = ar.alloc([128])
    k.dma("sp", oht[0:32, :], g.c["oht"], writes=[t_c])
    for a in range(2):
        k.dma("sp", cm[a], bcast_rows(g.c["cmul"][a], NEV), writes=[t_c])
    k.dma("sp", tab[0:32, :], g.w["rpb_table"], writes=[t_c])
    k.dma("sp", ones, g.c["ones"], writes=[t_c])
    tabb = Rot(ar, [128], 2)
    eb = Rot(ar, [NEV], 2)
    bi = 0
    for hh in range(8):
        tb, t_tb = tabb.get()
        ts(k, "dve", tb[0:32, :], ones[0:32, :], tab[0:32, hh:hh + 1], None, ALU.mult, None, [t_c], [t_tb])
        e_, t_e = eb.get()
        for c0 in range(0, NEV, 512):
            n = min(512, NEV - c0)
            bk = bi % 4
            bi += 1
            mm(k, g.banks[bk][:, :n], tb[0:32, :], oht[0:32, c0:c0 + n], [t_tb, t_c], [g.t_bank[bk]])
            act(k, e_[:, c0:c0 + n], g.banks[bk][:, :n], AF.Exp, [], [t_e, g.t_bank[bk]])
        tt(k, "dve", e_, e_, cm[0 if hh < 4 else 1], ALU.mult, [t_c], [t_e])
        gh = g.G[hh]
        dst = bass.AP(gh.tensor, gh.offset, [[GP + 1, 128], [1, NEV]])
        k.dma("pool", dst, e_, reads=[t_e], writes=[g.t_G])


def attn_block(g, I, kT, t_kT, qT, t_q, strips, t_st, vaug, t_v, vw, ysb, t_y, ycol, st, mask=None, t_mask=None, after_pair=None):
    k = g.k

    def scores(j):
        bk = st["sbanks"][st["sb"] % len(st["sbanks"])]
        st["sb"] += 1
        ps = g.banks[bk]
        if mask is None:
            mm(k, ps, kT[0:64, j * 128:(j + 1) * 128], qT, [t_kT, t_q], [g.t_bank[bk]])
        else:
            mm(k, ps, kT[0:64, j * 128:(j + 1) * 128], qT, [t_kT, t_q], [g.t_bank[bk]], start=True, stop=False)
            mm(k, ps, st["identb"], mask[:, j, :], [t_mask], [g.t_bank[bk]], start=False, stop=True)
        pt, t_pt = st["pt"].get()
        act(k, pt, ps, AF.Exp, [], [t_pt, g.t_bank[bk]], scale=0.125)
        o = 4 * I - j
        tt(k, "dve", pt, pt, strips[:, (o + 3) * 128:(o + 3) * 128 + 512], ALU.mult, [t_st], [t_pt])
        return pt, t_pt

    def pv(j, pt, t_pt):
        for qi in range(4):
            i = 4 * I + qi
            if j > i:
                continue
            ob = 4 + qi
            mm(k, g.banks[ob][:, 0:vw + 1], pt[:, qi * 128:(qi + 1) * 128], vaug(j), [t_pt, t_v], [g.t_bank[ob]],
               start=(j == 0), stop=(j == i))
            if j == i:
                rd, t_rd = st["rd"].get()
                k.op("dve", lambda e, o_=rd, i_=g.banks[ob][:, vw:vw + 1]: e.reciprocal(out=o_, in_=i_), [], [t_rd, g.t_bank[ob]])
                ts(k, "dve", ysb[:, i, ycol:ycol + vw], g.banks[ob][:, 0:vw], rd[:, 0:1], None, ALU.mult, None,
                   [t_rd], [t_y, g.t_bank[ob]])
        if after_pair is not None:
            after_pair()

    pend = []
    depth = st.get("depth", len(st["sbanks"]))
    for j in range(4 * I + 4):
        pend.append((j,) + scores(j))
        if len(pend) > depth - 1:
            pv(*pend.pop(0))
    while pend:
        pv(*pend.pop(0))


def gen_a(g, l, fresh=True):
    k, ar = g.k, g.ar
    if fresh:
        k.barrier()
        ar.reset()
    ysb = ar.alloc([NT, 256])
    t_y = T()
    st = {"sb": 0, "pt": Rot(ar, [512], 6, bf=True), "rd": Rot(ar, [1], 4), "sbanks": (0, 1, 2, 3)}
    qTs, kTs, sts, vas = Rot(ar, [SEQ], 2, bf=True), Rot(ar, [SEQ], 2, bf=True), Rot(ar, [2816], 2), Rot(ar, [NT, 66], 2, bf=True)
    ldq, ldk, ldv = Rot(ar, [SEQ], 1), Rot(ar, [SEQ], 1), Rot(ar, [NT, 64], 1)
    yield
    pend = []
    for h in range(4):
        qT, t_q = qTs.get()
        kT, t_kT = kTs.get()
        strips, t_st = sts.get()
        va, t_v = vas.get()
        fq, t_fq = ldq.get()
        fk, t_fk = ldk.get()
        fv, t_fv = ldv.get()
        k.dma("sp", fq[0:64, :], g.PT[O_AQ + h * 64:O_AQ + (h + 1) * 64, :], reads=[g.t_PT], writes=[t_fq])
        k.dma("sp", fk[0:64, :], g.PT[O_AK + h * 64:O_AK + (h + 1) * 64, :], reads=[g.t_PT], writes=[t_fk])
        k.dma("sp", strips, g.G[h][:, 127:127 + 2816], reads=[g.t_G], writes=[t_st])
        k.dma("sp", fv, g.P[:, O_AV + h * 64:O_AV + (h + 1) * 64].rearrange("(t p) c -> p t c", p=128),
              reads=[g.t_P], writes=[t_fv])
        cp(k, "pool", qT[0:64, :], fq[0:64, :], [t_fq], [t_q])
        cp(k, "act", kT[0:64, :], fk[0:64, :], [t_fk], [t_kT])
        cp(k, "pool", va[:, :, 0:64], fv, [t_fv], [t_v])
        k.op("dve", lambda e, o_=va[:, :, 64:65]: e.memset(o_, 1.0), [], [t_v])
        for I in range(4):
            attn_block(g, I, kT, t_kT, qT[0:64, I * 512:(I + 1) * 512], t_q, strips, t_st, lambda j, va=va: va[:, j, 0:65], t_v, 64, ysb, t_y,
                       h * 64, st, after_pair=lambda: pend.append(1))
            while pend:
                pend.pop()
                yield
    k.dma("pool", g.Y[:, 0:256].rearrange("(t p) c -> p t c", p=128), ysb, reads=[t_y], writes=[g.t_Y])


def stage_a(g, l):
    for _ in gen_a(g, l):
        pass


def stage_ad(g, l):
    k, ar = g.k, g.ar
    k.barrier()
    ar.reset()
    ga = gen_a(g, l, fresh=False)
    next(ga)
    gd = gen_d(g, l, fresh=False, banks=(2, 3))
    next(gd)
    da = dd = False
    while not (da and dd):
        if not da:
            try:
                next(ga)
            except StopIteration:
                da = True
        for _ in range(2):
            if not dd:
                try:
                    next(gd)
                except StopIteration:
                    dd = True


def ln_tile(k, r, t_r, gbc, bbc, t_c, sm, out, t_out):
    s1, t_s1 = sm.get()
    k.op("dve", lambda e: e.tensor_reduce(out=s1[:, 0:1], in_=r, axis=AX.X, op=ALU.add), [t_r], [t_s1])
    ts(k, "dve", s1[:, 0:1], s1[:, 0:1], -1.0 / D, None, ALU.mult, None, [], [t_s1])
    ts(k, "dve", r, r, s1[:, 0:1], None, ALU.add, None, [t_s1], [t_r])
    act(k, out, r, AF.Square, [t_r], [t_out, t_s1], accum=s1[:, 1:2])
    rsqrt(k, s1[:, 1:2], s1[:, 1:2], 1.0 / D, LN_EPS, [], [t_s1])
    stt(k, "dve", out, r, s1[:, 1:2], gbc, ALU.mult, ALU.mult, [t_r, t_s1, t_c], [t_out])
    tt(k, "pool", out, out, bbc, ALU.add, [t_c], [t_out])


def stage_m(g, l, s, xsrc):
    k, ar = g.k, g.ar
    k.barrier()
    ar.reset()
    W = g.w
    t_c = T()
    wb = ar.alloc_bf([8, D])
    wo = ar.alloc_bf([8, D])
    gbc = ar.alloc([D])
    bbc = ar.alloc([D])
    stg = Rot(ar, [2, D], 2)
    for c2 in range(4):
        sb_, t_sb = stg.get()
        k.dma("sp", sb_, W["w_branch"][l, c2].rearrange("(c p) d -> p c d", p=128), writes=[t_sb])
        cp(k, "pool" if c2 % 2 else "act", wb[:, 2 * c2:2 * c2 + 2, :], sb_, [t_sb], [t_c])
    for c2 in range(4):
        sb_, t_sb = stg.get()
        k.dma("sp", sb_, W["w_out"][l][c2 * 256:(c2 + 1) * 256, :].rearrange("(c p) d -> p c d", p=128), writes=[t_sb])
        cp(k, "pool" if c2 % 2 else "act", wo[:, 2 * c2:2 * c2 + 2, :], sb_, [t_sb], [t_c])
    k.dma("sp", gbc, bcast_rows(W["ln_g"][l, 0], D), writes=[t_c])
    k.dma("sp", bbc, bcast_rows(W["ln_b"][l, 0], D), writes=[t_c])
    r_y, r_gate, r_yT, r_mg, r_mT, r_x, r_tmp, r_out, sm = (Rot(ar, [D], 2), Rot(ar, [4 * D], 2), Rot(ar, [D], 2, bf=True), Rot(ar, [D], 2),
                                                          Rot(ar, [D], 2, bf=True), Rot(ar, [D], 2), Rot(ar, [512], 3), Rot(ar, [D], 2), Rot(ar, [2], 4))
    B, tB = g.banks, g.t_bank
    bi = 0
    for tt_ in range(NT):
        r0 = tt_ * 128
        y, t_y = r_y.get()
        k.dma("sp", y, g.Y[r0:r0 + 128, :], reads=[g.t_Y], writes=[t_y])
        gt, t_gt = r_gate.get()
        k.dma("sp", gt, g.P[r0:r0 + 128, O_GATE:O_GATE + 4 * D], reads=[g.t_P], writes=[t_gt])
        x, t_x = r_x.get()
        k.dma("sp", x, xsrc[s * SEQ + r0:s * SEQ + r0 + 128, :], writes=[t_x])
        act(k, gt, gt, AF.Sigmoid, [], [t_gt])
        yT, t_yT = r_yT.get()
        for hb in range(2):
            for c4 in range(4):
                c = hb * 4 + c4
                tp(g, B[hb][:, c4 * 128:(c4 + 1) * 128], y[:, c * 128:(c + 1) * 128], [t_y], [tB[hb]])
            cp(k, "act" if hb else "dve", yT[:, hb * 512:(hb + 1) * 512], B[hb], [], [t_yT, tB[hb]])
        mg, t_mg = r_mg.get()
        for n in range(4):
            for dh in range(2):
                bk = 2 + bi % 2
                bi += 1
                for cc in range(2):
                    c = 2 * n + cc
                    mm(k, B[bk], yT[:, c * 128:(c + 1) * 128], wb[:, c, dh * 512:(dh + 1) * 512], [t_yT, t_c], [tB[bk]],
                       start=(cc == 0), stop=(cc == 1))
                gsl = gt[:, n * D + dh * 512:n * D + (dh + 1) * 512]
                msl = mg[:, dh * 512:(dh + 1) * 512]
                if n == 0:
                    tt(k, "dve", msl, B[bk], gsl, ALU.mult, [t_gt], [t_mg, tB[bk]])
                else:
                    tmp, t_tmp = r_tmp.get()
                    tt(k, "dve", tmp, B[bk], gsl, ALU.mult, [t_gt], [t_tmp, tB[bk]])
                    tt(k, "pool", msl, msl, tmp, ALU.add, [t_tmp], [t_mg])
        mT, t_mT = r_mT.get()
        for hb in range(2):
            for c4 in range(4):
                c = hb * 4 + c4
                tp(g, B[4 + hb][:, c4 * 128:(c4 + 1) * 128], mg[:, c * 128:(c + 1) * 128], [t_mg], [tB[4 + hb]])
            cp(k, "act" if hb else "dve", mT[:, hb * 512:(hb + 1) * 512], B[4 + hb], [], [t_mT, tB[4 + hb]])
        for dh in range(2):
            bk = 6 + dh
            for c in range(8):
                mm(k, B[bk], mT[:, c * 128:(c + 1) * 128], wo[:, c, dh * 512:(dh + 1) * 512], [t_mT, t_c], [tB[bk]],
                   start=(c == 0), stop=(c == 7))
            stt(k, "dve", x[:, dh * 512:(dh + 1) * 512], x[:, dh * 512:(dh + 1) * 512], DN_ALPHA, B[bk], ALU.mult, ALU.add,
                [], [t_x, tB[bk]])
        o, t_o = r_out.get()
        ln_tile(k, x, t_x, gbc, bbc, t_c, sm, o, t_o)
        k.dma("pool", g.X1[r0:r0 + 128, :], o, reads=[t_o], writes=[g.t_X1])


def stage_e(g, l, s):
    k, ar = g.k, g.ar
    W = g.w
    B, tB = g.banks, g.t_bank
    for half in range(2):
        k.barrier()
        ar.reset()
        t_c = T()
        wr = ar.alloc([8, 36])
        rb = ar.alloc([36])
        gbc = ar.alloc([D])
        bbc = ar.alloc([D])
        k.dma("sp", wr[:, :, 0:4], W["router_g"][l].rearrange("(c p) e -> p c e", p=128), writes=[t_c])
        k.dma("sp", wr[:, :, 4:36], W["router_e"][l].rearrange("(c p) e -> p c e", p=128), writes=[t_c])
        k.dma("sp", rb[:, 0:4], bcast_rows(W["router_g_bias"][l], 4), writes=[t_c])
        k.dma("sp", rb[:, 4:36], bcast_rows(W["router_e_bias"][l], 32), writes=[t_c])
        k.dma("sp", gbc, bcast_rows(W["ln_g"][l, 1], D), writes=[t_c])
        k.dma("sp", bbc, bcast_rows(W["ln_b"][l, 1], D), writes=[t_c])
        r_x1 = Rot(ar, [D], 2)
        xT = ar.alloc_bf([8, 1024])
        t_xT = [T() for _ in range(8)]
        r_xf = Rot(ar, [8, 128], 2)
        yacc = ar.alloc([8, D])
        t_ya = [T() for _ in range(8)]
        gates = ar.alloc([8, 32])
        t_g = [T() for _ in range(8)]
        sm = Rot(ar, [40], 4)
        sm2 = Rot(ar, [8], 4)
        sm3 = Rot(ar, [32], 6)
        for ti in range(8):
            r0 = half * 1024 + ti * 128
            x1t, t_x1t = r_x1.get()
            k.dma("sp", x1t, g.X1[r0:r0 + 128, :], reads=[g.t_X1], writes=[t_x1t])
            for hb in range(2):
                for c4 in range(4):
                    c = hb * 4 + c4
                    tp(g, B[hb][:, c4 * 128:(c4 + 1) * 128], x1t[:, c * 128:(c + 1) * 128], [t_x1t], [tB[hb]])
                if hb == 0:
                    xf, t_xf = r_xf.get()
                cp(k, "act" if hb else "dve", xf[:, hb * 4:hb * 4 + 4, :], B[hb].rearrange("p (a b) -> p a b", b=128), [], [t_xf, tB[hb]])
            cp(k, "pool", xT[:, :, ti * 128:(ti + 1) * 128], xf, [t_xf], [t_xT[ti]])
            for c in range(8):
                mm(k, B[2][:, 0:36], xf[:, c, :], wr[:, c, :], [t_xf, t_c], [tB[2]], start=(c == 0), stop=(c == 7))
            lg, t_lg = sm.get()
            tt(k, "dve", lg[:, 0:36], B[2][:, 0:36], rb, ALU.add, [t_c], [t_lg, tB[2]])
            a, t_a = sm2.get()
            k.op("dve", lambda e, o_=a[:, 0:1], i_=lg[:, 0:4]: e.tensor_reduce(out=o_, in_=i_, axis=AX.X, op=ALU.max), [t_lg], [t_a])
            ts(k, "dve", a[:, 1:2], a[:, 0:1], -1.0, None, ALU.mult, None, [], [t_a])
            e4, t_e4 = sm3.get()
            act(k, e4[:, 0:4], lg[:, 0:4], AF.Exp, [t_lg, t_a], [t_e4, t_a], bias=a[:, 1:2], accum=a[:, 2:3])
            ohg, t_ohg = sm3.get()
            ts(k, "dve", ohg[:, 0:4], lg[:, 0:4], a[:, 0:1], None, ALU.is_equal, None, [t_lg, t_a], [t_ohg])
            ts(k, "dve", ohg[:, 0:4], ohg[:, 0:4], -1.0, 1e30, ALU.add, ALU.mult, [], [t_ohg])
            lem, t_lem = sm3.get()
            for gi in range(4):
                ts(k, "dve", lem[:, gi * 8:(gi + 1) * 8], lg[:, 4 + gi * 8:4 + (gi + 1) * 8], ohg[:, gi:gi + 1], None, ALU.add, None,
                   [t_lg, t_ohg], [t_lem])
            k.op("dve", lambda e, o_=a[:, 3:4], i_=lem: e.tensor_reduce(out=o_, in_=i_, axis=AX.X, op=ALU.max), [t_lem], [t_a])
            oh1, t_oh1 = sm3.get()
            ts(k, "dve", oh1, lem, a[:, 3:4], None, ALU.is_equal, None, [t_lem, t_a], [t_oh1])
            stt(k, "dve", lem, oh1, -1e30, lem, ALU.mult, ALU.add, [t_oh1], [t_lem])
            k.op("dve", lambda e, o_=a[:, 4:5], i_=lem: e.tensor_reduce(out=o_, in_=i_, axis=AX.X, op=ALU.max), [t_lem], [t_a])
            oh2, t_oh2 = sm3.get()
            ts(k, "dve", oh2, lem, a[:, 4:5], None, ALU.is_equal, None, [t_lem, t_a], [t_oh2])
            tt(k, "dve", a[:, 5:6], a[:, 4:5], a[:, 3:4], ALU.subtract, [], [t_a])
            act(k, a[:, 5:6], a[:, 5:6], AF.Exp, [], [t_a])
            ts(k, "dve", a[:, 6:7], a[:, 5:6], 1.0, None, ALU.add, None, [], [t_a])
            tt(k, "dve", a[:, 6:7], a[:, 6:7], a[:, 2:3], ALU.mult, [], [t_a])
            k.op("dve", lambda e, o_=a[:, 6:7]: e.reciprocal(out=o_, in_=o_), [], [t_a])
            tt(k, "dve", a[:, 7:8], a[:, 6:7], a[:, 5:6], ALU.mult, [], [t_a])
            ts(k, "dve", gates[:, ti, :], oh1, a[:, 6:7], None, ALU.mult, None, [t_oh1, t_a], [t_g[ti]])
            stt(k, "dve", gates[:, ti, :], oh2, a[:, 7:8], gates[:, ti, :], ALU.mult, ALU.add, [t_oh2, t_a], [t_g[ti]])
        r_wg, r_wu, r_wd = Rot(ar, [8, 256], 2, bf=True), Rot(ar, [8, 256], 2, bf=True), Rot(ar, [2, D], 2, bf=True)
        f_wg, f_wu, f_wd = Rot(ar, [8, 256], 2), Rot(ar, [8, 256], 2), Rot(ar, [2, D], 2)
        r_sg, r_h = Rot(ar, [512], 2), Rot(ar, [2, 512], 2, bf=True)
        bi = 0
        for ex in range(32):
            wg, t_wg = r_wg.get()
            wu, t_wu = r_wu.get()
            wd, t_wd = r_wd.get()
            fg, t_fg = f_wg.get()
            fu, t_fu = f_wu.get()
            fd, t_fd = f_wd.get()
            k.dma("sp", fg, W["moe_w_gate"][l, ex].rearrange("(c p) h -> p c h", p=128), writes=[t_fg])
            k.dma("sp", fu, W["moe_w_up"][l, ex].rearrange("(c p) h -> p c h", p=128), writes=[t_fu])
            k.dma("sp", fd, W["moe_w_down"][l, ex].rearrange("(c p) d -> p c d", p=128), writes=[t_fd])
            cp(k, "pool", wg, fg, [t_fg], [t_wg])
            cp(k, "pool", wu, fu, [t_fu], [t_wu])
            cp(k, "act", wd, fd, [t_fd], [t_wd])
            for tb in range(2):
                hT, t_h = r_h.get()
                for hc in range(2):
                    for c in range(8):
                        mm(k, B[0], wg[:, c, hc * 128:(hc + 1) * 128], xT[:, c, tb * 512:(tb + 1) * 512], [t_wg] + t_xT[tb * 4:tb * 4 + 4], [tB[0]],
                           start=(c == 0), stop=(c == 7))
                    for c in range(8):
                        mm(k, B[1], wu[:, c, hc * 128:(hc + 1) * 128], xT[:, c, tb * 512:(tb + 1) * 512], [t_wu] + t_xT[tb * 4:tb * 4 + 4], [tB[1]],
                           start=(c == 0), stop=(c == 7))
                    sg, t_sg = r_sg.get()
                    act(k, sg, B[0], AF.Silu, [], [t_sg, tB[0]])
                    tt(k, "dve", hT[:, hc, :], B[1], sg, ALU.mult, [t_sg], [t_h, tB[1]])
                for q4 in range(4):
                    ti = tb * 4 + q4
                    for dh in range(2):
                        bk = 2 + bi % 6
                        bi += 1
                        for hc in range(2):
                            mm(k, B[bk], hT[:, hc, q4 * 128:(q4 + 1) * 128], wd[:, hc, dh * 512:(dh + 1) * 512], [t_h, t_wd], [tB[bk]],
                               start=(hc == 0), stop=(hc == 1))
                        ysl = yacc[:, ti, dh * 512:(dh + 1) * 512]
                        if ex == 0:
                            ts(k, "dve", ysl, B[bk], gates[:, ti, ex:ex + 1], None, ALU.mult, None, [t_g[ti]], [t_ya[ti], tB[bk]])
                        else:
                            stt(k, "dve", ysl, B[bk], gates[:, ti, ex:ex + 1], ysl, ALU.mult, ALU.add, [t_g[ti]], [t_ya[ti], tB[bk]])
        r_out = Rot(ar, [D], 2)
        for ti in range(8):
            r0 = s * SEQ + half * 1024 + ti * 128
            x1t, t_x1t = r_x1.get()
            k.dma("sp", x1t, g.X1[half * 1024 + ti * 128:half * 1024 + (ti + 1) * 128, :], reads=[g.t_X1], writes=[t_x1t])
            stt(k, "dve", yacc[:, ti, :], x1t, DN_ALPHA, yacc[:, ti, :], ALU.mult, ALU.add, [t_x1t], [t_ya[ti]])
            o, t_o = r_out.get()
            ln_tile(k, yacc[:, ti, :], t_ya[ti], gbc, bbc, t_c, sm2, o, t_o)
            k.dma("pool", g.out[r0:r0 + 128, :], o, reads=[t_o], writes=[g.t_out])


def bc_last(ap, n):
    return bass.AP(ap.tensor, ap.offset, [list(x) for x in ap.ap] + [[0, n]])


def v3(ap, inner):
    return ap.rearrange("p (a b) -> p a b", b=inner)


def stage_d(g, l):
    for _ in gen_d(g, l):
        pass


def gen_d(g, l, fresh=True, banks=tuple(range(8))):
    k, ar = g.k, g.ar
    if fresh:
        k.barrier()
        ar.reset()
    W = g.w
    B, tB = g.banks, g.t_bank
    st = {"b": 0}

    def nb():
        st["b"] = (st["b"] + 1) % len(banks)
        return banks[st["b"]]
    t_c = T()
    mu = ar.alloc([832])
    wcat = ar.alloc([768])
    vec = {n: ar.alloc([256]) for n in ("d_w0", "d_a0", "d_k_k", "d_k_a", "d_r_k", "d_gn_w", "d_gn_b")}
    tri_i, tri_s, tril_s, ones, idn = ar.alloc([128]), ar.alloc([128]), ar.alloc([128]), ar.alloc([128]), g.ident
    k.dma("sp", mu, bcast_rows(W["d_mu"][l], 832), writes=[t_c])
    k.op("dve", lambda e: e.memset(wcat, 0.0), [], [t_c])
    k.dma("sp", wcat[0:16, 0:256], W["d_w2"][l], writes=[t_c])
    k.dma("sp", wcat[16:32, 256:512], W["d_a2"][l], writes=[t_c])
    k.dma("sp", wcat[32:64, 512:768], W["d_g2"][l], writes=[t_c])
    for n, a in vec.items():
        k.dma("sp", a, bcast_rows(W[n][l], 256), writes=[t_c])
    k.dma("sp", tri_i, g.c["tri_incl"], writes=[t_c])
    k.dma("sp", tri_s, g.c["tri_strict"], writes=[t_c])
    k.dma("sp", tril_s, g.c["tril_strict"], writes=[t_c])
    k.dma("sp", ones, g.c["ones"], writes=[t_c])
    S = ar.alloc([256])
    t_S = T()
    k.op("dve", lambda e: e.memset(S, 0.0), [], [t_S])
    R2 = lambda w: Rot(ar, [w], 2)
    r_cur, r_prev, r_seg, r_z, r_zT = R2(832), R2(832), R2(832), R2(64), R2(128)
    r_nlw, r_a, r_g, r_kk, r_kp, r_sm, r_tmp = R2(256), R2(256), R2(256), R2(256), R2(256), Rot(ar, [16], 4), Rot(ar, [256], 4)
    r_en, r_ep, r_ex = R2(256), R2(256), R2(256)
    r_at, r_bt, r_kt, r_rt = R2(256), R2(256), R2(256), R2(256)
    r_FT = Rot(ar, [4, 512], 2)
    r_wT = R2(4)
    r_M = Rot(ar, [5, 512], 1)
    r_P, r_PT, r_XT = Rot(ar, [512], 2), Rot(ar, [512], 2), Rot(ar, [512], 2)
    r_rhs0, r_U, r_y, r_o = R2(256), R2(256), R2(256), R2(256)
    yield
    for tt_ in range(NT):
        r0 = tt_ * 128
        cur, t_cur = r_cur.get()
        prev, t_prev = r_prev.get()
        k.dma("sp", cur, g.P[r0:r0 + 128, O_D:O_D + 832], reads=[g.t_P], writes=[t_cur])
        if tt_ == 0:
            k.op("dve", lambda e, o_=prev[0:1, :]: e.memset(o_, 0.0), [], [t_prev])
            k.dma("sp", prev[1:128, :], g.P[0:127, O_D:O_D + 832], reads=[g.t_P], writes=[t_prev])
        else:
            k.dma("sp", prev, g.P[r0 - 1:r0 + 127, O_D:O_D + 832], reads=[g.t_P], writes=[t_prev])
        seg, t_seg = r_seg.get()
        tt(k, "pool", prev, prev, cur, ALU.subtract, [t_cur], [t_prev])
        tt(k, "pool", prev, prev, mu, ALU.mult, [t_c], [t_prev])
        tt(k, "dve", seg, cur, prev, ALU.add, [t_cur, t_prev], [t_seg])
        r, kraw, v = seg[:, 0:256], seg[:, 256:512], seg[:, 512:768]
        z, t_z = r_z.get()
        act(k, z[:, 0:16], seg[:, 768:784], AF.Tanh, [t_seg], [t_z])
        cp(k, "dve", z[:, 16:32], seg[:, 784:800], [t_seg], [t_z])
        act(k, z[:, 32:64], seg[:, 800:832], AF.Sigmoid, [t_seg], [t_z])
        b0 = nb()
        tp(g, B[b0][0:64, 0:128], z, [t_z], [tB[b0]])
        zT, t_zT = r_zT.get()
        cp(k, "act", zT[0:64, :], B[b0][0:64, 0:128], [], [t_zT, tB[b0]])
        bw, bg_ = nb(), nb()
        mm(k, B[bw], zT[0:64, :], wcat[0:64, 0:512], [t_zT, t_c], [tB[bw]])
        mm(k, B[bg_][:, 0:256], zT[0:64, :], wcat[0:64, 512:768], [t_zT, t_c], [tB[bg_]])
        nlw, t_nlw = r_nlw.get()
        tt(k, "dve", nlw, B[bw][:, 0:256], vec["d_w0"], ALU.add, [t_c], [t_nlw, tB[bw]])
        a, t_a = r_a.get()
        tt(k, "dve", a, B[bw][:, 256:512], vec["d_a0"], ALU.add, [t_c], [t_a, tB[bw]])
        gsb, t_gs = r_g.get()
        cp(k, "act", gsb, B[bg_][:, 0:256], [], [t_gs, tB[bg_]])
        act(k, nlw, nlw, AF.Exp, [], [t_nlw], scale=-1.0)
        act(k, nlw, nlw, AF.Ln, [], [t_nlw], bias=1.0)
        act(k, nlw, nlw, AF.Exp, [], [t_nlw], scale=-1.0, bias=g.cm05[:, 0:1])
        act(k, a, a, AF.Sigmoid, [], [t_a])
        yield
        kk, t_kk = r_kk.get()
        tt(k, "pool", kk, kraw, vec["d_k_k"], ALU.mult, [t_seg, t_c], [t_kk])
        tmp, t_tmp = r_tmp.get()
        tt(k, "pool", tmp, kk, kk, ALU.mult, [t_kk], [t_tmp])
        sm, t_sm = r_sm.get()
        k.op("dve", lambda e, o_=sm[:, 0:4], i_=v3(tmp, 64): e.tensor_reduce(out=o_, in_=i_, axis=AX.X, op=ALU.add), [t_tmp], [t_sm])
        k.op("act", lambda e, o_=sm[:, 0:4]: e.activation(out=o_, in_=o_, func=AF.Sqrt), [], [t_sm])
        ts(k, "dve", sm[:, 0:4], sm[:, 0:4], 1e-12, None, ALU.max, None, [], [t_sm])
        k.op("dve", lambda e, o_=sm[:, 0:4]: e.reciprocal(out=o_, in_=o_), [], [t_sm])
        tt(k, "dve", v3(kk, 64), v3(kk, 64), bc_last(sm[:, 0:4], 64), ALU.mult, [t_sm], [t_kk])
        kp, t_kp = r_kp.get()
        stt(k, "dve", kp, a, -1.0, vec["d_k_a"], ALU.add, ALU.mult, [t_a, t_c], [t_kp])
        stt(k, "dve", kp, kp, 1.0, kraw, ALU.add, ALU.mult, [t_seg], [t_kp])
        yield
        bc = nb()
        mm(k, B[bc][:, 0:256], tri_i, nlw, [t_c, t_nlw], [tB[bc]])
        for h in range(4):
            mm(k, B[bc][0:64, 256 + h:257 + h], nlw[:, h * 64:(h + 1) * 64], ones[:, 0:1], [t_nlw, t_c], [tB[bc]])
        en, t_en = r_en.get()
        ep, t_ep = r_ep.get()
        ex, t_ex = r_ex.get()
        wT, t_wT = r_wT.get()
        act(k, en, B[bc][:, 0:256], AF.Exp, [], [t_en, tB[bc]], scale=-1.0)
        act(k, ep, B[bc][:, 0:256], AF.Exp, [], [t_ep, tB[bc]])
        tt(k, "dve", ex, B[bc][:, 0:256], nlw, ALU.subtract, [t_nlw], [t_ex, tB[bc]])
        act(k, wT[0:64, :], B[bc][0:64, 256:260], AF.Exp, [], [t_wT, tB[bc]], scale=-1.0)
        act(k, ex, ex, AF.Exp, [], [t_ex], scale=-1.0)
        at, t_at = r_at.get()
        bt, t_bt = r_bt.get()
        kt, t_kt = r_kt.get()
        rt, t_rt = r_rt.get()
        stt(k, "dve", at, kk, -1.0, ex, ALU.mult, ALU.mult, [t_kk, t_ex], [t_at])
        tt(k, "pool", bt, kk, a, ALU.mult, [t_kk, t_a], [t_bt])
        tt(k, "pool", bt, bt, ep, ALU.mult, [t_ep], [t_bt])
        tt(k, "dve", kt, kp, ep, ALU.mult, [t_kp, t_ep], [t_kt])
        tt(k, "pool", rt, r, en, ALU.mult, [t_seg, t_en], [t_rt])
        yield
        FT, t_FT = r_FT.get()
        for qi, (src, t_src) in enumerate(((at, t_at), (bt, t_bt), (kt, t_kt), (rt, t_rt))):
            bq = nb()
            for h in range(4):
                tp(g, B[bq][0:64, h * 128:(h + 1) * 128], src[:, h * 64:(h + 1) * 64], [t_src], [tB[bq]])
            cp(k, "act" if qi % 2 else "dve", FT[0:64, qi, :], B[bq][0:64, :], [], [t_FT, tB[bq]])
        aT = lambda h: FT[0:64, 0, h * 128:(h + 1) * 128]
        bT = lambda h: FT[0:64, 1, h * 128:(h + 1) * 128]
        kT = lambda h: FT[0:64, 2, h * 128:(h + 1) * 128]
        rT = lambda h: FT[0:64, 3, h * 128:(h + 1) * 128]
        yield
        M, t_M = r_M.get()
        specs = ((bT, aT, tri_s), (aT, bT, tril_s), (kT, aT, tri_s), (bT, rT, tri_i), (kT, rT, tri_i))
        for mi, (lf, rf, msk) in enumerate(specs):
            bq = nb()
            for h in range(4):
                mm(k, B[bq][:, h * 128:(h + 1) * 128], lf(h), rf(h), [t_FT], [tB[bq]])
            for h in range(4):
                tt(k, "dve", M[:, mi, h * 128:(h + 1) * 128], B[bq][:, h * 128:(h + 1) * 128], msk, ALU.mult, [t_c], [t_M, tB[bq]])
        LT, L, LakT, ArbT, ArkT = (M[:, i, :] for i in range(5))
        yield
        XT, t_XT = r_XT.get()
        for h in range(4):
            tt(k, "pool", XT[:, h * 128:(h + 1) * 128], LT[:, h * 128:(h + 1) * 128], idn, ALU.add, [t_M, g.t_ident], [t_XT])
        P_, t_P_ = L, t_M
        PT_, t_PT_ = LT, t_M
        for step in range(1, 7):
            b1 = nb()
            for h in range(4):
                hs = slice(h * 128, (h + 1) * 128)
                mm(k, B[b1][:, hs], PT_[:, hs], P_[:, hs], [t_P_, t_PT_], [tB[b1]])
            Pn, t_Pn = r_P.get()
            cp(k, "act", Pn, B[b1], [], [t_Pn, tB[b1]])
            if step < 6:
                b2 = nb()
                for h in range(4):
                    hs = slice(h * 128, (h + 1) * 128)
                    mm(k, B[b2][:, hs], P_[:, hs], PT_[:, hs], [t_P_, t_PT_], [tB[b2]])
                PTn, t_PTn = r_PT.get()
                cp(k, "dve", PTn, B[b2], [], [t_PTn, tB[b2]])
            b3 = nb()
            for h in range(4):
                hs = slice(h * 128, (h + 1) * 128)
                mm(k, B[b3][:, hs], Pn[:, hs], XT[:, hs], [t_Pn, t_XT], [tB[b3]])
            XTn, t_XTn = r_XT.get()
            tt(k, "dve", XTn, XT, B[b3], ALU.add, [t_XT], [t_XTn, tB[b3]])
            XT, t_XT = XTn, t_XTn
            yield
            P_, t_P_ = Pn, t_Pn
            if step < 6:
                PT_, t_PT_ = PTn, t_PTn
        yield
        b1 = nb()
        for h in range(4):
            vs = slice(h * 64, (h + 1) * 64)
            mm(k, B[b1][:, vs], aT(h), S[0:64, vs], [t_FT, t_S], [tB[b1]], start=True, stop=False)
            mm(k, B[b1][:, vs], LakT[:, h * 128:(h + 1) * 128], v[:, vs], [t_M, t_seg], [tB[b1]], start=False, stop=True)
        rhs0, t_rhs0 = r_rhs0.get()
        cp(k, "act", rhs0, B[b1][:, 0:256], [], [t_rhs0, tB[b1]])
        b2 = nb()
        for h in range(4):
            vs = slice(h * 64, (h + 1) * 64)
            mm(k, B[b2][:, vs], XT[:, h * 128:(h + 1) * 128], rhs0[:, vs], [t_XT, t_rhs0], [tB[b2]])
        U, t_U = r_U.get()
        cp(k, "dve", U, B[b2][:, 0:256], [], [t_U, tB[b2]])
        yield
        b3 = nb()
        for h in range(4):
            vs = slice(h * 64, (h + 1) * 64)
            hs = slice(h * 128, (h + 1) * 128)
            mm(k, B[b3][:, vs], rT(h), S[0:64, vs], [t_FT, t_S], [tB[b3]], start=True, stop=False)
            mm(k, B[b3][:, vs], ArbT[:, hs], U[:, vs], [t_M, t_U], [tB[b3]], start=False, stop=False)
            mm(k, B[b3][:, vs], ArkT[:, hs], v[:, vs], [t_M, t_seg], [tB[b3]], start=False, stop=True)
        y, t_y = r_y.get()
        cp(k, "act", y, B[b3][:, 0:256], [], [t_y, tB[b3]])
        yield
        b4 = nb()
        for h in range(4):
            vs = slice(h * 64, (h + 1) * 64)
            mm(k, B[b4][0:64, vs], bt[:, vs], U[:, vs], [t_bt, t_U], [tB[b4]], start=True, stop=False)
            mm(k, B[b4][0:64, vs], kt[:, vs], v[:, vs], [t_kt, t_seg], [tB[b4]], start=False, stop=True)
        tt(k, "dve", S[0:64, :], S[0:64, :], B[b4][0:64, 0:256], ALU.add, [], [t_S, tB[b4]])
        tt(k, "dve", v3(S[0:64, :], 64), v3(S[0:64, :], 64), bc_last(wT[0:64, :], 64), ALU.mult, [t_wT], [t_S])
        yield
        sm2, t_sm2 = r_sm.get()
        k.op("dve", lambda e, o_=sm2[:, 0:4], i_=v3(y, 64): e.tensor_reduce(out=o_, in_=i_, axis=AX.X, op=ALU.add), [t_y], [t_sm2])
        ts(k, "dve", sm2[:, 0:4], sm2[:, 0:4], -1.0 / 64, None, ALU.mult, None, [], [t_sm2])
        tt(k, "dve", v3(y, 64), v3(y, 64), bc_last(sm2[:, 0:4], 64), ALU.add, [t_sm2], [t_y])
        tmp2, t_tmp2 = r_tmp.get()
        tt(k, "pool", tmp2, y, y, ALU.mult, [t_y], [t_tmp2])
        k.op("dve", lambda e, o_=sm2[:, 4:8], i_=v3(tmp2, 64): e.tensor_reduce(out=o_, in_=i_, axis=AX.X, op=ALU.add), [t_tmp2], [t_sm2])
        rsqrt(k, sm2[:, 4:8], sm2[:, 4:8], 1.0 / 64, 64e-5, [], [t_sm2])
        tt(k, "dve", v3(y, 64), v3(y, 64), bc_last(sm2[:, 4:8], 64), ALU.mult, [t_sm2], [t_y])
        tt(k, "pool", y, y, vec["d_gn_w"], ALU.mult, [t_c], [t_y])
        tt(k, "pool", y, y, vec["d_gn_b"], ALU.add, [t_c], [t_y])
        tmp3, t_tmp3 = r_tmp.get()
        tt(k, "dve", tmp3, r, kp, ALU.mult, [t_seg, t_kp], [t_tmp3])
        tt(k, "dve", tmp3, tmp3, vec["d_r_k"], ALU.mult, [t_c], [t_tmp3])
        k.op("dve", lambda e, o_=sm2[:, 8:12], i_=v3(tmp3, 64): e.tensor_reduce(out=o_, in_=i_, axis=AX.X, op=ALU.add), [t_tmp3], [t_sm2])
        tt(k, "dve", v3(tmp3, 64), v3(v, 64), bc_last(sm2[:, 8:12], 64), ALU.mult, [t_seg, t_sm2], [t_tmp3])
        tt(k, "pool", y, y, tmp3, ALU.add, [t_tmp3], [t_y])
        o, t_o = r_o.get()
        tt(k, "dve", o, y, gsb, ALU.mult, [t_y, t_gs], [t_o])
        k.dma("pool", g.Y[r0:r0 + 128, 768:1024], o, reads=[t_o], writes=[g.t_Y])


def bc_mid(ap, n):
    a = [list(x) for x in ap.ap]
    return bass.AP(ap.tensor, ap.offset, [a[0], [0, n]] + a[1:])


def stage_b(g, l):
    k, ar = g.k, g.ar
    k.barrier()
    ar.reset()
    W = g.w
    B, tB = g.banks, g.t_bank
    t_c = T()
    gain = ar.alloc([64])
    wuv = ar.alloc([256])
    cneg = ar.alloc([128])
    k.dma("sp", gain, bcast_rows(W["b_kv_gain"][l], 64), writes=[t_c])
    k.dma("sp", v3(wuv[0:64, :], 64), W["b_w_uv"][l].rearrange("h c d -> c h d"), writes=[t_c])
    k.dma("sp", cneg, g.c["caus_neg"], writes=[t_c])
    ckv = ar.alloc([NT, 64])
    t_ckv = T()
    k.dma("sp", ckv, g.P[:, O_CKV:O_CKV + 64].rearrange("(t p) c -> p t c", p=128), reads=[g.t_P], writes=[t_ckv])
    iw = ar.alloc([NT, 8])
    t_iw = T()
    k.dma("sp", iw, g.P[:, O_IW:O_IW + 8].rearrange("(t p) c -> p t c", p=128), reads=[g.t_P], writes=[t_iw])
    ikT = ar.alloc([SEQ])
    t_ik = T()
    k.dma("sp", ikT[0:32, :], g.PT[O_IK:O_IK + 32, :], reads=[g.t_PT], writes=[t_ik])
    sq = ar.alloc([NT, 64])
    t_sq = T()
    ss = ar.alloc([NT])
    tt(k, "pool", sq, ckv, ckv, ALU.mult, [t_ckv], [t_sq])
    k.op("dve", lambda e: e.tensor_reduce(out=ss, in_=sq, axis=AX.X, op=ALU.add), [t_sq], [t_sq])
    rsqrt(k, ss, ss, 1.0 / 64, 1e-6, [], [t_sq])
    tt(k, "dve", ckv, ckv, bc_last(ss, 64), ALU.mult, [t_sq], [t_ckv])
    tt(k, "dve", ckv, ckv, bc_mid(gain, NT), ALU.mult, [t_c], [t_ckv])
    ckvT = ar.alloc_bf([SEQ])
    ckvTf = ar.alloc([SEQ])
    t_cT = T()
    for j4 in range(4):
        for jj in range(4):
            j = j4 * 4 + jj
            tp(g, B[j4][0:64, jj * 128:(jj + 1) * 128], ckv[:, j, :], [t_ckv], [tB[j4]])
        cp(k, "act" if j4 % 2 else "dve", ckvTf[0:64, j4 * 512:(j4 + 1) * 512], B[j4][0:64, :], [], [t_cT, tB[j4]])
        cp(k, "pool", ckvT[0:64, j4 * 512:(j4 + 1) * 512], ckvTf[0:64, j4 * 512:(j4 + 1) * 512], [], [t_cT])
    vaug = ar.alloc_bf([NT, 4, 66])
    t_v = T()
    k.op("dve", lambda e: e.memset(vaug[:, :, :, 64:65], 1.0), [], [t_v])
    for j in range(NT):
        bk = 4 + j % 4
        mm(k, B[bk][:, 0:256], ckvTf[0:64, j * 128:(j + 1) * 128], wuv[0:64, :], [t_cT, t_c], [tB[bk]])
        cp(k, "act" if j % 2 else "dve", vaug[:, j, :, 0:64], v3(B[bk][:, 0:256], 64), [], [t_v, tB[bk]])
    ysb = ar.alloc([NT, 256])
    t_y = T()
    iqT = ar.alloc([8, 512])
    t_iq = T()
    qT = ar.alloc_bf([4, 512])
    qTf = ar.alloc([4, 512])
    t_q = T()
    t_qf = T()
    strips = ar.alloc([2816])
    t_st = T()
    MTs = [(ar.alloc_bf([NT, 512]), T()) for _ in range(2)]
    r_sc, r_wk, r_tmp, r_m8 = Rot(ar, [SEQ], 2), Rot(ar, [SEQ], 1), Rot(ar, [512], 3), Rot(ar, [8], 2)
    identb = ar.alloc_bf([128])
    k.op("dve", lambda e: e.tensor_copy(out=identb, in_=g.ident), [g.t_ident], [t_c])
    st = {"sb": 0, "pt": Rot(ar, [512], 5, bf=True), "rd": Rot(ar, [1], 4), "identb": identb, "sbanks": (0, 1), "depth": 3}
    cnt = {"bi": 0}

    def prep_yields(I):
        n = 1
        for qi in range(4):
            i = 4 * I + qi
            n += 8 + (16 if i >= 2 else 0) + 1
        return n

    def prep(I):
        MT, t_MT = MTs[I % 2]
        for ih in range(8):
            k.dma("sp", iqT[0:32, ih, :], g.PT[O_IQ + ih * 32:O_IQ + (ih + 1) * 32, I * 512:(I + 1) * 512], reads=[g.t_PT], writes=[t_iq])
        k.op("pool", lambda e: e.memset(MT, -30000.0), [], [t_MT])
        yield
        for qi in range(4):
            i = 4 * I + qi
            nk = (i + 1) * 128
            sc, t_sc = r_sc.get()
            for ih in range(8):
                for kb in range(0, nk, 512):
                    n = min(512, nk - kb)
                    bk = 2 + cnt["bi"] % 2
                    cnt["bi"] += 1
                    mm(k, B[bk][:, 0:n], iqT[0:32, ih, qi * 128:(qi + 1) * 128], ikT[0:32, kb:kb + n], [t_iq, t_ik], [tB[bk]])
                    if ih == 0:
                        act(k, sc[:, kb:kb + n], B[bk][:, 0:n], AF.Relu, [], [t_sc, tB[bk]])
                        ts(k, "dve", sc[:, kb:kb + n], sc[:, kb:kb + n], iw[:, i, 0:1], None, ALU.mult, None, [t_iw], [t_sc])
                    else:
                        tmp, t_tmp = r_tmp.get()
                        act(k, tmp[:, 0:n], B[bk][:, 0:n], AF.Relu, [], [t_tmp, tB[bk]])
                        stt(k, "dve", sc[:, kb:kb + n], tmp[:, 0:n], iw[:, i, ih:ih + 1], sc[:, kb:kb + n], ALU.mult, ALU.add,
                            [t_tmp, t_iw], [t_sc])
                yield
            tt(k, "dve", sc[:, i * 128:nk], sc[:, i * 128:nk], cneg, ALU.add, [t_c], [t_sc])
            m8, t_m8 = r_m8.get()
            if i >= 2:
                wk, t_wk = r_wk.get()
                cp(k, "pool", wk[:, 0:nk], sc[:, 0:nk], [t_sc], [t_wk])
                for rnd in range(32):
                    k.op("dve", lambda e, o_=m8, i_=wk[:, 0:nk]: e.max(out=o_, in_=i_), [t_wk], [t_m8])
                    if rnd < 31:
                        k.op("dve", lambda e, o_=wk[:, 0:nk], r_=m8: e.match_replace(out=o_, in_to_replace=r_, in_values=o_, imm_value=-1e30),
                             [t_m8], [t_wk])
                    if rnd % 2 == 1:
                        yield
            else:
                k.op("dve", lambda e, o_=m8: e.memset(o_, -1e29), [], [t_m8])
            thr = m8[:, 7:8]
            ts(k, "dve", sc[:, 0:nk], sc[:, 0:nk], thr, None, ALU.is_ge, None, [t_m8], [t_sc])
            for j4 in range(0, i + 1, 4):
                nj = min(4, i + 1 - j4)
                bk = 2 + cnt["bi"] % 2
                cnt["bi"] += 1
                for jj in range(nj):
                    j = j4 + jj
                    tp(g, B[bk][:, jj * 128:(jj + 1) * 128], sc[:, j * 128:(j + 1) * 128], [t_sc], [tB[bk]])
                act(k, MT[:, j4:j4 + nj, qi * 128:(qi + 1) * 128], v3(B[bk][:, 0:nj * 128], 128), AF.Identity, [], [t_MT, tB[bk]],
                    scale=30000.0, bias=g.cm30k[:, 0:1])
            yield

    for _ in prep(0):
        pass
    for I in range(4):
        MT, t_MT = MTs[I % 2]
        nxt = prep(I + 1) if I < 3 else None
        npairs = (4 * I + 4) * 4
        step = -(-prep_yields(I + 1) // npairs) if nxt is not None else 0
        stn = {"g": nxt}

        def after_pair(stn=stn, step=step):
            for _ in range(step):
                if stn["g"] is not None:
                    try:
                        next(stn["g"])
                    except StopIteration:
                        stn["g"] = None
        for h in range(4):
            k.dma("sp", qTf[0:64, h, :], g.PT[O_BQ + h * 64:O_BQ + (h + 1) * 64, I * 512:(I + 1) * 512], reads=[g.t_PT], writes=[t_qf])
        cp(k, "act", qT[0:64, :, :], qTf[0:64, :, :], [t_qf], [t_q])
        for h in range(4):
            k.dma("sp", strips, g.G[4 + h][:, 127:127 + 2816], reads=[g.t_G], writes=[t_st])
            attn_block(g, I, ckvT, t_cT, qT[0:64, h, :], t_q, strips, t_st, lambda j, h=h: vaug[:, j, h, 0:65], t_v, 64, ysb, t_y, h * 64, st,
                       mask=MT, t_mask=[t_MT, t_c][0], after_pair=after_pair)
        if stn["g"] is not None:
            for _ in stn["g"]:
                pass
    k.dma("pool", g.Y[:, 256:512].rearrange("(t p) c -> p t c", p=128), ysb, reads=[t_y], writes=[g.t_Y])


_CACHE = {}


def kernel(**inputs):
    if "nc" not in _CACHE:
        _CACHE["nc"] = build(nlayers=DEPTH, nseq=4, stages=("P", "C", "A", "D", "B", "M", "E"))
    nc, consts = _CACHE["nc"]
    inputs = {n: np.asarray(a, dtype=np.float32) for n, a in inputs.items()}
    in_maps = [make_inputs(inputs, consts, c) for c in range(NCORES)]
    res = run_bass_kernel_spmd(nc, in_maps, core_ids=list(range(NCORES)))
    out = np.concatenate([np.asarray(r["out"]).reshape(4, SEQ, D) for r in res.results], axis=0)
    return out.astype(np.float32)
```

```python
import math
from contextlib import ExitStack
import numpy as np
import concourse.bass as bass
import concourse.mybir as mybir
from concourse.bass_utils import run_bass_kernel_spmd

F32 = mybir.dt.float32
BF16 = mybir.dt.bfloat16
AF = mybir.ActivationFunctionType
ALU = mybir.AluOpType
AX = mybir.AxisListType

NCORES = 8
D = 1024
SEQ = 2048
DEPTH = 4
NT = SEQ // 128
INW = 7096
BW = 256
DN_ALPHA = (2 * DEPTH) ** 0.25
LN_EPS = 1e-5
O_AQ, O_AK, O_AV, O_BQ, O_CKV, O_IQ, O_IK, O_IW = 0, 256, 512, 768, 1024, 1088, 1344, 1376
O_CQ, O_CK, O_CV, O_CA, O_CG, O_D, O_GATE = 1384, 1512, 1640, 1896, 1912, 2168, 3000
import os
CCUT = int(os.environ.get('CCUT', '0'))
NDS = 24
NE = 2048 + 128
WSHAPES = {
    "rpb_table": [32, 8], "b_kv_gain": [4, 64], "b_w_uv": [4, 4, 64, 64], "c_a_up": [4, 16, 128], "c_a_bias": [4, 128],
    "c_norm_gain": [4, 256], "d_mu": [4, 832], "d_w0": [4, 256], "d_w2": [4, 16, 256], "d_a0": [4, 256], "d_a2": [4, 16, 256],
    "d_g2": [4, 32, 256], "d_k_k": [4, 256], "d_k_a": [4, 256], "d_r_k": [4, 256], "d_gn_w": [4, 256], "d_gn_b": [4, 256],
    "w_branch": [4, 4, 256, 1024], "w_out": [4, 1024, 1024], "ln_g": [4, 2, 1024], "ln_b": [4, 2, 1024],
    "router_g": [4, 1024, 4], "router_g_bias": [4, 4], "router_e": [4, 1024, 32], "router_e_bias": [4, 32],
    "moe_w_gate": [4, 32, 1024, 256], "moe_w_up": [4, 32, 1024, 256], "moe_w_down": [4, 32, 256, 1024],
}


class T:
    __slots__ = ("lw", "rd")

    def __init__(self):
        self.lw = None
        self.rd = {}


class K:
    ENG = ("pe", "act", "dve", "pool", "sp")

    def __init__(self, nc, es):
        self.nc = nc
        self.prog = {e: [] for e in self.ENG}
        self.sem = {}
        self.cnt = {}
        for e in self.ENG:
            self.sem[e] = es.enter_context(nc.semaphore("s_" + e))
            self.cnt[e] = 0
        self.known = {e: {} for e in self.ENG}
        self.dq = {}
        self.dqi = {}
        for q in ("sp", "pool", "act"):
            self.dq[q] = []
            self.dqi[q] = 0
            for i in range(NDS):
                key = "d_%s%d" % (q, i)
                self.sem[key] = es.enter_context(nc.semaphore(key))
                self.cnt[key] = 0
                self.dq[q].append(key)

    def _deps(self, reads, writes):
        d = {}
        for r in reads:
            if r.lw is not None:
                k, v = r.lw
                if d.get(k, 0) < v:
                    d[k] = v
        for w in writes:
            if w.lw is not None:
                k, v = w.lw
                if d.get(k, 0) < v:
                    d[k] = v
            for k, v in w.rd.items():
                if d.get(k, 0) < v:
                    d[k] = v
        return d

    def _wait(self, e, d):
        kn = self.known[e]
        for k, v in d.items():
            if k == e and e == "pe":
                continue
            if kn.get(k, 0) < v:
                self.prog[e].append(("w", k, v))
                kn[k] = v

    def op(self, e, fn, reads=(), writes=()):
        self._wait(e, self._deps(reads, writes))
        self.cnt[e] += 1
        c = self.cnt[e]
        self.prog[e].append(("o", fn, e, 1))
        for w in writes:
            w.lw = (e, c)
            w.rd = {}
        for r in reads:
            if r not in writes:
                r.rd[e] = c

    def dma(self, q, out_ap, in_ap, reads=(), writes=(), **kw):
        i = self.dqi[q]
        self.dqi[q] = (i + 1) % NDS
        key = self.dq[q][i]
        d = self._deps(reads, writes)
        if self.cnt[key] > 0 and d.get(key, 0) < self.cnt[key]:
            d[key] = self.cnt[key]
        self._wait(q, d)
        self.cnt[key] += 16
        c = self.cnt[key]
        self.prog[q].append(("o", lambda eng: eng.dma_start(out=out_ap, in_=in_ap, **kw), key, 16))
        for w in writes:
            w.lw = (key, c)
            w.rd = {}
        for r in reads:
            r.rd[key] = c

    def barrier(self):
        d = {k: v for k, v in self.cnt.items() if v > 0}
        for e in self.ENG:
            self._wait(e, d)

    def replay(self):
        nc = self.nc
        with nc.Block() as block:
            def mk(e):
                prog = self.prog[e]
                sem = self.sem

                def body(eng):
                    for it in prog:
                        if it[0] == "w":
                            eng.wait_ge(sem[it[1]], it[2])
                        else:
                            it[1](eng).then_inc(sem[it[2]], it[3])
                return body
            block.tensor(mk("pe"))
            block.scalar(mk("act"))
            block.vector(mk("dve"))
            block.gpsimd(mk("pool"))
            block.sync(mk("sp"))


class Arena:
    def __init__(self, ap, words):
        self.ap = ap
        self.words = words
        self.off = 0
        self.base = 0

    def alloc(self, shape):
        n = int(np.prod(shape))
        assert self.off + n <= self.words, ("arena overflow", self.off, n, self.words)
        v = self.ap[:, self.off:self.off + n]
        self.off += n
        if len(shape) == 2:
            v = v.rearrange("p (a b) -> p a b", b=shape[1])
        elif len(shape) == 3:
            v = v.rearrange("p (a b c) -> p a b c", b=shape[1], c=shape[2])
        return v

    def alloc_bf(self, shape):
        n = int(np.prod(shape))
        assert n % 2 == 0 and self.off + n // 2 <= self.words, ("arena overflow", self.off, n, self.words)
        v = self.ap[:, self.off:self.off + n // 2].bitcast(BF16)
        self.off += n // 2
        if len(shape) == 2:
            v = v.rearrange("p (a b) -> p a b", b=shape[1])
        elif len(shape) == 3:
            v = v.rearrange("p (a b c) -> p a b c", b=shape[1], c=shape[2])
        return v

    def mark(self):
        self.base = self.off

    def reset(self):
        self.off = self.base


def host_consts():
    c = {}
    c["ident"] = np.eye(128, dtype=np.float32)
    s = np.arange(128)
    c["tri_incl"] = (s[:, None] <= s[None, :]).astype(np.float32)
    c["tri_strict"] = (s[:, None] < s[None, :]).astype(np.float32)
    c["ones"] = np.ones((128, 128), np.float32)
    c["tril_strict"] = (s[:, None] > s[None, :]).astype(np.float32)
    c["m05"] = np.full((128, 1), -0.5, np.float32)
    c["m30k"] = np.full((128, 1), -30000.0, np.float32)
    c["caus_neg"] = np.where(s[None, :] > s[:, None], -1e30, 0.0).astype(np.float32)
    hc = np.arange(128) // 32
    hv = np.arange(256) // 64
    c["bm_c"] = (hc[:, None] == hv[None, :]).astype(np.float32)
    c.update(rpb_consts())
    return c


class Ctx:
    pass


def build(nlayers=DEPTH, nseq=4, stages=("P",), dbg=(), extra=None):
    nc = bass.Bass("TRN2", target_bir_lowering=False)
    g = Ctx()
    g.nc = nc
    NTOK = nseq * SEQ

    def din(name, shape):
        return nc.dram_tensor(name, list(shape), F32, kind="ExternalInput").ap()

    def dscr(name, shape, out=False):
        return nc.dram_tensor(name, list(shape), F32, kind="ExternalOutput" if out else "Internal").ap()

    g.x_in = din("x", [NTOK, D])
    g.w_in = din("w_in", [DEPTH, D, INW])
    g.w = {n: din(n, shp) for n, shp in WSHAPES.items()}
    consts = host_consts()
    if extra:
        consts.update(extra)
    g.c = {n: din("c_" + n, a.shape) for n, a in consts.items()}
    g.out = dscr("out", [NTOK, D], out=True)
    g.P = dscr("P", [SEQ, INW], out=("P" in dbg))
    g.PT = dscr("PT", [2048, SEQ], out=("PT" in dbg))
    g.Y = dscr("Y", [SEQ, D], out=("Y" in dbg))
    g.X1 = dscr("X1", [SEQ, D], out=("X1" in dbg))
    g.t_P, g.t_PT, g.t_Y, g.t_X1, g.t_G, g.t_out = T(), T(), T(), T(), T(), T()
    g.G = dscr("G", [8, 128, GP])

    with ExitStack() as es:
        k = K(nc, es)
        g.k = k
        AW = 46 * 1024
        arena_t = es.enter_context(nc.sbuf_tensor("arena", [128, AW], F32))
        g.ar = Arena(arena_t[:, :], AW)
        g.banks = [es.enter_context(nc.psum_tensor("bank%d" % i, [128, 512], F32))[:, :] for i in range(8)]
        g.t_bank = [T() for _ in range(8)]
        ar = g.ar
        g.ident = ar.alloc([128])
        g.t_ident = T()
        k.dma("sp", g.ident, g.c["ident"], writes=[g.t_ident])
        g.cm05 = ar.alloc([1])
        k.dma("sp", g.cm05, g.c["m05"], writes=[g.t_ident])
        g.cm30k = ar.alloc([1])
        k.dma("sp", g.cm30k, g.c["m30k"], writes=[g.t_ident])
        ar.mark()

        if "A" in stages or "B" in stages:
            stage_setup(g)
        for l in range(nlayers):
            for s in range(nseq):
                xsrc = g.x_in if l == 0 else g.out
                if "P" in stages:
                    stage_proj(g, l, s, xsrc)
                if "C" in stages:
                    stage_c(g, l)
                if "A" in stages:
                    stage_a(g, l)
                if "D" in stages:
                    stage_d(g, l)
                if "B" in stages:
                    stage_b(g, l)
                if "Yref" in stages:
                    k.barrier()
                    k.dma("sp", g.Y, g.c["yref"], writes=[g.t_Y])
                if "M" in stages:
                    stage_m(g, l, s, xsrc)
                if "X1ref" in stages:
                    k.barrier()
                    k.dma("sp", g.X1, g.c["x1ref"], writes=[g.t_X1])
                if "E" in stages:
                    stage_e(g, l, s)
        k.barrier()
        k.replay()
    return nc, consts


def stage_proj(g, l, s, xsrc):
    k, ar, nc = g.k, g.ar, g.nc
    k.barrier()
    ar.reset()
    xT = ar.alloc_bf([8, SEQ])
    t_xT = [T() for _ in range(NT)]
    xin = [ar.alloc([D]) for _ in range(2)]
    t_xin = [T(), T()]
    t_bank = [T() for _ in range(8)]
    for tt in range(NT):
        b = tt % 2
        r0 = s * SEQ + tt * 128
        k.dma("sp", xin[b], xsrc[r0:r0 + 128, :], writes=[t_xin[b]])
        for hb in range(2):
            bk = 2 * b + hb
            for kc4 in range(4):
                kc = hb * 4 + kc4
                k.op("pe", lambda e, o=g.banks[bk][:, kc4 * 128:(kc4 + 1) * 128], i=xin[b][:, kc * 128:(kc + 1) * 128]:
                     e.transpose(o, i, g.ident), reads=[t_xin[b], g.t_ident], writes=[t_bank[bk]])
            eng = "dve" if hb == 0 else "act"
            o = xT[:, hb * 4:hb * 4 + 4, tt * 128:(tt + 1) * 128]
            i = g.banks[bk].rearrange("p (a b) -> p a b", b=128)
            if eng == "dve":
                k.op("dve", lambda e, o=o, i=i: e.tensor_copy(out=o, in_=i), reads=[t_bank[bk]], writes=[t_xT[tt]])
            else:
                k.op("act", lambda e, o=o, i=i: e.copy(out=o, in_=i), reads=[t_bank[bk]], writes=[t_xT[tt]])
    wtf = [ar.alloc([8, 512]) for _ in range(2)]
    t_wtf = [T(), T()]
    wt = [ar.alloc_bf([8, 512]) for _ in range(2)]
    t_wt = [T(), T()]
    ot = [ar.alloc([512]) for _ in range(4)]
    t_ot = [T() for _ in range(4)]
    t_P = [g.t_P] * NT
    t_PT = g.t_PT
    oi = 0
    bi = 0
    ncb = (INW + 511) // 512
    for cb in range(ncb):
        c0 = cb * 512
        ncol = min(512, INW - c0)
        wb = cb % 2
        k.dma("sp", wtf[wb][:, :, :ncol], g.w_in[l][:, c0:c0 + ncol].rearrange("(a p) n -> p a n", p=128),
              writes=[t_wtf[wb]])
        cp(k, "pool", wt[wb][:, :, :ncol], wtf[wb][:, :, :ncol], [t_wtf[wb]], [t_wt[wb]])
        for tt in (range(NT) if cb > 0 else ()):
            bk = 4 + (bi % 4)
            bi += 1
            for kc in range(8):
                k.op("pe", lambda e, o=g.banks[bk][:, :ncol], a=xT[:, kc, tt * 128:(tt + 1) * 128], b=wt[wb][:, kc, :ncol], kc=kc:
                     e.matmul(o, a, b, start=(kc == 0), stop=(kc == 7)), reads=[t_xT[tt], t_wt[wb]], writes=[t_bank[bk]])
            ob = oi % 4
            oi += 1
            if oi % 2 == 0:
                k.op("dve", lambda e, o=ot[ob][:, :ncol], i=g.banks[bk][:, :ncol]: e.tensor_copy(out=o, in_=i),
                     reads=[t_bank[bk]], writes=[t_ot[ob]])
            else:
                k.op("act", lambda e, o=ot[ob][:, :ncol], i=g.banks[bk][:, :ncol]: e.copy(out=o, in_=i),
                     reads=[t_bank[bk]], writes=[t_ot[ob]])
            k.dma("pool", g.P[tt * 128:(tt + 1) * 128, c0:c0 + ncol], ot[ob][:, :ncol], reads=[t_ot[ob]], writes=[t_P[tt]])
        for sub in range(4):
            r0 = c0 + sub * 128
            if not (r0 < 1408 or r0 == 1792):
                continue
            for tb in range(4):
                bk = 4 + (bi % 4)
                bi += 1
                for kc in range(8):
                    k.op("pe", lambda e, o=g.banks[bk], a=wt[wb][:, kc, sub * 128:(sub + 1) * 128], b=xT[:, kc, tb * 512:(tb + 1) * 512], kc=kc:
                         e.matmul(o, a, b, start=(kc == 0), stop=(kc == 7)),
                         reads=t_xT[tb * 4:tb * 4 + 4] + [t_wt[wb]], writes=[t_bank[bk]])
                ob = oi % 4
                oi += 1
                if oi % 2 == 0:
                    k.op("dve", lambda e, o=ot[ob], i=g.banks[bk]: e.tensor_copy(out=o, in_=i), reads=[t_bank[bk]], writes=[t_ot[ob]])
                else:
                    k.op("act", lambda e, o=ot[ob], i=g.banks[bk]: e.copy(out=o, in_=i), reads=[t_bank[bk]], writes=[t_ot[ob]])
                k.dma("pool", g.PT[r0:r0 + 128, tb * 512:(tb + 1) * 512], ot[ob], reads=[t_ot[ob]], writes=[t_PT])


def mm(k, out, lhsT, rhs, R, W, start=True, stop=True):
    k.op("pe", lambda e: e.matmul(out, lhsT, rhs, start=start, stop=stop), R, W)


def tp(g, out, in_, R, W):
    n = in_.shape[0]
    g.k.op("pe", lambda e: e.transpose(out, in_, g.ident[:n, :n]), list(R) + [g.t_ident], W)


def tt(k, eng, out, a, b, op, R, W):
    k.op(eng, lambda e: e.tensor_tensor(out=out, in0=a, in1=b, op=op), R, W)


def ts(k, eng, out, a, s1, s2, op0, op1, R, W, accum=None):
    if s2 is None:
        k.op(eng, lambda e: e.tensor_scalar(out=out, in0=a, scalar1=s1, scalar2=None, op0=op0), R, W)
    elif accum is None:
        k.op(eng, lambda e: e.tensor_scalar(out=out, in0=a, scalar1=s1, scalar2=s2, op0=op0, op1=op1), R, W)
    else:
        k.op(eng, lambda e: e.tensor_scalar(out=out, in0=a, scalar1=s1, scalar2=s2, op0=op0, op1=op1, accum_out=accum), R, W)


def stt(k, eng, out, a, sc, b, op0, op1, R, W):
    k.op(eng, lambda e: e.scalar_tensor_tensor(out=out, in0=a, scalar=sc, in1=b, op0=op0, op1=op1), R, W)


def act(k, out, in_, func, R, W, bias=None, scale=1.0, accum=None):
    kw = {}
    if bias is not None:
        kw["bias"] = bias
    if accum is not None:
        kw["accum_out"] = accum
    k.op("act", lambda e: e.activation(out=out, in_=in_, func=func, scale=scale, **kw), R, W)


def rsqrt(k, out, in_, scale, eps, R, W):
    ts(k, "dve", out, in_, scale, eps, ALU.mult, ALU.add, R, W)
    k.op("act", lambda e: e.activation(out=out, in_=out, func=AF.Sqrt), [], W)
    k.op("dve", lambda e: e.reciprocal(out=out, in_=out), [], W)


def cp(k, eng, out, in_, R, W):
    if eng == "act":
        k.op("act", lambda e: e.copy(out=out, in_=in_), R, W)
    else:
        k.op(eng, lambda e: e.tensor_copy(out=out, in_=in_), R, W)


def bcast_rows(ap1d, n):
    return bass.AP(ap1d.tensor, ap1d.offset, [[0, 128], [1, n]])


class Rot:
    def __init__(self, ar, shape, n, bf=False):
        self.b = [((ar.alloc_bf(shape) if bf else ar.alloc(shape)), T()) for _ in range(n)]
        self.i = 0

    def get(self):
        r = self.b[self.i % len(self.b)]
        self.i += 1
        return r


class Banks:
    def __init__(self, g, ids):
        self.b = [(g.banks[i], g.t_bank[i]) for i in ids]
        self.i = 0

    def get(self):
        r = self.b[self.i % len(self.b)]
        self.i += 1
        return r


def stage_c(g, l):
    k, ar = g.k, g.ar
    k.barrier()
    ar.reset()
    W = g.w
    t_c = T()
    aup = ar.alloc([128])
    abias = ar.alloc([128])
    gain = ar.alloc([256])
    bm = ar.alloc([512])
    tri = ar.alloc([128])
    ones = ar.alloc([128])
    k.dma("sp", aup[0:16, :], W["c_a_up"][l], writes=[t_c])
    k.dma("sp", abias, bcast_rows(W["c_a_bias"][l], 128), writes=[t_c])
    k.dma("sp", gain, bcast_rows(W["c_norm_gain"][l], 256), writes=[t_c])
    for p_ in range(2):
        k.dma("sp", bm[0:64, p_ * 256:(p_ + 1) * 256], g.c["bm_c"][p_ * 64:(p_ + 1) * 64, :], writes=[t_c])
    k.dma("sp", tri, g.c["tri_incl"], writes=[t_c])
    k.dma("sp", ones, g.c["ones"], writes=[t_c])
    state = ar.alloc([256])
    t_state = T()
    k.op("dve", lambda e: e.memset(state, 0.0), [], [t_state])
    pin = Rot(ar, [784], 2)
    alT = Rot(ar, [128], 2)
    sb = lambda n, w: Rot(ar, [w], n)
    r_zb, r_sp, r_cum, r_eq, r_ek, r_el, r_dec = sb(2, 128), sb(2, 128), sb(2, 128), sb(2, 128), sb(2, 128), sb(2, 128), sb(2, 4)
    r_qd, r_ki, r_kl, r_qkT, r_att, r_o, r_tmp, r_sq, r_ss, r_sg, r_y = (sb(2, 128), sb(2, 128), sb(2, 128), sb(2, 1024), sb(2, 512),
                                                                        sb(2, 256), sb(2, 256), sb(2, 256), sb(2, 4), sb(2, 256), sb(2, 256))
    pb = g.banks
    ps_z, ps_cum, ps_last, ps_lt = pb[0][:, 0:128], pb[1][:, 0:128], pb[2][:, 0:128], pb[5][:, 0:4]
    ps_tr = pb[3]
    ps_att = pb[4]
    ps_o = pb[6][:, 0:256]
    ps_upd = pb[7][:, 0:256]
    tb_ = g.t_bank
    t_z, t_cum, t_last, t_lt, t_tr, t_att, t_o, t_upd = tb_[0], tb_[1], tb_[2], tb_[5], tb_[3], tb_[4], tb_[6], tb_[7]
    for tt_ in range(NT):
        r0 = tt_ * 128
        x, t_x = pin.get()
        k.dma("sp", x, g.P[r0:r0 + 128, O_CQ:O_CQ + 784], reads=[g.t_P], writes=[t_x])
        al, t_al = alT.get()
        k.dma("sp", al[0:16, :], g.PT[O_CA:O_CA + 16, r0:r0 + 128], reads=[g.t_PT], writes=[t_al])
        q, kk, v, og = x[:, 0:128], x[:, 128:256], x[:, 256:512], x[:, 528:784]
        mm(k, ps_z, al[0:16, :], aup[0:16, :], [t_al, t_c], [t_z])
        zb, t_zb = r_zb.get()
        tt(k, "dve", zb, ps_z, abias, ALU.add, [t_c], [t_zb, t_z])
        sp_, t_sp = r_sp.get()
        act(k, sp_, zb, AF.Exp, [t_zb], [t_sp], scale=-1.0)
        act(k, sp_, sp_, AF.Ln, [], [t_sp], bias=1.0)
        if CCUT == 1:
            continue
        mm(k, ps_cum, tri, sp_, [t_c, t_sp], [t_cum])
        mm(k, ps_last, ones, sp_, [t_c, t_sp], [t_last])
        for h in range(4):
            mm(k, ps_lt[0:32, h:h + 1], sp_[:, h * 32:(h + 1) * 32], ones[:, 0:1], [t_c, t_sp], [t_lt])
        cum, t_cs = r_cum.get()
        cp(k, "dve", cum, ps_cum, [], [t_cs, t_cum])
        eq, t_eq = r_eq.get()
        act(k, eq, cum, AF.Exp, [t_cs], [t_eq], scale=-1.0 / 16)
        ek, t_ek = r_ek.get()
        act(k, ek, cum, AF.Exp, [t_cs], [t_ek], scale=1.0 / 16)
        el, t_el = r_el.get()
        tt(k, "dve", el, ps_last, cum, ALU.subtract, [t_cs], [t_el, t_last])
        act(k, el, el, AF.Exp, [], [t_el], scale=-1.0 / 16)
        dec, t_dec = r_dec.get()
        act(k, dec[0:32, :], ps_lt[0:32, :], AF.Exp, [], [t_dec, t_lt], scale=-1.0 / 16)
        if CCUT == 2:
            continue
        qd, t_qd = r_qd.get()
        stt(k, "dve", qd, q, 32 ** -0.5, eq, ALU.mult, ALU.mult, [t_x, t_eq], [t_qd])
        ki, t_ki = r_ki.get()
        tt(k, "pool", ki, kk, ek, ALU.mult, [t_x, t_ek], [t_ki])
        kl, t_kl = r_kl.get()
        tt(k, "pool", kl, kk, el, ALU.mult, [t_x, t_el], [t_kl])
        if CCUT == 5:
            continue
        for h in range(4):
            tp(g, ps_tr[0:32, h * 128:(h + 1) * 128], qd[:, h * 32:(h + 1) * 32], [t_qd], [t_tr])
        tp4 = g.banks[2]
        for h in range(4):
            tp(g, tp4[0:32, h * 128:(h + 1) * 128], ki[:, h * 32:(h + 1) * 32], [t_ki], [t_last])
        qkT, t_qkT = r_qkT.get()
        cp(k, "act", qkT[0:32, 0:512], ps_tr[0:32, :], [], [t_qkT, t_tr])
        cp(k, "act", qkT[0:32, 512:1024], tp4[0:32, :], [], [t_qkT, t_last])
        for h in range(4):
            mm(k, ps_att[:, h * 128:(h + 1) * 128], qkT[0:32, 512 + h * 128:512 + (h + 1) * 128],
               qkT[0:32, h * 128:(h + 1) * 128], [t_qkT], [t_att])
        att, t_at = r_att.get()
        for h in range(4):
            tt(k, "dve", att[:, h * 128:(h + 1) * 128], ps_att[:, h * 128:(h + 1) * 128], tri, ALU.mult, [t_c], [t_at, t_att])
        for h in range(4):
            mm(k, ps_o[:, h * 64:(h + 1) * 64], qkT[0:32, h * 128:(h + 1) * 128], state[0:32, h * 64:(h + 1) * 64],
               [t_qkT, t_state], [t_o], start=True, stop=False)
            mm(k, ps_o[:, h * 64:(h + 1) * 64], att[:, h * 128:(h + 1) * 128], v[:, h * 64:(h + 1) * 64], [t_at, t_x], [t_o],
               start=False, stop=True)
        for h in range(4):
            mm(k, ps_upd[0:32, h * 64:(h + 1) * 64], kl[:, h * 32:(h + 1) * 32], v[:, h * 64:(h + 1) * 64], [t_kl, t_x], [t_upd])
        tmp, t_tmp = r_tmp.get()
        cp(k, "act", tmp[0:32, :], ps_upd[0:32, :], [], [t_tmp, t_upd])
        for h in range(4):
            stt(k, "dve", state[0:32, h * 64:(h + 1) * 64], state[0:32, h * 64:(h + 1) * 64], dec[0:32, h:h + 1],
                tmp[0:32, h * 64:(h + 1) * 64], ALU.mult, ALU.add, [t_dec, t_tmp], [t_state])
        if CCUT == 4:
            continue
        o, t_os = r_o.get()
        cp(k, "act", o, ps_o, [], [t_os, t_o])
        sq, t_sq = r_sq.get()
        tt(k, "pool", sq, o, o, ALU.mult, [t_os], [t_sq])
        ss, t_ss = r_ss.get()
        k.op("dve", lambda e, o_=ss, i_=sq.rearrange("p (h v) -> p h v", v=64): e.tensor_reduce(out=o_, in_=i_, axis=AX.X, op=ALU.add),
             [t_sq], [t_ss])
        rsqrt(k, ss, ss, 1.0 / 64, 1e-6, [], [t_ss])
        sg, t_sg = r_sg.get()
        act(k, sg, og, AF.Silu, [t_x], [t_sg])
        tt(k, "pool", sg, sg, gain, ALU.mult, [t_c], [t_sg])
        y, t_y = r_y.get()
        for h in range(4):
            stt(k, "dve", y[:, h * 64:(h + 1) * 64], o[:, h * 64:(h + 1) * 64], ss[:, h:h + 1], sg[:, h * 64:(h + 1) * 64],
                ALU.mult, ALU.mult, [t_os, t_ss, t_sg], [t_y])
        k.dma("pool", g.Y[r0:r0 + 128, 512:768], y, reads=[t_y], writes=[g.t_Y])


def make_inputs(inputs, consts, core, nseq=4, small=True):
    m = {}
    m["x"] = np.ascontiguousarray(inputs["x"][core * nseq:(core + 1) * nseq]).reshape(nseq * SEQ, D)
    m["w_in"] = np.ascontiguousarray(inputs["w_in"])
    for n in WSHAPES:
        m[n] = np.ascontiguousarray(inputs[n])
    for n, a in consts.items():
        m["c_" + n] = a
    return m


NEV = 2944
GP = NEV + 128


def t5_bucket_np(dist):
    d = np.maximum(dist, 0)
    df = np.maximum(d, 1).astype(np.float32)
    large = 16 + (np.log(df / np.float32(16)) / np.float32(math.log(2048 / 16)) * np.float32(16)).astype(np.int32)
    large = np.minimum(large, 31)
    return np.where(d < 16, d, large)


def rpb_consts():
    i = np.arange(NEV)
    dist = i - 511
    valid = (dist >= 0) & (dist <= 2047)
    b = t5_bucket_np(dist)
    oht = np.zeros((32, NEV), np.float32)
    oht[b[valid], i[valid]] = 1.0
    cA = ((dist <= 128).astype(np.float32) + ((dist % 4 == 0) & (dist <= 512)) + ((dist % 16 == 0) & (dist <= 2048))) * valid
    cB = valid.astype(np.float32)
    return {"oht": oht, "cmul": np.stack([cA, cB]).astype(np.float32)}


def stage_setup(g):
    k, ar = g.k, g.ar
    k.barrier()
    ar.reset()
    t_c = T()
    oht = ar.alloc([NEV])
    cm = [ar.alloc([NEV]), ar.alloc([NEV])]
    tab = ar.alloc([8])
    ones = ar.alloc([128])
    k.dma("sp", oht[0:32, :], g.c["oht"], writes=[t_c])
    for a in range(2):
        k.dma("sp", cm[a], bcast_rows(g.c["cmul"][a], NEV), writes=[t_c])
    k.dma("sp", tab[0:32, :], g.w["rpb_table"], writes=[t_c])
    k.dma("sp", ones, g.c["ones"], writes=[t_c])
    tabb = Rot(ar, [128], 2)
    eb = Rot(ar, [NEV], 2)
    bi = 0
    for hh in range(8):
        tb, t_tb = tabb.get()
        ts(k, "dve", tb[0:32, :], ones[0:32, :], tab[0:32, hh:hh + 1], None, ALU.mult, None, [t_c], [t_tb])
        e_, t_e = eb.get()
        for c0 in range(0, NEV, 512):
            n = min(512, NEV - c0)
            bk = bi % 4
            bi += 1
            mm(k, g.banks[bk][:, :n], tb[0:32, :], oht[0:32, c0:c0 + n], [t_tb, t_c], [g.t_bank[bk]])
            act(k, e_[:, c0:c0 + n], g.banks[bk][:, :n], AF.Exp, [], [t_e, g.t_bank[bk]])
        tt(k, "dve", e_, e_, cm[0 if hh < 4 else 1], ALU.mult, [t_c], [t_e])
        gh = g.G[hh]
        dst = bass.AP(gh.tensor, gh.offset, [[GP + 1, 128], [1, NEV]])
        k.dma("pool", dst, e_, reads=[t_e], writes=[g.t_G])


def attn_block(g, I, kT, t_kT, qT, t_q, strips, t_st, vaug, t_v, vw, ysb, t_y, ycol, st, mask=None, t_mask=None, after_pair=None):
    k = g.k

    def scores(j):
        bk = st["sbanks"][st["sb"] % len(st["sbanks"])]
        st["sb"] += 1
        ps = g.banks[bk]
        if mask is None:
            mm(k, ps, kT[0:64, j * 128:(j + 1) * 128], qT, [t_kT, t_q], [g.t_bank[bk]])
        else:
            mm(k, ps, kT[0:64, j * 128:(j + 1) * 128], qT, [t_kT, t_q], [g.t_bank[bk]], start=True, stop=False)
            mm(k, ps, st["identb"], mask[:, j, :], [t_mask], [g.t_bank[bk]], start=False, stop=True)
        pt, t_pt = st["pt"].get()
        act(k, pt, ps, AF.Exp, [], [t_pt, g.t_bank[bk]], scale=0.125)
        o = 4 * I - j
        tt(k, "dve", pt, pt, strips[:, (o + 3) * 128:(o + 3) * 128 + 512], ALU.mult, [t_st], [t_pt])
        return pt, t_pt

    def pv(j, pt, t_pt):
        for qi in range(4):
            i = 4 * I + qi
            if j > i:
                continue
            ob = 4 + qi
            mm(k, g.banks[ob][:, 0:vw + 1], pt[:, qi * 128:(qi + 1) * 128], vaug(j), [t_pt, t_v], [g.t_bank[ob]],
               start=(j == 0), stop=(j == i))
            if j == i:
                rd, t_rd = st["rd"].get()
                k.op("dve", lambda e, o_=rd, i_=g.banks[ob][:, vw:vw + 1]: e.reciprocal(out=o_, in_=i_), [], [t_rd, g.t_bank[ob]])
                ts(k, "dve", ysb[:, i, ycol:ycol + vw], g.banks[ob][:, 0:vw], rd[:, 0:1], None, ALU.mult, None,
                   [t_rd], [t_y, g.t_bank[ob]])
        if after_pair is not None:
            after_pair()

    pend = []
    depth = st.get("depth", len(st["sbanks"]))
    for j in range(4 * I + 4):
        pend.append((j,) + scores(j))
        if len(pend) > depth - 1:
            pv(*pend.pop(0))
    while pend:
        pv(*pend.pop(0))


def gen_a(g, l, fresh=True):
    k, ar = g.k, g.ar
    if fresh:
        k.barrier()
        ar.reset()
    ysb = ar.alloc([NT, 256])
    t_y = T()
    st = {"sb": 0, "pt": Rot(ar, [512], 6, bf=True), "rd": Rot(ar, [1], 4), "sbanks": (0, 1, 2, 3)}
    qTs, kTs, sts, vas = Rot(ar, [SEQ], 2, bf=True), Rot(ar, [SEQ], 2, bf=True), Rot(ar, [2816], 2), Rot(ar, [NT, 66], 2, bf=True)
    ldq, ldk, ldv = Rot(ar, [SEQ], 1), Rot(ar, [SEQ], 1), Rot(ar, [NT, 64], 1)
    yield
    pend = []
    for h in range(4):
        qT, t_q = qTs.get()
        kT, t_kT = kTs.get()
        strips, t_st = sts.get()
        va, t_v = vas.get()
        fq, t_fq = ldq.get()
        fk, t_fk = ldk.get()
        fv, t_fv = ldv.get()
        k.dma("sp", fq[0:64, :], g.PT[O_AQ + h * 64:O_AQ + (h + 1) * 64, :], reads=[g.t_PT], writes=[t_fq])
        k.dma("sp", fk[0:64, :], g.PT[O_AK + h * 64:O_AK + (h + 1) * 64, :], reads=[g.t_PT], writes=[t_fk])
        k.dma("sp", strips, g.G[h][:, 127:127 + 2816], reads=[g.t_G], writes=[t_st])
        k.dma("sp", fv, g.P[:, O_AV + h * 64:O_AV + (h + 1) * 64].rearrange("(t p) c -> p t c", p=128),
              reads=[g.t_P], writes=[t_fv])
        cp(k, "pool", qT[0:64, :], fq[0:64, :], [t_fq], [t_q])
        cp(k, "act", kT[0:64, :], fk[0:64, :], [t_fk], [t_kT])
        cp(k, "pool", va[:, :, 0:64], fv, [t_fv], [t_v])
        k.op("dve", lambda e, o_=va[:, :, 64:65]: e.memset(o_, 1.0), [], [t_v])
        for I in range(4):
            attn_block(g, I, kT, t_kT, qT[0:64, I * 512:(I + 1) * 512], t_q, strips, t_st, lambda j, va=va: va[:, j, 0:65], t_v, 64, ysb, t_y,
                       h * 64, st, after_pair=lambda: pend.append(1))
            while pend:
                pend.pop()
                yield
    k.dma("pool", g.Y[:, 0:256].rearrange("(t p) c -> p t c", p=128), ysb, reads=[t_y], writes=[g.t_Y])


def stage_a(g, l):
    for _ in gen_a(g, l):
        pass


def stage_ad(g, l):
    k, ar = g.k, g.ar
    k.barrier()
    ar.reset()
    ga = gen_a(g, l, fresh=False)
    next(ga)
    gd = gen_d(g, l, fresh=False, banks=(2, 3))
    next(gd)
    da = dd = False
    while not (da and dd):
        if not da:
            try:
                next(ga)
            except StopIteration:
                da = True
        for _ in range(2):
            if not dd:
                try:
                    next(gd)
                except StopIteration:
                    dd = True


def ln_tile(k, r, t_r, gbc, bbc, t_c, sm, out, t_out):
    s1, t_s1 = sm.get()
    k.op("dve", lambda e: e.tensor_reduce(out=s1[:, 0:1], in_=r, axis=AX.X, op=ALU.add), [t_r], [t_s1])
    ts(k, "dve", s1[:, 0:1], s1[:, 0:1], -1.0 / D, None, ALU.mult, None, [], [t_s1])
    ts(k, "dve", r, r, s1[:, 0:1], None, ALU.add, None, [t_s1], [t_r])
    act(k, out, r, AF.Square, [t_r], [t_out, t_s1], accum=s1[:, 1:2])
    rsqrt(k, s1[:, 1:2], s1[:, 1:2], 1.0 / D, LN_EPS, [], [t_s1])
    stt(k, "dve", out, r, s1[:, 1:2], gbc, ALU.mult, ALU.mult, [t_r, t_s1, t_c], [t_out])
    tt(k, "pool", out, out, bbc, ALU.add, [t_c], [t_out])


def stage_m(g, l, s, xsrc):
    k, ar = g.k, g.ar
    k.barrier()
    ar.reset()
    W = g.w
    t_c = T()
    wb = ar.alloc_bf([8, D])
    wo = ar.alloc_bf([8, D])
    gbc = ar.alloc([D])
    bbc = ar.alloc([D])
    stg = Rot(ar, [2, D], 2)
    for c2 in range(4):
        sb_, t_sb = stg.get()
        k.dma("sp", sb_, W["w_branch"][l, c2].rearrange("(c p) d -> p c d", p=128), writes=[t_sb])
        cp(k, "pool" if c2 % 2 else "act", wb[:, 2 * c2:2 * c2 + 2, :], sb_, [t_sb], [t_c])
    for c2 in range(4):
        sb_, t_sb = stg.get()
        k.dma("sp", sb_, W["w_out"][l][c2 * 256:(c2 + 1) * 256, :].rearrange("(c p) d -> p c d", p=128), writes=[t_sb])
        cp(k, "pool" if c2 % 2 else "act", wo[:, 2 * c2:2 * c2 + 2, :], sb_, [t_sb], [t_c])
    k.dma("sp", gbc, bcast_rows(W["ln_g"][l, 0], D), writes=[t_c])
    k.dma("sp", bbc, bcast_rows(W["ln_b"][l, 0], D), writes=[t_c])
    r_y, r_gate, r_yT, r_mg, r_mT, r_x, r_tmp, r_out, sm = (Rot(ar, [D], 2), Rot(ar, [4 * D], 2), Rot(ar, [D], 2, bf=True), Rot(ar, [D], 2),
                                                          Rot(ar, [D], 2, bf=True), Rot(ar, [D], 2), Rot(ar, [512], 3), Rot(ar, [D], 2), Rot(ar, [2], 4))
    B, tB = g.banks, g.t_bank
    bi = 0
    for tt_ in range(NT):
        r0 = tt_ * 128
        y, t_y = r_y.get()
        k.dma("sp", y, g.Y[r0:r0 + 128, :], reads=[g.t_Y], writes=[t_y])
        gt, t_gt = r_gate.get()
        k.dma("sp", gt, g.P[r0:r0 + 128, O_GATE:O_GATE + 4 * D], reads=[g.t_P], writes=[t_gt])
        x, t_x = r_x.get()
        k.dma("sp", x, xsrc[s * SEQ + r0:s * SEQ + r0 + 128, :], writes=[t_x])
        act(k, gt, gt, AF.Sigmoid, [], [t_gt])
        yT, t_yT = r_yT.get()
        for hb in range(2):
            for c4 in range(4):
                c = hb * 4 + c4
                tp(g, B[hb][:, c4 * 128:(c4 + 1) * 128], y[:, c * 128:(c + 1) * 128], [t_y], [tB[hb]])
            cp(k, "act" if hb else "dve", yT[:, hb * 512:(hb + 1) * 512], B[hb], [], [t_yT, tB[hb]])
        mg, t_mg = r_mg.get()
        for n in range(4):
            for dh in range(2):
                bk = 2 + bi % 2
                bi += 1
                for cc in range(2):
                    c = 2 * n + cc
                    mm(k, B[bk], yT[:, c * 128:(c + 1) * 128], wb[:, c, dh * 512:(dh + 1) * 512], [t_yT, t_c], [tB[bk]],
                       start=(cc == 0), stop=(cc == 1))
                gsl = gt[:, n * D + dh * 512:n * D + (dh + 1) * 512]
                msl = mg[:, dh * 512:(dh + 1) * 512]
                if n == 0:
                    tt(k, "dve", msl, B[bk], gsl, ALU.mult, [t_gt], [t_mg, tB[bk]])
                else:
                    tmp, t_tmp = r_tmp.get()
                    tt(k, "dve", tmp, B[bk], gsl, ALU.mult, [t_gt], [t_tmp, tB[bk]])
                    tt(k, "pool", msl, msl, tmp, ALU.add, [t_tmp], [t_mg])
        mT, t_mT = r_mT.get()
        for hb in range(2):
            for c4 in range(4):
                c = hb * 4 + c4
                tp(g, B[4 + hb][:, c4 * 128:(c4 + 1) * 128], mg[:, c * 128:(c + 1) * 128], [t_mg], [tB[4 + hb]])
            cp(k, "act" if hb else "dve", mT[:, hb * 512:(hb + 1) * 512], B[4 + hb], [], [t_mT, tB[4 + hb]])
        for dh in range(2):
            bk = 6 + dh
            for c in range(8):
                mm(k, B[bk], mT[:, c * 128:(c + 1) * 128], wo[:, c, dh * 512:(dh + 1) * 512], [t_mT, t_c], [tB[bk]],
                   start=(c == 0), stop=(c == 7))
            stt(k, "dve", x[:, dh * 512:(dh + 1) * 512], x[:, dh * 512:(dh + 1) * 512], DN_ALPHA, B[bk], ALU.mult, ALU.add,
                [], [t_x, tB[bk]])
        o, t_o = r_out.get()
        ln_tile(k, x, t_x, gbc, bbc, t_c, sm, o, t_o)
        k.dma("pool", g.X1[r0:r0 + 128, :], o, reads=[t_o], writes=[g.t_X1])


def stage_e(g, l, s):
    k, ar = g.k, g.ar
    W = g.w
    B, tB = g.banks, g.t_bank
    for half in range(2):
        k.barrier()
        ar.reset()
        t_c = T()
        wr = ar.alloc([8, 36])
        rb = ar.alloc([36])
        gbc = ar.alloc([D])
        bbc = ar.alloc([D])
        k.dma("sp", wr[:, :, 0:4], W["router_g"][l].rearrange("(c p) e -> p c e", p=128), writes=[t_c])
        k.dma("sp", wr[:, :, 4:36], W["router_e"][l].rearrange("(c p) e -> p c e", p=128), writes=[t_c])
        k.dma("sp", rb[:, 0:4], bcast_rows(W["router_g_bias"][l], 4), writes=[t_c])
        k.dma("sp", rb[:, 4:36], bcast_rows(W["router_e_bias"][l], 32), writes=[t_c])
        k.dma("sp", gbc, bcast_rows(W["ln_g"][l, 1], D), writes=[t_c])
        k.dma("sp", bbc, bcast_rows(W["ln_b"][l, 1], D), writes=[t_c])
        r_x1 = Rot(ar, [D], 2)
        xT = ar.alloc_bf([8, 1024])
        t_xT = [T() for _ in range(8)]
        r_xf = Rot(ar, [8, 128], 2)
        yacc = ar.alloc([8, D])
        t_ya = [T() for _ in range(8)]
        gates = ar.alloc([8, 32])
        t_g = [T() for _ in range(8)]
        sm = Rot(ar, [40], 4)
        sm2 = Rot(ar, [8], 4)
        sm3 = Rot(ar, [32], 6)
        for ti in range(8):
            r0 = half * 1024 + ti * 128
            x1t, t_x1t = r_x1.get()
            k.dma("sp", x1t, g.X1[r0:r0 + 128, :], reads=[g.t_X1], writes=[t_x1t])
            for hb in range(2):
                for c4 in range(4):
                    c = hb * 4 + c4
                    tp(g, B[hb][:, c4 * 128:(c4 + 1) * 128], x1t[:, c * 128:(c + 1) * 128], [t_x1t], [tB[hb]])
                if hb == 0:
                    xf, t_xf = r_xf.get()
                cp(k, "act" if hb else "dve", xf[:, hb * 4:hb * 4 + 4, :], B[hb].rearrange("p (a b) -> p a b", b=128), [], [t_xf, tB[hb]])
            cp(k, "pool", xT[:, :, ti * 128:(ti + 1) * 128], xf, [t_xf], [t_xT[ti]])
            for c in range(8):
                mm(k, B[2][:, 0:36], xf[:, c, :], wr[:, c, :], [t_xf, t_c], [tB[2]], start=(c == 0), stop=(c == 7))
            lg, t_lg = sm.get()
            tt(k, "dve", lg[:, 0:36], B[2][:, 0:36], rb, ALU.add, [t_c], [t_lg, tB[2]])
            a, t_a = sm2.get()
            k.op("dve", lambda e, o_=a[:, 0:1], i_=lg[:, 0:4]: e.tensor_reduce(out=o_, in_=i_, axis=AX.X, op=ALU.max), [t_lg], [t_a])
            ts(k, "dve", a[:, 1:2], a[:, 0:1], -1.0, None, ALU.mult, None, [], [t_a])
            e4, t_e4 = sm3.get()
            act(k, e4[:, 0:4], lg[:, 0:4], AF.Exp, [t_lg, t_a], [t_e4, t_a], bias=a[:, 1:2], accum=a[:, 2:3])
            ohg, t_ohg = sm3.get()
            ts(k, "dve", ohg[:, 0:4], lg[:, 0:4], a[:, 0:1], None, ALU.is_equal, None, [t_lg, t_a], [t_ohg])
            ts(k, "dve", ohg[:, 0:4], ohg[:, 0:4], -1.0, 1e30, ALU.add, ALU.mult, [], [t_ohg])
            lem, t_lem = sm3.get()
            for gi in range(4):
                ts(k, "dve", lem[:, gi * 8:(gi + 1) * 8], lg[:, 4 + gi * 8:4 + (gi + 1) * 8], ohg[:, gi:gi + 1], None, ALU.add, None,
                   [t_lg, t_ohg], [t_lem])
            k.op("dve", lambda e, o_=a[:, 3:4], i_=lem: e.tensor_reduce(out=o_, in_=i_, axis=AX.X, op=ALU.max), [t_lem], [t_a])
            oh1, t_oh1 = sm3.get()
            ts(k, "dve", oh1, lem, a[:, 3:4], None, ALU.is_equal, None, [t_lem, t_a], [t_oh1])
            stt(k, "dve", lem, oh1, -1e30, lem, ALU.mult, ALU.add, [t_oh1], [t_lem])
            k.op("dve", lambda e, o_=a[:, 4:5], i_=lem: e.tensor_reduce(out=o_, in_=i_, axis=AX.X, op=ALU.max), [t_lem], [t_a])
            oh2, t_oh2 = sm3.get()
            ts(k, "dve", oh2, lem, a[:, 4:5], None, ALU.is_equal, None, [t_lem, t_a], [t_oh2])
            tt(k, "dve", a[:, 5:6], a[:, 4:5], a[:, 3:4], ALU.subtract, [], [t_a])
            act(k, a[:, 5:6], a[:, 5:6], AF.Exp, [], [t_a])
            ts(k, "dve", a[:, 6:7], a[:, 5:6], 1.0, None, ALU.add, None, [], [t_a])
            tt(k, "dve", a[:, 6:7], a[:, 6:7], a[:, 2:3], ALU.mult, [], [t_a])
            k.op("dve", lambda e, o_=a[:, 6:7]: e.reciprocal(out=o_, in_=o_), [], [t_a])
            tt(k, "dve", a[:, 7:8], a[:, 6:7], a[:, 5:6], ALU.mult, [], [t_a])
            ts(k, "dve", gates[:, ti, :], oh1, a[:, 6:7], None, ALU.mult, None, [t_oh1, t_a], [t_g[ti]])
            stt(k, "dve", gates[:, ti, :], oh2, a[:, 7:8], gates[:, ti, :], ALU.mult, ALU.add, [t_oh2, t_a], [t_g[ti]])
        r_wg, r_wu, r_wd = Rot(ar, [8, 256], 2, bf=True), Rot(ar, [8, 256], 2, bf=True), Rot(ar, [2, D], 2, bf=True)
        f_wg, f_wu, f_wd = Rot(ar, [8, 256], 2), Rot(ar, [8, 256], 2), Rot(ar, [2, D], 2)
        r_sg, r_h = Rot(ar, [512], 2), Rot(ar, [2, 512], 2, bf=True)
        bi = 0
        for ex in range(32):
            wg, t_wg = r_wg.get()
            wu, t_wu = r_wu.get()
            wd, t_wd = r_wd.get()
            fg, t_fg = f_wg.get()
            fu, t_fu = f_wu.get()
            fd, t_fd = f_wd.get()
            k.dma("sp", fg, W["moe_w_gate"][l, ex].rearrange("(c p) h -> p c h", p=128), writes=[t_fg])
            k.dma("sp", fu, W["moe_w_up"][l, ex].rearrange("(c p) h -> p c h", p=128), writes=[t_fu])
            k.dma("sp", fd, W["moe_w_down"][l, ex].rearrange("(c p) d -> p c d", p=128), writes=[t_fd])
            cp(k, "pool", wg, fg, [t_fg], [t_wg])
            cp(k, "pool", wu, fu, [t_fu], [t_wu])
            cp(k, "act", wd, fd, [t_fd], [t_wd])
            for tb in range(2):
                hT, t_h = r_h.get()
                for hc in range(2):
                    for c in range(8):
                        mm(k, B[0], wg[:, c, hc * 128:(hc + 1) * 128], xT[:, c, tb * 512:(tb + 1) * 512], [t_wg] + t_xT[tb * 4:tb * 4 + 4], [tB[0]],
                           start=(c == 0), stop=(c == 7))
                    for c in range(8):
                        mm(k, B[1], wu[:, c, hc * 128:(hc + 1) * 128], xT[:, c, tb * 512:(tb + 1) * 512], [t_wu] + t_xT[tb * 4:tb * 4 + 4], [tB[1]],
                           start=(c == 0), stop=(c == 7))
                    sg, t_sg = r_sg.get()
                    act(k, sg, B[0], AF.Silu, [], [t_sg, tB[0]])
                    tt(k, "dve", hT[:, hc, :], B[1], sg, ALU.mult, [t_sg], [t_h, tB[1]])
                for q4 in range(4):
                    ti = tb * 4 + q4
                    for dh in range(2):
                        bk = 2 + bi % 6
                        bi += 1
                        for hc in range(2):
                            mm(k, B[bk], hT[:, hc, q4 * 128:(q4 + 1) * 128], wd[:, hc, dh * 512:(dh + 1) * 512], [t_h, t_wd], [tB[bk]],
                               start=(hc == 0), stop=(hc == 1))
                        ysl = yacc[:, ti, dh * 512:(dh + 1) * 512]
                        if ex == 0:
                            ts(k, "dve", ysl, B[bk], gates[:, ti, ex:ex + 1], None, ALU.mult, None, [t_g[ti]], [t_ya[ti], tB[bk]])
                        else:
                            stt(k, "dve", ysl, B[bk], gates[:, ti, ex:ex + 1], ysl, ALU.mult, ALU.add, [t_g[ti]], [t_ya[ti], tB[bk]])
        r_out = Rot(ar, [D], 2)
        for ti in range(8):
            r0 = s * SEQ + half * 1024 + ti * 128
            x1t, t_x1t = r_x1.get()
            k.dma("sp", x1t, g.X1[half * 1024 + ti * 128:half * 1024 + (ti + 1) * 128, :], reads=[g.t_X1], writes=[t_x1t])
            stt(k, "dve", yacc[:, ti, :], x1t, DN_ALPHA, yacc[:, ti, :], ALU.mult, ALU.add, [t_x1t], [t_ya[ti]])
            o, t_o = r_out.get()
            ln_tile(k, yacc[:, ti, :], t_ya[ti], gbc, bbc, t_c, sm2, o, t_o)
            k.dma("pool", g.out[r0:r0 + 128, :], o, reads=[t_o], writes=[g.t_out])


def bc_last(ap, n):
    return bass.AP(ap.tensor, ap.offset, [list(x) for x in ap.ap] + [[0, n]])


def v3(ap, inner):
    return ap.rearrange("p (a b) -> p a b", b=inner)


def stage_d(g, l):
    for _ in gen_d(g, l):
        pass


def gen_d(g, l, fresh=True, banks=tuple(range(8))):
    k, ar = g.k, g.ar
    if fresh:
        k.barrier()
        ar.reset()
    W = g.w
    B, tB = g.banks, g.t_bank
    st = {"b": 0}

    def nb():
        st["b"] = (st["b"] + 1) % len(banks)
        return banks[st["b"]]
    t_c = T()
    mu = ar.alloc([832])
    wcat = ar.alloc([768])
    vec = {n: ar.alloc([256]) for n in ("d_w0", "d_a0", "d_k_k", "d_k_a", "d_r_k", "d_gn_w", "d_gn_b")}
    tri_i, tri_s, tril_s, ones, idn = ar.alloc([128]), ar.alloc([128]), ar.alloc([128]), ar.alloc([128]), g.ident
    k.dma("sp", mu, bcast_rows(W["d_mu"][l], 832), writes=[t_c])
    k.op("dve", lambda e: e.memset(wcat, 0.0), [], [t_c])
    k.dma("sp", wcat[0:16, 0:256], W["d_w2"][l], writes=[t_c])
    k.dma("sp", wcat[16:32, 256:512], W["d_a2"][l], writes=[t_c])
    k.dma("sp", wcat[32:64, 512:768], W["d_g2"][l], writes=[t_c])
    for n, a in vec.items():
        k.dma("sp", a, bcast_rows(W[n][l], 256), writes=[t_c])
    k.dma("sp", tri_i, g.c["tri_incl"], writes=[t_c])
    k.dma("sp", tri_s, g.c["tri_strict"], writes=[t_c])
    k.dma("sp", tril_s, g.c["tril_strict"], writes=[t_c])
    k.dma("sp", ones, g.c["ones"], writes=[t_c])
    S = ar.alloc([256])
    t_S = T()
    k.op("dve", lambda e: e.memset(S, 0.0), [], [t_S])
    R2 = lambda w: Rot(ar, [w], 2)
    r_cur, r_prev, r_seg, r_z, r_zT = R2(832), R2(832), R2(832), R2(64), R2(128)
    r_nlw, r_a, r_g, r_kk, r_kp, r_sm, r_tmp = R2(256), R2(256), R2(256), R2(256), R2(256), Rot(ar, [16], 4), Rot(ar, [256], 4)
    r_en, r_ep, r_ex = R2(256), R2(256), R2(256)
    r_at, r_bt, r_kt, r_rt = R2(256), R2(256), R2(256), R2(256)
    r_FT = Rot(ar, [4, 512], 2)
    r_wT = R2(4)
    r_M = Rot(ar, [5, 512], 1)
    r_P, r_PT, r_XT = Rot(ar, [512], 2), Rot(ar, [512], 2), Rot(ar, [512], 2)
    r_rhs0, r_U, r_y, r_o = R2(256), R2(256), R2(256), R2(256)
    yield
    for tt_ in range(NT):
        r0 = tt_ * 128
        cur, t_cur = r_cur.get()
        prev, t_prev = r_prev.get()
        k.dma("sp", cur, g.P[r0:r0 + 128, O_D:O_D + 832], reads=[g.t_P], writes=[t_cur])
        if tt_ == 0:
            k.op("dve", lambda e, o_=prev[0:1, :]: e.memset(o_, 0.0), [], [t_prev])
            k.dma("sp", prev[1:128, :], g.P[0:127, O_D:O_D + 832], reads=[g.t_P], writes=[t_prev])
        else:
            k.dma("sp", prev, g.P[r0 - 1:r0 + 127, O_D:O_D + 832], reads=[g.t_P], writes=[t_prev])
        seg, t_seg = r_seg.get()
        tt(k, "pool", prev, prev, cur, ALU.subtract, [t_cur], [t_prev])
        tt(k, "pool", prev, prev, mu, ALU.mult, [t_c], [t_prev])
        tt(k, "dve", seg, cur, prev, ALU.add, [t_cur, t_prev], [t_seg])
        r, kraw, v = seg[:, 0:256], seg[:, 256:512], seg[:, 512:768]
        z, t_z = r_z.get()
        act(k, z[:, 0:16], seg[:, 768:784], AF.Tanh, [t_seg], [t_z])
        cp(k, "dve", z[:, 16:32], seg[:, 784:800], [t_seg], [t_z])
        act(k, z[:, 32:64], seg[:, 800:832], AF.Sigmoid, [t_seg], [t_z])
        b0 = nb()
        tp(g, B[b0][0:64, 0:128], z, [t_z], [tB[b0]])
        zT, t_zT = r_zT.get()
        cp(k, "act", zT[0:64, :], B[b0][0:64, 0:128], [], [t_zT, tB[b0]])
        bw, bg_ = nb(), nb()
        mm(k, B[bw], zT[0:64, :], wcat[0:64, 0:512], [t_zT, t_c], [tB[bw]])
        mm(k, B[bg_][:, 0:256], zT[0:64, :], wcat[0:64, 512:768], [t_zT, t_c], [tB[bg_]])
        nlw, t_nlw = r_nlw.get()
        tt(k, "dve", nlw, B[bw][:, 0:256], vec["d_w0"], ALU.add, [t_c], [t_nlw, tB[bw]])
        a, t_a = r_a.get()
        tt(k, "dve", a, B[bw][:, 256:512], vec["d_a0"], ALU.add, [t_c], [t_a, tB[bw]])
        gsb, t_gs = r_g.get()
        cp(k, "act", gsb, B[bg_][:, 0:256], [], [t_gs, tB[bg_]])
        act(k, nlw, nlw, AF.Exp, [], [t_nlw], scale=-1.0)
        act(k, nlw, nlw, AF.Ln, [], [t_nlw], bias=1.0)
        act(k, nlw, nlw, AF.Exp, [], [t_nlw], scale=-1.0, bias=g.cm05[:, 0:1])
        act(k, a, a, AF.Sigmoid, [], [t_a])
        yield
        kk, t_kk = r_kk.get()
        tt(k, "pool", kk, kraw, vec["d_k_k"], ALU.mult, [t_seg, t_c], [t_kk])
        tmp, t_tmp = r_tmp.get()
        tt(k, "pool", tmp, kk, kk, ALU.mult, [t_kk], [t_tmp])
        sm, t_sm = r_sm.get()
        k.op("dve", lambda e, o_=sm[:, 0:4], i_=v3(tmp, 64): e.tensor_reduce(out=o_, in_=i_, axis=AX.X, op=ALU.add), [t_tmp], [t_sm])
        k.op("act", lambda e, o_=sm[:, 0:4]: e.activation(out=o_, in_=o_, func=AF.Sqrt), [], [t_sm])
        ts(k, "dve", sm[:, 0:4], sm[:, 0:4], 1e-12, None, ALU.max, None, [], [t_sm])
        k.op("dve", lambda e, o_=sm[:, 0:4]: e.reciprocal(out=o_, in_=o_), [], [t_sm])
        tt(k, "dve", v3(kk, 64), v3(kk, 64), bc_last(sm[:, 0:4], 64), ALU.mult, [t_sm], [t_kk])
        kp, t_kp = r_kp.get()
        stt(k, "dve", kp, a, -1.0, vec["d_k_a"], ALU.add, ALU.mult, [t_a, t_c], [t_kp])
        stt(k, "dve", kp, kp, 1.0, kraw, ALU.add, ALU.mult, [t_seg], [t_kp])
        yield
        bc = nb()
        mm(k, B[bc][:, 0:256], tri_i, nlw, [t_c, t_nlw], [tB[bc]])
        for h in range(4):
            mm(k, B[bc][0:64, 256 + h:257 + h], nlw[:, h * 64:(h + 1) * 64], ones[:, 0:1], [t_nlw, t_c], [tB[bc]])
        en, t_en = r_en.get()
        ep, t_ep = r_ep.get()
        ex, t_ex = r_ex.get()
        wT, t_wT = r_wT.get()
        act(k, en, B[bc][:, 0:256], AF.Exp, [], [t_en, tB[bc]], scale=-1.0)
        act(k, ep, B[bc][:, 0:256], AF.Exp, [], [t_ep, tB[bc]])
        tt(k, "dve", ex, B[bc][:, 0:256], nlw, ALU.subtract, [t_nlw], [t_ex, tB[bc]])
        act(k, wT[0:64, :], B[bc][0:64, 256:260], AF.Exp, [], [t_wT, tB[bc]], scale=-1.0)
        act(k, ex, ex, AF.Exp, [], [t_ex], scale=-1.0)
        at, t_at = r_at.get()
        bt, t_bt = r_bt.get()
        kt, t_kt = r_kt.get()
        rt, t_rt = r_rt.get()
        stt(k, "dve", at, kk, -1.0, ex, ALU.mult, ALU.mult, [t_kk, t_ex], [t_at])
        tt(k, "pool", bt, kk, a, ALU.mult, [t_kk, t_a], [t_bt])
        tt(k, "pool", bt, bt, ep, ALU.mult, [t_ep], [t_bt])
        tt(k, "dve", kt, kp, ep, ALU.mult, [t_kp, t_ep], [t_kt])
        tt(k, "pool", rt, r, en, ALU.mult, [t_seg, t_en], [t_rt])
        yield
        FT, t_FT = r_FT.get()
        for qi, (src, t_src) in enumerate(((at, t_at), (bt, t_bt), (kt, t_kt), (rt, t_rt))):
            bq = nb()
            for h in range(4):
                tp(g, B[bq][0:64, h * 128:(h + 1) * 128], src[:, h * 64:(h + 1) * 64], [t_src], [tB[bq]])
            cp(k, "act" if qi % 2 else "dve", FT[0:64, qi, :], B[bq][0:64, :], [], [t_FT, tB[bq]])
        aT = lambda h: FT[0:64, 0, h * 128:(h + 1) * 128]
        bT = lambda h: FT[0:64, 1, h * 128:(h + 1) * 128]
        kT = lambda h: FT[0:64, 2, h * 128:(h + 1) * 128]
        rT = lambda h: FT[0:64, 3, h * 128:(h + 1) * 128]
        yield
        M, t_M = r_M.get()
        specs = ((bT, aT, tri_s), (aT, bT, tril_s), (kT, aT, tri_s), (bT, rT, tri_i), (kT, rT, tri_i))
        for mi, (lf, rf, msk) in enumerate(specs):
            bq = nb()
            for h in range(4):
                mm(k, B[bq][:, h * 128:(h + 1) * 128], lf(h), rf(h), [t_FT], [tB[bq]])
            for h in range(4):
                tt(k, "dve", M[:, mi, h * 128:(h + 1) * 128], B[bq][:, h * 128:(h + 1) * 128], msk, ALU.mult, [t_c], [t_M, tB[bq]])
        LT, L, LakT, ArbT, ArkT = (M[:, i, :] for i in range(5))
        yield
        XT, t_XT = r_XT.get()
        for h in range(4):
            tt(k, "pool", XT[:, h * 128:(h + 1) * 128], LT[:, h * 128:(h + 1) * 128], idn, ALU.add, [t_M, g.t_ident], [t_XT])
        P_, t_P_ = L, t_M
        PT_, t_PT_ = LT, t_M
        for step in range(1, 7):
            b1 = nb()
            for h in range(4):
                hs = slice(h * 128, (h + 1) * 128)
                mm(k, B[b1][:, hs], PT_[:, hs], P_[:, hs], [t_P_, t_PT_], [tB[b1]])
            Pn, t_Pn = r_P.get()
            cp(k, "act", Pn, B[b1], [], [t_Pn, tB[b1]])
            if step < 6:
                b2 = nb()
                for h in range(4):
                    hs = slice(h * 128, (h + 1) * 128)
                    mm(k, B[b2][:, hs], P_[:, hs], PT_[:, hs], [t_P_, t_PT_], [tB[b2]])
                PTn, t_PTn = r_PT.get()
                cp(k, "dve", PTn, B[b2], [], [t_PTn, tB[b2]])
            b3 = nb()
            for h in range(4):
                hs = slice(h * 128, (h + 1) * 128)
                mm(k, B[b3][:, hs], Pn[:, hs], XT[:, hs], [t_Pn, t_XT], [tB[b3]])
            XTn, t_XTn = r_XT.get()
            tt(k, "dve", XTn, XT, B[b3], ALU.add, [t_XT], [t_XTn, tB[b3]])
            XT, t_XT = XTn, t_XTn
            yield
            P_, t_P_ = Pn, t_Pn
            if step < 6:
                PT_, t_PT_ = PTn, t_PTn
        yield
        b1 = nb()
        for h in range(4):
            vs = slice(h * 64, (h + 1) * 64)
            mm(k, B[b1][:, vs], aT(h), S[0:64, vs], [t_FT, t_S], [tB[b1]], start=True, stop=False)
            mm(k, B[b1][:, vs], LakT[:, h * 128:(h + 1) * 128], v[:, vs], [t_M, t_seg], [tB[b1]], start=False, stop=True)
        rhs0, t_rhs0 = r_rhs0.get()
        cp(k, "act", rhs0, B[b1][:, 0:256], [], [t_rhs0, tB[b1]])
        b2 = nb()
        for h in range(4):
            vs = slice(h * 64, (h + 1) * 64)
            mm(k, B[b2][:, vs], XT[:, h * 128:(h + 1) * 128], rhs0[:, vs], [t_XT, t_rhs0], [tB[b2]])
        U, t_U = r_U.get()
        cp(k, "dve", U, B[b2][:, 0:256], [], [t_U, tB[b2]])
        yield
        b3 = nb()
        for h in range(4):
            vs = slice(h * 64, (h + 1) * 64)
            hs = slice(h * 128, (h + 1) * 128)
            mm(k, B[b3][:, vs], rT(h), S[0:64, vs], [t_FT, t_S], [tB[b3]], start=True, stop=False)
            mm(k, B[b3][:, vs], ArbT[:, hs], U[:, vs], [t_M, t_U], [tB[b3]], start=False, stop=False)
            mm(k, B[b3][:, vs], ArkT[:, hs], v[:, vs], [t_M, t_seg], [tB[b3]], start=False, stop=True)
        y, t_y = r_y.get()
        cp(k, "act", y, B[b3][:, 0:256], [], [t_y, tB[b3]])
        yield
        b4 = nb()
        for h in range(4):
            vs = slice(h * 64, (h + 1) * 64)
            mm(k, B[b4][0:64, vs], bt[:, vs], U[:, vs], [t_bt, t_U], [tB[b4]], start=True, stop=False)
            mm(k, B[b4][0:64, vs], kt[:, vs], v[:, vs], [t_kt, t_seg], [tB[b4]], start=False, stop=True)
        tt(k, "dve", S[0:64, :], S[0:64, :], B[b4][0:64, 0:256], ALU.add, [], [t_S, tB[b4]])
        tt(k, "dve", v3(S[0:64, :], 64), v3(S[0:64, :], 64), bc_last(wT[0:64, :], 64), ALU.mult, [t_wT], [t_S])
        yield
        sm2, t_sm2 = r_sm.get()
        k.op("dve", lambda e, o_=sm2[:, 0:4], i_=v3(y, 64): e.tensor_reduce(out=o_, in_=i_, axis=AX.X, op=ALU.add), [t_y], [t_sm2])
        ts(k, "dve", sm2[:, 0:4], sm2[:, 0:4], -1.0 / 64, None, ALU.mult, None, [], [t_sm2])
        tt(k, "dve", v3(y, 64), v3(y, 64), bc_last(sm2[:, 0:4], 64), ALU.add, [t_sm2], [t_y])
        tmp2, t_tmp2 = r_tmp.get()
        tt(k, "pool", tmp2, y, y, ALU.mult, [t_y], [t_tmp2])
        k.op("dve", lambda e, o_=sm2[:, 4:8], i_=v3(tmp2, 64): e.tensor_reduce(out=o_, in_=i_, axis=AX.X, op=ALU.add), [t_tmp2], [t_sm2])
        rsqrt(k, sm2[:, 4:8], sm2[:, 4:8], 1.0 / 64, 64e-5, [], [t_sm2])
        tt(k, "dve", v3(y, 64), v3(y, 64), bc_last(sm2[:, 4:8], 64), ALU.mult, [t_sm2], [t_y])
        tt(k, "pool", y, y, vec["d_gn_w"], ALU.mult, [t_c], [t_y])
        tt(k, "pool", y, y, vec["d_gn_b"], ALU.add, [t_c], [t_y])
        tmp3, t_tmp3 = r_tmp.get()
        tt(k, "dve", tmp3, r, kp, ALU.mult, [t_seg, t_kp], [t_tmp3])
        tt(k, "dve", tmp3, tmp3, vec["d_r_k"], ALU.mult, [t_c], [t_tmp3])
        k.op("dve", lambda e, o_=sm2[:, 8:12], i_=v3(tmp3, 64): e.tensor_reduce(out=o_, in_=i_, axis=AX.X, op=ALU.add), [t_tmp3], [t_sm2])
        tt(k, "dve", v3(tmp3, 64), v3(v, 64), bc_last(sm2[:, 8:12], 64), ALU.mult, [t_seg, t_sm2], [t_tmp3])
        tt(k, "pool", y, y, tmp3, ALU.add, [t_tmp3], [t_y])
        o, t_o = r_o.get()
        tt(k, "dve", o, y, gsb, ALU.mult, [t_y, t_gs], [t_o])
        k.dma("pool", g.Y[r0:r0 + 128, 768:1024], o, reads=[t_o], writes=[g.t_Y])


def bc_mid(ap, n):
    a = [list(x) for x in ap.ap]
    return bass.AP(ap.tensor, ap.offset, [a[0], [0, n]] + a[1:])


def stage_b(g, l):
    k, ar = g.k, g.ar
    k.barrier()
    ar.reset()
    W = g.w
    B, tB = g.banks, g.t_bank
    t_c = T()
    gain = ar.alloc([64])
    wuv = ar.alloc([256])
    cneg = ar.alloc([128])
    k.dma("sp", gain, bcast_rows(W["b_kv_gain"][l], 64), writes=[t_c])
    k.dma("sp", v3(wuv[0:64, :], 64), W["b_w_uv"][l].rearrange("h c d -> c h d"), writes=[t_c])
    k.dma("sp", cneg, g.c["caus_neg"], writes=[t_c])
    ckv = ar.alloc([NT, 64])
    t_ckv = T()
    k.dma("sp", ckv, g.P[:, O_CKV:O_CKV + 64].rearrange("(t p) c -> p t c", p=128), reads=[g.t_P], writes=[t_ckv])
    iw = ar.alloc([NT, 8])
    t_iw = T()
    k.dma("sp", iw, g.P[:, O_IW:O_IW + 8].rearrange("(t p) c -> p t c", p=128), reads=[g.t_P], writes=[t_iw])
    ikT = ar.alloc([SEQ])
    t_ik = T()
    k.dma("sp", ikT[0:32, :], g.PT[O_IK:O_IK + 32, :], reads=[g.t_PT], writes=[t_ik])
    sq = ar.alloc([NT, 64])
    t_sq = T()
    ss = ar.alloc([NT])
    tt(k, "pool", sq, ckv, ckv, ALU.mult, [t_ckv], [t_sq])
    k.op("dve", lambda e: e.tensor_reduce(out=ss, in_=sq, axis=AX.X, op=ALU.add), [t_sq], [t_sq])
    rsqrt(k, ss, ss, 1.0 / 64, 1e-6, [], [t_sq])
    tt(k, "dve", ckv, ckv, bc_last(ss, 64), ALU.mult, [t_sq], [t_ckv])
    tt(k, "dve", ckv, ckv, bc_mid(gain, NT), ALU.mult, [t_c], [t_ckv])
    ckvT = ar.alloc_bf([SEQ])
    ckvTf = ar.alloc([SEQ])
    t_cT = T()
    for j4 in range(4):
        for jj in range(4):
            j = j4 * 4 + jj
            tp(g, B[j4][0:64, jj * 128:(jj + 1) * 128], ckv[:, j, :], [t_ckv], [tB[j4]])
        cp(k, "act" if j4 % 2 else "dve", ckvTf[0:64, j4 * 512:(j4 + 1) * 512], B[j4][0:64, :], [], [t_cT, tB[j4]])
        cp(k, "pool", ckvT[0:64, j4 * 512:(j4 + 1) * 512], ckvTf[0:64, j4 * 512:(j4 + 1) * 512], [], [t_cT])
    vaug = ar.alloc_bf([NT, 4, 66])
    t_v = T()
    k.op("dve", lambda e: e.memset(vaug[:, :, :, 64:65], 1.0), [], [t_v])
    for j in range(NT):
        bk = 4 + j % 4
        mm(k, B[bk][:, 0:256], ckvTf[0:64, j * 128:(j + 1) * 128], wuv[0:64, :], [t_cT, t_c], [tB[bk]])
        cp(k, "act" if j % 2 else "dve", vaug[:, j, :, 0:64], v3(B[bk][:, 0:256], 64), [], [t_v, tB[bk]])
    ysb = ar.alloc([NT, 256])
    t_y = T()
    iqT = ar.alloc([8, 512])
    t_iq = T()
    qT = ar.alloc_bf([4, 512])
    qTf = ar.alloc([4, 512])
    t_q = T()
    t_qf = T()
    strips = ar.alloc([2816])
    t_st = T()
    MTs = [(ar.alloc_bf([NT, 512]), T()) for _ in range(2)]
    r_sc, r_wk, r_tmp, r_m8 = Rot(ar, [SEQ], 2), Rot(ar, [SEQ], 1), Rot(ar, [512], 3), Rot(ar, [8], 2)
    identb = ar.alloc_bf([128])
    k.op("dve", lambda e: e.tensor_copy(out=identb, in_=g.ident), [g.t_ident], [t_c])
    st = {"sb": 0, "pt": Rot(ar, [512], 5, bf=True), "rd": Rot(ar, [1], 4), "identb": identb, "sbanks": (0, 1), "depth": 3}
    cnt = {"bi": 0}

    def prep_yields(I):
        n = 1
        for qi in range(4):
            i = 4 * I + qi
            n += 8 + (16 if i >= 2 else 0) + 1
        return n

    def prep(I):
        MT, t_MT = MTs[I % 2]
        for ih in range(8):
            k.dma("sp", iqT[0:32, ih, :], g.PT[O_IQ + ih * 32:O_IQ + (ih + 1) * 32, I * 512:(I + 1) * 512], reads=[g.t_PT], writes=[t_iq])
        k.op("pool", lambda e: e.memset(MT, -30000.0), [], [t_MT])
        yield
        for qi in range(4):
            i = 4 * I + qi
            nk = (i + 1) * 128
            sc, t_sc = r_sc.get()
            for ih in range(8):
                for kb in range(0, nk, 512):
                    n = min(512, nk - kb)
                    bk = 2 + cnt["bi"] % 2
                    cnt["bi"] += 1
                    mm(k, B[bk][:, 0:n], iqT[0:32, ih, qi * 128:(qi + 1) * 128], ikT[0:32, kb:kb + n], [t_iq, t_ik], [tB[bk]])
                    if ih == 0:
                        act(k, sc[:, kb:kb + n], B[bk][:, 0:n], AF.Relu, [], [t_sc, tB[bk]])
                        ts(k, "dve", sc[:, kb:kb + n], sc[:, kb:kb + n], iw[:, i, 0:1], None, ALU.mult, None, [t_iw], [t_sc])
                    else:
                        tmp, t_tmp = r_tmp.get()
                        act(k, tmp[:, 0:n], B[bk][:, 0:n], AF.Relu, [], [t_tmp, tB[bk]])
                        stt(k, "dve", sc[:, kb:kb + n], tmp[:, 0:n], iw[:, i, ih:ih + 1], sc[:, kb:kb + n], ALU.mult, ALU.add,
                            [t_tmp, t_iw], [t_sc])
                yield
            tt(k, "dve", sc[:, i * 128:nk], sc[:, i * 128:nk], cneg, ALU.add, [t_c], [t_sc])
            m8, t_m8 = r_m8.get()
            if i >= 2:
                wk, t_wk = r_wk.get()
                cp(k, "pool", wk[:, 0:nk], sc[:, 0:nk], [t_sc], [t_wk])
                for rnd in range(32):
                    k.op("dve", lambda e, o_=m8, i_=wk[:, 0:nk]: e.max(out=o_, in_=i_), [t_wk], [t_m8])
                    if rnd < 31:
                        k.op("dve", lambda e, o_=wk[:, 0:nk], r_=m8: e.match_replace(out=o_, in_to_replace=r_, in_values=o_, imm_value=-1e30),
                             [t_m8], [t_wk])
                    if rnd % 2 == 1:
                        yield
            else:
                k.op("dve", lambda e, o_=m8: e.memset(o_, -1e29), [], [t_m8])
            thr = m8[:, 7:8]
            ts(k, "dve", sc[:, 0:nk], sc[:, 0:nk], thr, None, ALU.is_ge, None, [t_m8], [t_sc])
            for j4 in range(0, i + 1, 4):
                nj = min(4, i + 1 - j4)
                bk = 2 + cnt["bi"] % 2
                cnt["bi"] += 1
                for jj in range(nj):
                    j = j4 + jj
                    tp(g, B[bk][:, jj * 128:(jj + 1) * 128], sc[:, j * 128:(j + 1) * 128], [t_sc], [tB[bk]])
                act(k, MT[:, j4:j4 + nj, qi * 128:(qi + 1) * 128], v3(B[bk][:, 0:nj * 128], 128), AF.Identity, [], [t_MT, tB[bk]],
                    scale=30000.0, bias=g.cm30k[:, 0:1])
            yield

    for _ in prep(0):
        pass
    for I in range(4):
        MT, t_MT = MTs[I % 2]
        nxt = prep(I + 1) if I < 3 else None
        npairs = (4 * I + 4) * 4
        step = -(-prep_yields(I + 1) // npairs) if nxt is not None else 0
        stn = {"g": nxt}

        def after_pair(stn=stn, step=step):
            for _ in range(step):
                if stn["g"] is not None:
                    try:
                        next(stn["g"])
                    except StopIteration:
                        stn["g"] = None
        for h in range(4):
            k.dma("sp", qTf[0:64, h, :], g.PT[O_BQ + h * 64:O_BQ + (h + 1) * 64, I * 512:(I + 1) * 512], reads=[g.t_PT], writes=[t_qf])
        cp(k, "act", qT[0:64, :, :], qTf[0:64, :, :], [t_qf], [t_q])
        for h in range(4):
            k.dma("sp", strips, g.G[4 + h][:, 127:127 + 2816], reads=[g.t_G], writes=[t_st])
            attn_block(g, I, ckvT, t_cT, qT[0:64, h, :], t_q, strips, t_st, lambda j, h=h: vaug[:, j, h, 0:65], t_v, 64, ysb, t_y, h * 64, st,
                       mask=MT, t_mask=[t_MT, t_c][0], after_pair=after_pair)
        if stn["g"] is not None:
            for _ in stn["g"]:
                pass
    k.dma("pool", g.Y[:, 256:512].rearrange("(t p) c -> p t c", p=128), ysb, reads=[t_y], writes=[g.t_Y])


_CACHE = {}


def kernel(**inputs):
    if "nc" not in _CACHE:
        _CACHE["nc"] = build(nlayers=DEPTH, nseq=4, stages=("P", "C", "A", "D", "B", "M", "E"))
    nc, consts = _CACHE["nc"]
    inputs = {n: np.asarray(a, dtype=np.float32) for n, a in inputs.items()}
    in_maps = [make_inputs(inputs, consts, c) for c in range(NCORES)]
    res = run_bass_kernel_spmd(nc, in_maps, core_ids=list(range(NCORES)))
    out = np.concatenate([np.asarray(r["out"]).reshape(4, SEQ, D) for r in res.results], axis=0)
    return out.astype(np.float32)
```
